# Optimizing a Trainium2 kernel written in Bass

```python
import math
import jax
import jax.numpy as jnp
from jax import lax
import numpy as np

D_MODEL = 2048
BATCH = 8
SEQ = 4096
DEPTH = 4

CTX_LEN = 256
GRID_W = 64

RW_HEADS = 16
RW_HEAD = 64
RW_WIDTH = RW_HEADS * RW_HEAD
DECAY_LORA = 96
ICLR_LORA = 96
GATE_LORA = 64
RW_STREAM = 3 * RW_WIDTH + GATE_LORA + 2 * DECAY_LORA + 2 * ICLR_LORA
RW_GN_EPS = 64e-5

CONV_WIDTH = 1024
CONV_K = 31
EVEN_IN = RW_STREAM + 2 * CONV_WIDTH
EVEN_MIX = RW_WIDTH + CONV_WIDTH

ATT_HEADS = 16
ATT_KV_HEADS = 4
ATT_HEAD = 128
ATT_GROUP = ATT_HEADS // ATT_KV_HEADS
ATT_Q = ATT_HEADS * ATT_HEAD
ATT_KV = ATT_KV_HEADS * ATT_HEAD
ATT_IN = ATT_Q + 2 * ATT_KV
Q_BLOCK = 128
ROPE_THETA = 10000.0
ROPE_PAIRS = ATT_HEAD // 4
QK_EPS = 1e-6

N_EXPERTS = 64
TOP_K = 6
EXPERT_FF = 384
SHARED_FF = 384
ROUTE_SCALE = 2.5
EXPERT_BLOCK = 128

N_EVEN = (DEPTH + 1) // 2
N_ODD = DEPTH // 2
DEEPNORM_ALPHA = (2 * DEPTH) ** 0.25
DEEPNORM_BETA = (8 * DEPTH) ** -0.25
LN_EPS = 1e-5

kernel_name = 'hybrid_rwkv7_conformer_gqa_moe_dit'


def layer_norm(x, g, b):
    xf = x.astype(jnp.float32)
    mu = xf.mean(-1, keepdims=True)
    var = jnp.square(xf - mu).mean(-1, keepdims=True)
    return ((xf - mu) * lax.rsqrt(var + LN_EPS) * g + b).astype(x.dtype)


def rms_norm(x, g):
    xf = x.astype(jnp.float32)
    return (xf * lax.rsqrt(jnp.square(xf).mean(-1, keepdims=True) + QK_EPS) * g).astype(x.dtype)


def split_heads(t, n_heads, head_dim):
    return t.reshape(t.shape[:-1] + (n_heads, head_dim))


def axial_angles(n_tokens):
    rows_n = n_tokens // GRID_W
    row = jnp.repeat(jnp.arange(rows_n), GRID_W)
    col = jnp.tile(jnp.arange(GRID_W), rows_n)
    inv = ROPE_THETA ** (-jnp.arange(ROPE_PAIRS, dtype=jnp.float32) / ROPE_PAIRS)
    ang = jnp.stack([row[:, None] * inv, col[:, None] * inv], axis=1)
    return jnp.cos(ang), jnp.sin(ang)


def axial_rope(x, cos, sin):
    b_, s_, h_, _ = x.shape
    xr = x.astype(jnp.float32).reshape(b_, s_, h_, 2, 2, ROPE_PAIRS)
    x1, x2 = xr[..., 0, :], xr[..., 1, :]
    c = cos[None, :, None]
    s = sin[None, :, None]
    out = jnp.stack([x1 * c - x2 * s, x1 * s + x2 * c], axis=-2)
    return out.reshape(x.shape).astype(x.dtype)


def centred_shift(p, mu):
    prev = jnp.pad(p, ((0, 0), (1, 0), (0, 0)))[:, :-1]
    nxt = jnp.pad(p, ((0, 0), (0, 1), (0, 0)))[:, 1:]
    return p + mu[0] * (prev - p) + mu[1] * (nxt - p)


def rwkv_features(p, mu, w0, w_up, a0, a_up, g_up, k_k, k_a):
    p = centred_shift(p.astype(jnp.float32), mu)
    w_ = RW_WIDTH
    r, k, v = p[..., :w_], p[..., w_:2 * w_], p[..., 2 * w_:3 * w_]
    o = 3 * w_
    gd = p[..., o:o + GATE_LORA]
    o += GATE_LORA
    wd = p[..., o:o + 2 * DECAY_LORA].reshape(p.shape[:-1] + (2, DECAY_LORA))
    o += 2 * DECAY_LORA
    ad = p[..., o:o + 2 * ICLR_LORA].reshape(p.shape[:-1] + (2, ICLR_LORA))
    w_log = -jax.nn.softplus(-(w0 + jnp.einsum('btdl,dlc->btdc', jnp.tanh(wd), w_up))) - 0.5
    decay = jnp.exp(-jnp.exp(w_log))
    a = jax.nn.sigmoid(a0 + jnp.einsum('btdl,dlc->btdc', ad, a_up))
    g = jax.nn.sigmoid(gd) @ g_up
    kk = split_heads(k * k_k, RW_HEADS, RW_HEAD)
    kk = kk * lax.rsqrt(jnp.sum(jnp.square(kk), -1, keepdims=True) + 1e-12)
    k_dir = k[..., None, :] * (1.0 + (a - 1.0) * k_a)
    return r, v, g, kk, decay, a, k_dir


def wkv_scan(state0, r, w, k, v, kk, a, reverse):
    xs = tuple(jnp.swapaxes(t, 0, 1) for t in (r, w, k, v, kk, kk * a))

    def step(s, inp):
        r_t, w_t, k_t, v_t, kk_t, b_t = inp
        sk = jnp.einsum('bhvk,bhk->bhv', s, kk_t)
        s = s * w_t[:, :, None, :] - sk[..., None] * b_t[:, :, None, :] + v_t[..., None] * k_t[:, :, None, :]
        return s, jnp.einsum('bhvk,bhk->bhv', s, r_t)

    s_last, ys = lax.scan(step, state0, xs, reverse=reverse)
    return s_last, jnp.swapaxes(ys, 0, 1)


def rwkv_bidir(feats, states0, r_k, gn_g, gn_b):
    r, v, g, kk, decay, a, k_dir = feats
    rh = split_heads(r, RW_HEADS, RW_HEAD)
    vh = split_heads(v, RW_HEADS, RW_HEAD)
    ys, bonuses, finals = [], [], []
    for d in range(2):
        kd = split_heads(k_dir[:, :, d], RW_HEADS, RW_HEAD)
        s_fin, y_d = wkv_scan(states0[d], rh, split_heads(decay[:, :, d], RW_HEADS, RW_HEAD), kd, vh, kk,
                              split_heads(a[:, :, d], RW_HEADS, RW_HEAD), reverse=(d == 1))
        ys.append(y_d)
        finals.append(s_fin)
        bonuses.append(jnp.sum(rh * kd * r_k, -1, keepdims=True) * vh)
    y = ys[0] + ys[1]
    mean = y.mean(-1, keepdims=True)
    var = jnp.square(y - mean).mean(-1, keepdims=True)
    y = ((y - mean) * lax.rsqrt(var + RW_GN_EPS)).reshape(r.shape) * gn_g + gn_b
    out = (y + (bonuses[0] + bonuses[1]).reshape(r.shape)) * g
    return out, finals


def rwkv_time_mix(p_lat, p_ctx, mu, w0, w_up, a0, a_up, g_up, k_k, k_a, r_k, gn_g, gn_b):
    feats_ctx = rwkv_features(p_ctx, mu, w0, w_up, a0, a_up, g_up, k_k, k_a)
    feats_lat = rwkv_features(p_lat, mu, w0, w_up, a0, a_up, g_up, k_k, k_a)
    zero = jnp.zeros((p_lat.shape[0], RW_HEADS, RW_HEAD, RW_HEAD), jnp.float32)
    out_ctx, states_ctx = rwkv_bidir(feats_ctx, (zero, zero), r_k, gn_g, gn_b)
    out_lat, _ = rwkv_bidir(feats_lat, states_ctx, r_k, gn_g, gn_b)
    return out_lat, out_ctx


def conformer_conv(p, w, b, g, beta):
    val, gate = jnp.split(p, 2, axis=-1)
    z = val * jax.nn.sigmoid(gate)
    z = lax.conv_general_dilated(z, w[:, None, :].astype(z.dtype), (1,), [(CONV_K // 2, CONV_K // 2)],
                                 dimension_numbers=('NWC', 'WIO', 'NWC'),
                                 feature_group_count=CONV_WIDTH) + b
    return jax.nn.silu(layer_norm(z, g, beta))


def even_mixer(u, uc, w_in, w_out, mu, w0, w_up, a0, a_up, g_up, k_k, k_a, r_k, gn_g, gn_b,
               cv_w, cv_b, cv_g, cv_beta):
    p = u @ w_in
    pc = uc @ w_in
    rw_l, rw_c = rwkv_time_mix(p[..., :RW_STREAM], pc[..., :RW_STREAM], mu, w0, w_up, a0, a_up, g_up,
                               k_k, k_a, r_k, gn_g, gn_b)
    cv_l = conformer_conv(p[..., RW_STREAM:], cv_w, cv_b, cv_g, cv_beta)
    cv_c = conformer_conv(pc[..., RW_STREAM:], cv_w, cv_b, cv_g, cv_beta)
    y = jnp.concatenate([rw_l.astype(u.dtype), cv_l.astype(u.dtype)], -1) @ w_out
    yc = jnp.concatenate([rw_c.astype(u.dtype), cv_c.astype(u.dtype)], -1) @ w_out
    return y, yc


def gqa_attend(q, k, v):
    s = jnp.einsum('bqkgd,bskd->bkgqs', q, k).astype(jnp.float32) * (1.0 / math.sqrt(ATT_HEAD))
    p = jax.nn.softmax(s, axis=-1).astype(v.dtype)
    return jnp.einsum('bkgqs,bskd->bqkgd', p, v)


def attn_mixer(u, uc, w_in, w_out, qn, kn, cos, sin, with_ctx):
    def qkv(t):
        b_, t_ = t.shape[:2]
        p = t @ w_in
        q = rms_norm(p[..., :ATT_Q].reshape(b_, t_, ATT_HEADS, ATT_HEAD), qn)
        k = rms_norm(p[..., ATT_Q:ATT_Q + ATT_KV].reshape(b_, t_, ATT_KV_HEADS, ATT_HEAD), kn)
        v = p[..., ATT_Q + ATT_KV:].reshape(b_, t_, ATT_KV_HEADS, ATT_HEAD)
        return q, k, v

    q, k, v = qkv(u)
    qc, kc, vc = qkv(uc)
    q = axial_rope(q, cos, sin)
    k = axial_rope(k, cos, sin)
    keys = jnp.concatenate([k, kc], axis=1)
    vals = jnp.concatenate([v, vc], axis=1)
    b_, s_ = u.shape[:2]
    nb = s_ // Q_BLOCK
    qb = q.reshape(b_, nb, Q_BLOCK, ATT_KV_HEADS, ATT_GROUP, ATT_HEAD).swapaxes(0, 1)
    ob = lax.map(lambda qblk: gqa_attend(qblk, keys, vals), qb)
    y = ob.swapaxes(0, 1).reshape(b_, s_, ATT_Q) @ w_out
    yc = None
    if with_ctx:
        tc = qc.shape[1]
        qcg = qc.reshape(b_, tc, ATT_KV_HEADS, ATT_GROUP, ATT_HEAD)
        yc = gqa_attend(qcg, kc, vc).reshape(b_, tc, ATT_Q) @ w_out
    return y, yc


def swiglu(x, w1, w3, w2):
    return (jax.nn.silu(x @ w1) * (x @ w3)) @ w2


def moe(xf, router, bias, w1, w3, w2, sw1, sw3, sw2):
    n_tok = xf.shape[0]
    scores = jax.nn.sigmoid((xf @ router).astype(jnp.float32))
    _, idx = lax.top_k(scores + bias, TOP_K)
    gate = jnp.take_along_axis(scores, idx, axis=1)
    gate = ROUTE_SCALE * gate / jnp.sum(gate, -1, keepdims=True)
    n_assign = n_tok * TOP_K
    e_flat = idx.reshape(-1)
    order = jnp.argsort(e_flat)
    e_sorted = e_flat[order]
    tok_sorted = (order // TOP_K).astype(jnp.int32)
    g_sorted = gate.reshape(-1)[order]
    counts = jnp.zeros((N_EXPERTS,), jnp.int32).at[e_flat].add(1)
    padded = (counts + EXPERT_BLOCK - 1) // EXPERT_BLOCK * EXPERT_BLOCK
    start = jnp.cumsum(counts) - counts
    pend = jnp.cumsum(padded)
    pstart = pend - padded
    dest = pstart[e_sorted] + (jnp.arange(n_assign, dtype=jnp.int32) - start[e_sorted])
    n_blocks = (n_assign + N_EXPERTS * (EXPERT_BLOCK - 1) + EXPERT_BLOCK - 1) // EXPERT_BLOCK
    n_rows = n_blocks * EXPERT_BLOCK
    buf_tok = jnp.zeros((n_rows,), jnp.int32).at[dest].set(tok_sorted)
    buf_g = jnp.zeros((n_rows,), jnp.float32).at[dest].set(g_sorted)
    blk_pos = jnp.arange(n_blocks, dtype=jnp.int32) * EXPERT_BLOCK
    blk_e = jnp.minimum(jnp.searchsorted(pend, blk_pos, side='right'), N_EXPERTS - 1)

    def step(acc, inp):
        tok, g_b, e = inp
        xb = xf[tok]
        yb = swiglu(xb, w1[e], w3[e], w2[e]).astype(jnp.float32)
        return acc.at[tok].add(g_b[:, None] * yb), None

    routed, _ = lax.scan(step, jnp.zeros(xf.shape, jnp.float32),
                         (buf_tok.reshape(n_blocks, EXPERT_BLOCK), buf_g.reshape(n_blocks, EXPERT_BLOCK), blk_e))
    return swiglu(xf, sw1, sw3, sw2) + routed.astype(xf.dtype)


def setup_inputs(seed: int = 0) -> dict:
    key = jax.random.key(seed)
    ks = iter(jax.random.split(key, 48))
    D = D_MODEL

    def nrm(shape, scale):
        return jax.random.normal(next(ks), shape, jnp.float32) * scale

    def unif(shape, lo, hi):
        return jax.random.uniform(next(ks), shape, jnp.float32, minval=lo, maxval=hi)

    return {
        'x': nrm((BATCH, SEQ, D), 1.0),
        'c': nrm((BATCH, D), 1.0),
        'ctx': nrm((BATCH, CTX_LEN, D), 1.0),
        'c_ctx': nrm((D,), 1.0),
        'ada_w': nrm((DEPTH, D, 6 * D), 0.5 * D ** -0.5),
        'ada_b': nrm((DEPTH, 6 * D), 0.02),
        'ln1_g': 1.0 + nrm((DEPTH, D), 0.02),
        'ln1_b': nrm((DEPTH, D), 0.02),
        'ln2_g': 1.0 + nrm((DEPTH, D), 0.02),
        'ln2_b': nrm((DEPTH, D), 0.02),
        'even_w_in': nrm((N_EVEN, D, EVEN_IN), D ** -0.5),
        'even_w_out': nrm((N_EVEN, EVEN_MIX, D), DEEPNORM_BETA * EVEN_MIX ** -0.5),
        'rw_mu': unif((N_EVEN, 2, RW_STREAM), 0.0, 0.5),
        'rw_w0': unif((N_EVEN, 2, RW_WIDTH), -6.0, -0.5),
        'rw_w_up': nrm((N_EVEN, 2, DECAY_LORA, RW_WIDTH), 0.5 * DECAY_LORA ** -0.5),
        'rw_a0': nrm((N_EVEN, 2, RW_WIDTH), 0.5),
        'rw_a_up': nrm((N_EVEN, 2, ICLR_LORA, RW_WIDTH), 0.5 * ICLR_LORA ** -0.5),
        'rw_g_up': nrm((N_EVEN, GATE_LORA, RW_WIDTH), GATE_LORA ** -0.5),
        'rw_kk': 0.85 + nrm((N_EVEN, RW_WIDTH), 0.05),
        'rw_ka': 1.0 + nrm((N_EVEN, RW_WIDTH), 0.05),
        'rw_rk': nrm((N_EVEN, RW_HEADS, RW_HEAD), 0.1),
        'rw_gn_g': 1.0 + nrm((N_EVEN, RW_WIDTH), 0.02),
        'rw_gn_b': nrm((N_EVEN, RW_WIDTH), 0.02),
        'cv_w': nrm((N_EVEN, CONV_K, CONV_WIDTH), CONV_K ** -0.5),
        'cv_b': nrm((N_EVEN, CONV_WIDTH), 0.02),
        'cv_ln_g': 1.0 + nrm((N_EVEN, CONV_WIDTH), 0.02),
        'cv_ln_b': nrm((N_EVEN, CONV_WIDTH), 0.02),
        'odd_w_in': nrm((N_ODD, D, ATT_IN), D ** -0.5),
        'odd_w_out': nrm((N_ODD, ATT_Q, D), DEEPNORM_BETA * ATT_Q ** -0.5),
        'q_norm': 1.0 + nrm((N_ODD, ATT_HEAD), 0.02),
        'k_norm': 1.0 + nrm((N_ODD, ATT_HEAD), 0.02),
        'moe_router': nrm((DEPTH, D, N_EXPERTS), D ** -0.5),
        'moe_bias': nrm((DEPTH, N_EXPERTS), 0.01),
        'moe_w1': nrm((DEPTH, N_EXPERTS, D, EXPERT_FF), D ** -0.5),
        'moe_w3': nrm((DEPTH, N_EXPERTS, D, EXPERT_FF), D ** -0.5),
        'moe_w2': nrm((DEPTH, N_EXPERTS, EXPERT_FF, D), DEEPNORM_BETA * EXPERT_FF ** -0.5),
        'sh_w1': nrm((DEPTH, D, SHARED_FF), D ** -0.5),
        'sh_w3': nrm((DEPTH, D, SHARED_FF), D ** -0.5),
        'sh_w2': nrm((DEPTH, SHARED_FF, D), DEEPNORM_BETA * SHARED_FF ** -0.5),
    }


def reference(x, c, ctx, c_ctx, ada_w, ada_b, ln1_g, ln1_b, ln2_g, ln2_b, even_w_in, even_w_out,
              rw_mu, rw_w0, rw_w_up, rw_a0, rw_a_up, rw_g_up, rw_kk, rw_ka, rw_rk, rw_gn_g, rw_gn_b,
              cv_w, cv_b, cv_ln_g, cv_ln_b, odd_w_in, odd_w_out, q_norm, k_norm,
              moe_router, moe_bias, moe_w1, moe_w3, moe_w2, sh_w1, sh_w3, sh_w2):
    bsz, s_len, d = x.shape
    n_lat = bsz * s_len
    cos, sin = axial_angles(s_len)
    silu_c = jax.nn.silu(c)
    silu_cc = jax.nn.silu(c_ctx)
    h, hc = x, ctx
    for layer in range(DEPTH):
        last = layer == DEPTH - 1
        j = layer // 2
        mod = (silu_c @ ada_w[layer] + ada_b[layer])[:, None, :]
        mod_c = silu_cc @ ada_w[layer] + ada_b[layer]
        sh_a, sc_a, g_a, sh_f, sc_f, g_f = jnp.split(mod, 6, axis=-1)
        csh_a, csc_a, cg_a, csh_f, csc_f, cg_f = jnp.split(mod_c, 6, axis=-1)
        u = h * (1.0 + sc_a) + sh_a
        uc = hc * (1.0 + csc_a) + csh_a
        if layer % 2 == 0:
            y, yc = even_mixer(u, uc, even_w_in[j], even_w_out[j], rw_mu[j], rw_w0[j], rw_w_up[j], rw_a0[j],
                               rw_a_up[j], rw_g_up[j], rw_kk[j], rw_ka[j], rw_rk[j], rw_gn_g[j], rw_gn_b[j],
                               cv_w[j], cv_b[j], cv_ln_g[j], cv_ln_b[j])
        else:
            y, yc = attn_mixer(u, uc, odd_w_in[j], odd_w_out[j], q_norm[j], k_norm[j], cos, sin,
                               with_ctx=not last)
        h = layer_norm(DEEPNORM_ALPHA * h + g_a * y, ln1_g[layer], ln1_b[layer])
        vf = h * (1.0 + sc_f) + sh_f
        if last:
            f = moe(vf.reshape(-1, d), moe_router[layer], moe_bias[layer], moe_w1[layer], moe_w3[layer],
                    moe_w2[layer], sh_w1[layer], sh_w3[layer], sh_w2[layer])
            h = layer_norm(DEEPNORM_ALPHA * h + g_f * f.reshape(h.shape), ln2_g[layer], ln2_b[layer])
        else:
            hc = layer_norm(DEEPNORM_ALPHA * hc + cg_a * yc, ln1_g[layer], ln1_b[layer])
            vfc = hc * (1.0 + csc_f) + csh_f
            tokens = jnp.concatenate([vf.reshape(-1, d), vfc.reshape(-1, d)], axis=0)
            f = moe(tokens, moe_router[layer], moe_bias[layer], moe_w1[layer], moe_w3[layer], moe_w2[layer],
                    sh_w1[layer], sh_w3[layer], sh_w2[layer])
            h = layer_norm(DEEPNORM_ALPHA * h + g_f * f[:n_lat].reshape(h.shape), ln2_g[layer], ln2_b[layer])
            hc = layer_norm(DEEPNORM_ALPHA * hc + cg_f * f[n_lat:].reshape(hc.shape), ln2_g[layer], ln2_b[layer])
    return h
```

```python
import numpy as np
import concourse.bass as bass
import concourse.mybir as mybir
from concourse.bass_utils import run_bass_kernel_spmd

F32 = mybir.dt.float32
BF16 = mybir.dt.bfloat16
I32 = mybir.dt.int32
U32 = mybir.dt.uint32
ALU = mybir.AluOpType
AF = mybir.ActivationFunctionType
AX = mybir.AxisListType

ENGS = ['pe', 'act', 'dve', 'pool', 'sp']
NSLOT = 8


class Res:
    __slots__ = ('name', 'w', 'r')

    def __init__(self, name=''):
        self.name = name
        self.w = {}
        self.r = {}


def _merge(dst, src):
    for k, v in src.items():
        if dst.get(k, 0) < v:
            dst[k] = v


class Prog:
    def __init__(self, nc):
        self.nc = nc
        self.ops = {e: [] for e in ENGS}
        self.cnt = {e: 0 for e in ENGS}
        self.known = {e: {} for e in ENGS}
        self.ndma = {e: 0 for e in ENGS}
        self.sems = {}
        self.nops = 0

    def add(self, eng, fn, reads=(), writes=(), pwrites=(), inc=True, dma=False):
        waits = {}
        for r in reads:
            _merge(waits, r.w)
        for w in writes:
            _merge(waits, w.w)
            _merge(waits, w.r)
        for w in pwrites:
            _merge(waits, w.w)
            _merge(waits, w.r)
        own = 'c_' + eng
        if own in waits and waits[own] > self.cnt[eng]:
            waits[own] = self.cnt[eng]
        if dma:
            n = self.ndma[eng]
            self.ndma[eng] = n + 1
            slot = 'd_%s_%d' % (eng, n % NSLOT)
            rnd = n // NSLOT
            if rnd > 0:
                _merge(waits, {slot: 16 * rnd})
            ev = (slot, 16 * (rnd + 1))
            incv = 16
        else:
            key = 'c_' + eng
            if inc:
                self.cnt[eng] += 1
                ev = (key, self.cnt[eng])
            else:
                ev = (key, self.cnt[eng] + 1)
            incv = 1
        kn = self.known[eng]
        wl = []
        for k, v in waits.items():
            if kn.get(k, 0) < v:
                kn[k] = v
                wl.append((k, v))
        for r in reads:
            if r.r.get(ev[0], 0) < ev[1]:
                r.r[ev[0]] = ev[1]
        for w in writes:
            w.w = {ev[0]: ev[1]}
            w.r = {}
        for w in pwrites:
            if w.w.get(ev[0], 0) < ev[1]:
                w.w[ev[0]] = ev[1]
        self.ops[eng].append((wl, fn, ev if (inc or dma) else None, incv))
        self.nops += 1

    def finish(self, reslist, eng='sp'):
        waits = {}
        for r in reslist:
            _merge(waits, r.w)
        self.ops[eng].append((list(waits.items()), None, None, 0))

    def emit(self):
        nc = self.nc
        import contextlib
        names = set()
        for e in ENGS:
            for wl, fn, ev, incv in self.ops[e]:
                for k, v in wl:
                    names.add(k)
                if ev is not None:
                    names.add(ev[0])
        with contextlib.ExitStack() as st:
            sems = {k: st.enter_context(nc.semaphore(k)) for k in sorted(names)}
            block = st.enter_context(nc.Block())

            def run(e):
                def body(eng):
                    for wl, fn, ev, incv in self.ops[e]:
                        for k, v in wl:
                            eng.wait_ge(sems[k], v)
                        if fn is not None:
                            ins = fn(eng)
                            if ev is not None:
                                ins.then_inc(sems[ev[0]], incv)
                return body
            block.tensor(run('pe'))
            block.scalar(run('act'))
            block.vector(run('dve'))
            block.gpsimd(run('pool'))
            block.sync(run('sp'))

    def barrier(self):
        tgt = {}
        for e in ENGS:
            if self.cnt[e] > 0:
                tgt['c_' + e] = self.cnt[e]
            n = self.ndma[e]
            for s in range(min(n, NSLOT)):
                last = n - 1 - ((n - 1 - s) % NSLOT)
                tgt['d_%s_%d' % (e, s)] = 16 * (last // NSLOT + 1)
        for e in ENGS:
            kn = self.known[e]
            wl = []
            for k, v in tgt.items():
                if kn.get(k, 0) < v:
                    kn[k] = v
                    wl.append((k, v))
            if wl:
                self.ops[e].append((wl, None, None, 0))


import contextlib
import math

T = 4352
NT = 34
D = 2048
TP = 4416
BLOCKS = [(512 * b, 512, 16 + 512 * b, 0) for b in range(8)] + [(4096, 256, 4144, 1)]
ALPHA = 8 ** 0.25
EGROUPS = [(i * 128, 128) for i in range(24)] + [(3072, 64), (3136, 96), (3232, 96), (3328, 96), (3424, 96)] \
    + [(3520 + i * 128, 128) for i in range(16)]
ESLABS = [list(range(4 * i, 4 * i + 4)) for i in range(6)] + [[24, 25, 26, 27, 28]] \
    + [list(range(29 + 4 * i, 33 + 4 * i)) for i in range(4)]


class Tl:
    __slots__ = ('t', 'r')

    def __init__(self, t):
        self.t = t
        self.r = Res()


class MK:
    def __init__(self, nc, dbg_out=()):
        self.nc = nc
        self.P = Prog(nc)
        self.uid = 0
        self.dbg_out = set(dbg_out)
        self.top = contextlib.ExitStack()
        self.outs = []

    def sb(self, scope, shape, dt, name='t'):
        self.uid += 1
        return Tl(scope.enter_context(self.nc.sbuf_tensor('%s_%d' % (name, self.uid), list(shape), dt)))

    def dram(self, name, shape, dt, kind=None):
        if kind is None:
            kind = "ExternalOutput" if name in self.dbg_out else "Internal"
        return self.nc.dram_tensor(name, list(shape), dt, kind=kind).ap()

    def dma(self, out, in_, reads=(), writes=(), pwrites=(), q='sp', **kw):
        self.P.add(q, lambda e: e.dma_start(out=out, in_=in_, **kw), reads=reads, writes=writes, pwrites=pwrites, dma=True)

    def mm(self, out, lhsT, rhs, start, stop, reads=(), writes=(), pwrites=(), inc=True):
        self.P.add('pe', lambda e: e.matmul(out, lhsT=lhsT, rhs=rhs, start=start, stop=stop),
                   reads=reads, writes=writes, pwrites=pwrites, inc=inc)

    def tr(self, out, in_, ident, reads=(), writes=(), pwrites=(), inc=True):
        self.P.add('pe', lambda e: e.transpose(out=out, in_=in_, identity=ident),
                   reads=reads, writes=writes, pwrites=pwrites, inc=inc)

    def act(self, out, in_, func, reads=(), writes=(), pwrites=(), **kw):
        self.P.add('act', lambda e: e.activation(out=out, in_=in_, func=func, **kw), reads=reads, writes=writes, pwrites=pwrites)

    def tt(self, eng, out, in0, in1, op, reads=(), writes=(), pwrites=()):
        self.P.add(eng, lambda e: e.tensor_tensor(out=out, in0=in0, in1=in1, op=op), reads=reads, writes=writes, pwrites=pwrites)

    def ts(self, eng, out, in0, s1, s2, op0, op1=None, reads=(), writes=(), pwrites=()):
        if op1 is None:
            self.P.add(eng, lambda e: e.tensor_scalar(out=out, in0=in0, scalar1=s1, scalar2=None, op0=op0),
                       reads=reads, writes=writes, pwrites=pwrites)
        else:
            self.P.add(eng, lambda e: e.tensor_scalar(out=out, in0=in0, scalar1=s1, scalar2=s2, op0=op0, op1=op1),
                       reads=reads, writes=writes, pwrites=pwrites)

    def stt(self, eng, out, in0, scalar, in1, op0, op1, reads=(), writes=(), pwrites=()):
        self.P.add(eng, lambda e: e.scalar_tensor_tensor(out=out, in0=in0, scalar=scalar, in1=in1, op0=op0, op1=op1),
                   reads=reads, writes=writes, pwrites=pwrites)

    def cp(self, eng, out, in_, reads=(), writes=(), pwrites=()):
        if eng == 'act':
            self.P.add(eng, lambda e: e.copy(out=out, in_=in_), reads=reads, writes=writes, pwrites=pwrites)
        else:
            self.P.add(eng, lambda e: e.tensor_copy(out=out, in_=in_), reads=reads, writes=writes, pwrites=pwrites)

    def memset(self, eng, ap, val, writes=(), pwrites=()):
        self.P.add(eng, lambda e: e.memset(ap, val), writes=writes, pwrites=pwrites)

    def recip(self, out, in_, reads=(), writes=(), pwrites=()):
        self.P.add('dve', lambda e: e.reciprocal(out=out, in_=in_), reads=reads, writes=writes, pwrites=pwrites)

    def setup(self):
        nc = self.nc
        top = self.top
        self.pb = []
        self.pp = []
        for i in range(4):
            pair = top.enter_context(nc.psum_tensor('pp%d' % i, [128, 1024], F32))
            self.pp.append(pair)
            for hh in range(2):
                self.pb.append(Tl(pair[:, hh * 512:(hh + 1) * 512]))
        self.ident32 = self.sb(top, [128, 128], F32, 'ident32')
        self.ident16 = self.sb(top, [128, 128], BF16, 'ident16')
        self.ones16 = self.sb(top, [128, 128], BF16, 'ones16')
        self.ones32 = self.sb(top, [128, 128], F32, 'ones32')
        self.bones32 = self.sb(top, [128, 128], F32, 'bones32')
        self.bones16 = self.sb(top, [128, 128], BF16, 'bones16')
        self.bind32 = self.sb(top, [128, 2], F32, 'bind32')
        i32, i16 = self.ident32, self.ident16
        self.memset('pool', i32.t[:], 1.0, writes=[i32.r])
        self.P.add('pool', lambda e: e.affine_select(out=i32.t[:], in_=i32.t[:], pattern=[[-1, 128]], compare_op=ALU.is_equal,
                                                     fill=0.0, base=0, channel_multiplier=1), reads=[i32.r], writes=[i32.r])
        self.cp('dve', i16.t[:], i32.t[:], reads=[i32.r], writes=[i16.r])
        self.memset('pool', self.ones16.t[:], 1.0, writes=[self.ones16.r])
        self.memset('pool', self.ones32.t[:], 1.0, writes=[self.ones32.r])
        b32 = self.bones32
        self.memset('pool', b32.t[:], 0.0, writes=[b32.r])
        self.memset('pool', b32.t[0:64, 0:64], 1.0, pwrites=[b32.r])
        self.memset('pool', b32.t[64:128, 64:128], 1.0, pwrites=[b32.r])
        self.cp('dve', self.bones16.t[:], b32.t[:], reads=[b32.r], writes=[self.bones16.r])
        self.eps5 = self.sb(top, [128, 1], F32, 'eps5')
        self.epsq = self.sb(top, [128, 1], F32, 'epsq')
        self.epsg = self.sb(top, [128, 1], F32, 'epsg')
        self.eps12 = self.sb(top, [128, 1], F32, 'eps12')
        self.memset('pool', self.eps5.t[:], 1e-5, writes=[self.eps5.r])
        self.memset('pool', self.epsq.t[:], 1e-6, writes=[self.epsq.r])
        self.memset('pool', self.epsg.t[:], 64e-5, writes=[self.epsg.r])
        self.memset('pool', self.eps12.t[:], 1e-12, writes=[self.eps12.r])
        bi = self.bind32
        self.memset('pool', bi.t[:], 0.0, writes=[bi.r])
        self.memset('pool', bi.t[0:64, 0:1], 1.0, pwrites=[bi.r])
        self.memset('pool', bi.t[64:128, 1:2], 1.0, pwrites=[bi.r])

    def stage_mod(self, cvec, ada_w, ada_b, MODB, layers=range(4)):
        with contextlib.ExitStack() as sc:
            cT = self.sb(sc, [128, 2, 16], F32, 'cT')
            sil = self.sb(sc, [128, 2, 16], F32, 'sil')
            silb = self.sb(sc, [128, 2, 16, 128], BF16, 'silb')
            self.dma(cT.t[:], cvec.rearrange("w (c p) -> p w c", p=128), writes=[cT.r], allow_slow_non_contiguous=True)
            self.act(sil.t[:], cT.t[:], AF.Silu, reads=[cT.r], writes=[sil.r])
            self.cp('dve', silb.t[:], sil.t[:].unsqueeze(3).to_broadcast([128, 2, 16, 128]), reads=[sil.r], writes=[silb.r])
            wsl = [self.sb(sc, [128, 16, 512], BF16, 'wsl') for _ in range(2)]
            bias = [self.sb(sc, [128, 512], F32, 'bias') for _ in range(2)]
            ot = [self.sb(sc, [128, 512], F32, 'ot') for _ in range(4)]
            it = 0
            for l in layers:
                for nb in range(24):
                    w = wsl[it % 2]
                    bt = bias[it % 2]
                    self.dma(w.t[:], ada_w[l, :, nb * 512:(nb + 1) * 512].rearrange("(c p) n -> p c n", p=128), writes=[w.r], q='pool')
                    self.dma(bt.t[:], ada_b[l, nb * 512:(nb + 1) * 512].partition_broadcast(128), writes=[bt.r])
                    for which in range(2):
                        pbk = self.pb[(it * 2 + which) % 4]
                        for kc in range(16):
                            self.mm(pbk.t[:], silb.t[:, which, kc, :], w.t[:, kc, :], kc == 0, kc == 15,
                                    reads=[silb.r, w.r], writes=[pbk.r] if kc == 0 else [], pwrites=[] if kc == 0 else [pbk.r], inc=(kc == 15))
                        o = ot[(it * 2 + which) % 4]
                        is_sc = (nb // 4) in (1, 4)
                        self.stt('dve', o.t[:], pbk.t[:], 1.0 if is_sc else 0.0, bt.t[:], ALU.add, ALU.add,
                                 reads=[pbk.r, bt.r], writes=[o.r])
                        self.dma(MODB['ap'][l, which, :, nb * 512:(nb + 1) * 512], o.t[:], reads=[o.r], pwrites=[MODB['r']])
                    it += 1
        self.P.barrier()

    def load_mods(self, sc, MODB, l, offs):
        res = {}
        for off in offs:
            t = self.sb(sc, [128, 2, 2048], F32, 'mod')
            for which in range(2):
                self.dma(t.t[:, which, :], MODB['ap'][l, which, :, off:off + 2048], reads=[MODB['r']], pwrites=[t.r])
            res[off] = t
        return res

    def load_bcast(self, sc, vec_ap, n, name='bc'):
        t = self.sb(sc, [128, n], F32, name)
        self.dma(t.t[:], vec_ap.partition_broadcast(128), writes=[t.r])
        return t

    def make_modT(self, sc_bufs, H, blk, mods_sc, mods_sh, uT, tile_off=0, also32=None):
        tok0, ntok, col0, which = blk
        for i in range(ntok // 128):
            ti = tok0 // 128 + i
            k = sc_bufs['n']
            sc_bufs['n'] += 1
            hb = sc_bufs['hb'][k % 2]
            tmp = sc_bufs['tmp'][k % 2]
            ub = sc_bufs['ub'][k % 2]
            self.dma(hb.t[:], H['ap'][ti * 128:(ti + 1) * 128, :], reads=[H['r'][ti]], writes=[hb.r])
            self.tt('dve', tmp.t[:], hb.t[:], mods_sc.t[:, which, :], ALU.mult, reads=[hb.r, mods_sc.r], writes=[tmp.r])
            self.tt('pool', ub.t[:], tmp.t[:], mods_sh.t[:, which, :], ALU.add, reads=[tmp.r, mods_sh.r], writes=[ub.r])
            if also32 is not None:
                also32(i, ti, tmp, mods_sh, which)
            for half in range(2):
                pbk = self.pb[sc_bufs['pbase'] + half]
                pv = pbk.t[:].bitcast(BF16)
                for q in range(8):
                    kc = half * 8 + q
                    self.tr(pv[:, q * 128:(q + 1) * 128], ub.t[:, kc * 128:(kc + 1) * 128], self.ident16.t[:],
                            reads=[ub.r, self.ident16.r], writes=[pbk.r] if q == 0 else [], pwrites=[] if q == 0 else [pbk.r], inc=(q == 7))
                c0 = tile_off + i * 128
                self.cp('act', uT.t[:, half * 8:(half + 1) * 8, c0:c0 + 128], pv.rearrange("p (q k) -> p q k", q=8),
                        reads=[pbk.r], pwrites=[uT.r])

    def modT_bufs(self, sc, pbase=6):
        return {'n': 0, 'pbase': pbase,
                'hb': [self.sb(sc, [128, 2048], F32, 'hb') for _ in range(2)],
                'tmp': [self.sb(sc, [128, 2048], F32, 'tmp') for _ in range(2)],
                'ub': [self.sb(sc, [128, 2048], BF16, 'ub') for _ in range(2)]}

    def gemm_tok(self, sc, xT, ntok, W_ap, N, OUT, tok0, wsl, stg, cnt, pbanks=(0, 1, 2, 3)):
        for nb in range((N + 511) // 512):
            n0 = nb * 512
            nn = min(512, N - n0)
            w = wsl[cnt[0] % 2]
            self.dma(w.t[:, :, 0:nn], W_ap[:, n0:n0 + nn].rearrange("(c p) n -> p c n", p=128), writes=[w.r], q='pool')
            for i in range(ntok // 128):
                pbk = self.pb[pbanks[cnt[1] % len(pbanks)]]
                for kc in range(16):
                    self.mm(pbk.t[:, 0:nn], xT.t[:, kc, i * 128:(i + 1) * 128], w.t[:, kc, 0:nn], kc == 0, kc == 15,
                            reads=[xT.r, w.r], writes=[pbk.r] if kc == 0 else [], pwrites=[] if kc == 0 else [pbk.r], inc=(kc == 15))
                s = stg[cnt[1] % len(stg)]
                eng = 'act' if cnt[1] % 2 == 0 else 'dve'
                self.cp(eng, s.t[:, 0:nn], pbk.t[:, 0:nn], reads=[pbk.r], writes=[s.r])
                ti = tok0 // 128 + i
                self.dma(OUT['ap'][ti * 128:(ti + 1) * 128, n0:n0 + nn], s.t[:, 0:nn], reads=[s.r], pwrites=[OUT['r'][ti]])
                cnt[1] += 1
            cnt[0] += 1

    def stage_ln(self, H, Y, MODB, l, gate_off, g_ap, b_ap, tiles, OUTF=None):
        with contextlib.ExitStack() as sc:
            gate = self.load_mods(sc, MODB, l, [gate_off])[gate_off]
            gt = self.load_bcast(sc, g_ap, 2048, 'lng')
            bt = self.load_bcast(sc, b_ap, 2048, 'lnb')
            hb = [self.sb(sc, [128, 2048], F32, 'hb') for _ in range(2)]
            yb = [self.sb(sc, [128, 2048], F32, 'yb') for _ in range(2)]
            t1 = [self.sb(sc, [128, 2048], F32, 't1') for _ in range(2)]
            sq = self.sb(sc, [128, 2048], F32, 'sq')
            st = [self.sb(sc, [128, 8], F32, 'st') for _ in range(2)]
            for n, ti in enumerate(tiles):
                which = 0 if ti < 32 else 1
                h, y, t, s = hb[n % 2], yb[n % 2], t1[n % 2], st[n % 2]
                rows = slice(ti * 128, (ti + 1) * 128)
                self.dma(h.t[:], H['ap'][rows, :], reads=[H['r'][ti]], writes=[h.r])
                self.dma(y.t[:], Y['ap'][rows, :], reads=[Y['r'][ti]], writes=[y.r])
                self.tt('pool', y.t[:], y.t[:], gate.t[:, which, :], ALU.mult, reads=[y.r, gate.r], writes=[y.r])
                self.stt('dve', t.t[:], h.t[:], ALPHA, y.t[:], ALU.mult, ALU.add, reads=[h.r, y.r], writes=[t.r])
                self.P.add('dve', lambda e, t=t, s=s: e.reduce_sum(out=s.t[:, 0:1], in_=t.t[:], axis=AX.X), reads=[t.r], writes=[s.r])
                self.ts('dve', s.t[:, 1:2], s.t[:, 0:1], -1.0 / 2048, None, ALU.mult, reads=[s.r], writes=[s.r])
                self.act(sq.t[:], t.t[:], AF.Square, reads=[t.r, s.r], writes=[sq.r], pwrites=[s.r], bias=s.t[:, 1:2], scale=1.0, accum_out=s.t[:, 2:3])
                self.act(s.t[:, 3:4], s.t[:, 2:3], AF.Sqrt, reads=[s.r], writes=[s.r], bias=self.eps5.t[:, 0:1], scale=1.0 / 2048)
                self.recip(s.t[:, 4:5], s.t[:, 3:4], reads=[s.r], writes=[s.r])
                self.tt('dve', s.t[:, 5:6], s.t[:, 1:2], s.t[:, 4:5], ALU.mult, reads=[s.r], writes=[s.r])
                self.act(t.t[:], t.t[:], AF.Identity, reads=[t.r, s.r], writes=[t.r], bias=s.t[:, 5:6], scale=s.t[:, 4:5])
                self.tt('pool', t.t[:], t.t[:], gt.t[:], ALU.mult, reads=[t.r, gt.r], writes=[t.r])
                self.tt('dve', t.t[:], t.t[:], bt.t[:], ALU.add, reads=[t.r, bt.r], writes=[t.r])
                self.dma(H['ap'][rows, :], t.t[:], reads=[t.r], writes=[H['r'][ti]])
                if OUTF is not None and ti < 32:
                    self.dma(OUTF['ap'][rows, :], t.t[:], reads=[t.r], writes=[OUTF['r'][ti]])
        self.P.barrier()

    def stage_moe(self, H, F, MODB, l, router, rbias, w1, w3, w2, sw1, sw3, sw2, with_ctx=True, n_exp=64):
        sblocks = [(1024 * b, 1024, 0) for b in range(4)] + ([(4096, 256, 1)] if with_ctx else [])
        with contextlib.ExitStack() as sc0:
            vT = self.sb(sc0, [128, 16, 1024], BF16, 'vT')
            yacc = self.sb(sc0, [128, 8, 2048], F32, 'yacc')
            Gs = self.sb(sc0, [128, 8, 66], F32, 'Gs')
            rt32 = self.sb(sc0, [128, 16, 64], F32, 'rt32')
            rb = self.load_bcast(sc0, rbias, 64, 'rb')
            self.dma(rt32.t[:], router.rearrange("(c p) e -> p c e", p=128), writes=[rt32.r])
            for (tok0, ntok, which) in sblocks:
                ntile = ntok // 128
                with contextlib.ExitStack() as sc:
                    msc = self.sb(sc, [128, 1, 2048], F32, 'msc')
                    msh = self.sb(sc, [128, 1, 2048], F32, 'msh')
                    self.dma(msc.t[:, 0, :], MODB['ap'][l, which, :, 8192:10240], reads=[MODB['r']], writes=[msc.r])
                    self.dma(msh.t[:, 0, :], MODB['ap'][l, which, :, 6144:8192], reads=[MODB['r']], writes=[msh.r])
                    bufs = self.modT_bufs(sc, pbase=6)
                    vf32 = self.sb(sc, [128, 2048], F32, 'vf32')
                    vT32 = self.sb(sc, [128, 16, 128], F32, 'vT32')
                    g1 = self.sb(sc, [128, 64], F32, 'g1')
                    g2 = self.sb(sc, [128, 64], F32, 'g2')
                    g3 = self.sb(sc, [128, 16], F32, 'g3')
                    self.memset('pool', Gs.t[:, :, 64:65], 1.0, pwrites=[Gs.r])

                    def gates(i, ti, tmp, mods_sh, w_):
                        self.tt('dve', vf32.t[:], tmp.t[:], mods_sh.t[:, 0, :], ALU.add, reads=[tmp.r, mods_sh.r], writes=[vf32.r])
                        for q4 in range(4):
                            pbk = self.pb[q4 % 2]
                            for q in range(4):
                                kc = q4 * 4 + q
                                self.tr(pbk.t[:, q * 128:(q + 1) * 128], vf32.t[:, kc * 128:(kc + 1) * 128], self.ident32.t[:],
                                        reads=[vf32.r, self.ident32.r], writes=[pbk.r] if q == 0 else [], pwrites=[] if q == 0 else [pbk.r], inc=(q == 3))
                            self.cp('dve', vT32.t[:, q4 * 4:(q4 + 1) * 4, :], pbk.t[:].rearrange("p (q k) -> p q k", q=4),
                                    reads=[pbk.r], pwrites=[vT32.r])
                        pr = self.pb[2]
                        for kc in range(16):
                            self.mm(pr.t[:, 0:64], vT32.t[:, kc, :], rt32.t[:, kc, :], kc == 0, kc == 15,
                                    reads=[vT32.r, rt32.r], writes=[pr.r] if kc == 0 else [], pwrites=[] if kc == 0 else [pr.r], inc=(kc == 15))
                        self.act(g1.t[:], pr.t[:, 0:64], AF.Sigmoid, reads=[pr.r], writes=[g1.r])
                        self.tt('dve', g2.t[:], g1.t[:], rb.t[:], ALU.add, reads=[g1.r, rb.r], writes=[g2.r])
                        self.P.add('dve', lambda e: e.max(out=g3.t[:, 0:8], in_=g2.t[:]), reads=[g2.r], writes=[g3.r])
                        self.ts('dve', g2.t[:], g2.t[:], g3.t[:, 5:6], None, ALU.is_ge, reads=[g2.r, g3.r], writes=[g2.r])
                        self.tt('dve', g1.t[:], g1.t[:], g2.t[:], ALU.mult, reads=[g1.r, g2.r], writes=[g1.r])
                        self.P.add('dve', lambda e: e.reduce_sum(out=g3.t[:, 8:9], in_=g1.t[:], axis=AX.X), reads=[g1.r], writes=[g3.r])
                        self.recip(g3.t[:, 9:10], g3.t[:, 8:9], reads=[g3.r], writes=[g3.r])
                        self.ts('dve', Gs.t[:, i, 0:64], g1.t[:], g3.t[:, 9:10], 2.5, ALU.mult, ALU.mult, reads=[g1.r, g3.r], pwrites=[Gs.r])
                    self.make_modT(bufs, H, (tok0, ntok, 0, 0), msc, msh, vT, also32=gates)
                self.P.barrier()
                with contextlib.ExitStack() as sc:
                    W13 = [self.sb(sc, [128, 16, 768], BF16, 'W13') for _ in range(2)]
                    W2 = [self.sb(sc, [128, 3, 2048], BF16, 'W2') for _ in range(2)]
                    aT = [self.sb(sc, [128, 3, 512], BF16, 'aT') for _ in range(2)]
                    silt = [self.sb(sc, [128, 512], F32, 'silt') for _ in range(2)]
                    cA = 0
                    cY = 0
                    cS = 0
                    for e in range(n_exp + 1):
                        if e < n_exp:
                            a1, a3, a2 = w1[l, e], w3[l, e], w2[l, e]
                        else:
                            a1, a3, a2 = sw1[l], sw3[l], sw2[l]
                        gcol = e if e < n_exp else 64
                        wa = W13[e % 2]
                        wb = W2[e % 2]
                        self.dma(wa.t[:, :, 0:384], a1.rearrange("(c p) f -> p c f", p=128), pwrites=[wa.r], q='pool')
                        self.dma(wa.t[:, :, 384:768], a3.rearrange("(c p) f -> p c f", p=128), pwrites=[wa.r], q='pool')
                        self.dma(wb.t[:], a2.rearrange("(c p) n -> p c n", p=128), writes=[wb.r], q='pool')
                        for sub in range((ntok + 511) // 512):
                            ns = min(512, ntok - sub * 512)
                            at = aT[cS % 2]
                            cS += 1
                            for fc in range(3):
                                A = self.pb[(cA * 2) % 4]
                                B = self.pb[(cA * 2 + 1) % 4]
                                sl = silt[cA % 2]
                                cA += 1
                                for kc in range(16):
                                    self.mm(A.t[:, 0:ns], wa.t[:, kc, fc * 128:(fc + 1) * 128], vT.t[:, kc, sub * 512:sub * 512 + ns], kc == 0, kc == 15,
                                            reads=[wa.r, vT.r], writes=[A.r] if kc == 0 else [], pwrites=[] if kc == 0 else [A.r], inc=(kc == 15))
                                for kc in range(16):
                                    self.mm(B.t[:, 0:ns], wa.t[:, kc, 384 + fc * 128:384 + (fc + 1) * 128], vT.t[:, kc, sub * 512:sub * 512 + ns], kc == 0, kc == 15,
                                            reads=[wa.r, vT.r], writes=[B.r] if kc == 0 else [], pwrites=[] if kc == 0 else [B.r], inc=(kc == 15))
                                self.act(sl.t[:, 0:ns], A.t[:, 0:ns], AF.Silu, reads=[A.r], writes=[sl.r])
                                self.tt('dve', at.t[:, fc, 0:ns], sl.t[:, 0:ns], B.t[:, 0:ns], ALU.mult, reads=[sl.r, B.r], pwrites=[at.r])
                            for i in range(ns // 128):
                                tl = sub * 4 + i
                                for cb in range(4):
                                    Yp = self.pb[4 + cY % 4]
                                    cY += 1
                                    for fc in range(3):
                                        self.mm(Yp.t[:], at.t[:, fc, i * 128:(i + 1) * 128], wb.t[:, fc, cb * 512:(cb + 1) * 512], fc == 0, fc == 2,
                                                reads=[at.r, wb.r], writes=[Yp.r] if fc == 0 else [], pwrites=[] if fc == 0 else [Yp.r], inc=(fc == 2))
                                    ya = yacc.t[:, tl, cb * 512:(cb + 1) * 512]
                                    if e == 0:
                                        self.ts('dve', ya, Yp.t[:], Gs.t[:, tl, gcol:gcol + 1], None, ALU.mult, reads=[Yp.r, Gs.r], pwrites=[yacc.r])
                                    else:
                                        self.stt('dve', ya, Yp.t[:], Gs.t[:, tl, gcol:gcol + 1], ya, ALU.mult, ALU.add, reads=[Yp.r, Gs.r], pwrites=[yacc.r])
                    for i in range(ntile):
                        ti = tok0 // 128 + i
                        self.dma(F['ap'][ti * 128:(ti + 1) * 128, :], yacc.t[:, i, :], reads=[yacc.r], writes=[F['r'][ti]])
                self.P.barrier()

    def stage_qkv(self, H, MODB, l, w_in, qn_ap, kn_ap, cosT, sinT, rotT_ap, QT, V):
        with contextlib.ExitStack() as sc:
            mods = self.load_mods(sc, MODB, l, [2048, 0])
            msc, msh = mods[2048], mods[0]
            bufs = self.modT_bufs(sc, pbase=6)
            uT = self.sb(sc, [128, 16, 512], BF16, 'uT')
            wsl = [self.sb(sc, [128, 16, 512], BF16, 'wsl') for _ in range(2)]
            rotT = self.sb(sc, [128, 128], F32, 'rotT')
            gn = self.sb(sc, [128, 2], F32, 'gn')
            self.dma(rotT.t[:], rotT_ap, writes=[rotT.r])
            self.dma(gn.t[:, 0:1], qn_ap.rearrange("(p o) -> p o", o=1), pwrites=[gn.r])
            self.dma(gn.t[:, 1:2], kn_ap.rearrange("(p o) -> p o", o=1), pwrites=[gn.r])
            ct = self.sb(sc, [128, 512], F32, 'ct')
            stt_ = self.sb(sc, [128, 512], F32, 'st')
            sq = [self.sb(sc, [128, 512], F32, 'sq') for _ in range(2)]
            rn = [self.sb(sc, [128, 512], F32, 'rn') for _ in range(2)]
            qn = [self.sb(sc, [128, 512], F32, 'qn') for _ in range(2)]
            t1 = [self.sb(sc, [128, 512], F32, 't1') for _ in range(2)]
            qo = [self.sb(sc, [128, 512], BF16, 'qo') for _ in range(2)]
            vs = [self.sb(sc, [128, 512], BF16, 'vs') for _ in range(2)]
            cw = 0
            ch = 0
            for blk in BLOCKS:
                tok0, n, col0, which = blk
                self.make_modT(bufs, H, blk, msc, msh, uT)
                if not which:
                    self.dma(ct.t[:, 0:n], cosT[:, tok0:tok0 + n], writes=[ct.r])
                    self.dma(stt_.t[:, 0:n], sinT[:, tok0:tok0 + n], writes=[stt_.r])
                for s in range(6):
                    w = wsl[cw % 2]
                    cw += 1
                    self.dma(w.t[:], w_in[:, s * 512:(s + 1) * 512].rearrange("(c p) n -> p c n", p=128), writes=[w.r], q='pool')
                    if s < 5:
                        for hh in range(4):
                            head = s * 4 + hh
                            gcol = 0 if head < 16 else 1
                            k = ch % 2
                            ch += 1
                            X = self.pb[k]
                            Sx = self.pb[2 + k]
                            R = self.pb[4 + k]
                            for kc in range(16):
                                self.mm(X.t[:, 0:n], w.t[:, kc, hh * 128:(hh + 1) * 128], uT.t[:, kc, 0:n], kc == 0, kc == 15,
                                        reads=[w.r, uT.r], writes=[X.r] if kc == 0 else [], pwrites=[] if kc == 0 else [X.r], inc=(kc == 15))
                            self.act(sq[k].t[:, 0:n], X.t[:, 0:n], AF.Square, reads=[X.r], writes=[sq[k].r])
                            self.mm(Sx.t[:, 0:n], self.ones32.t[:], sq[k].t[:, 0:n], True, True, reads=[self.ones32.r, sq[k].r], writes=[Sx.r])
                            self.act(rn[k].t[:, 0:n], Sx.t[:, 0:n], AF.Sqrt, reads=[Sx.r], writes=[rn[k].r], bias=self.epsq.t[:, 0:1], scale=1.0 / 128)
                            self.recip(rn[k].t[:, 0:n], rn[k].t[:, 0:n], reads=[rn[k].r], writes=[rn[k].r])
                            self.stt('dve', qn[k].t[:, 0:n], X.t[:, 0:n], gn.t[:, gcol:gcol + 1], rn[k].t[:, 0:n], ALU.mult, ALU.mult,
                                     reads=[X.r, gn.r, rn[k].r], writes=[qn[k].r])
                            if not which:
                                self.mm(R.t[:, 0:n], rotT.t[:], qn[k].t[:, 0:n], True, True, reads=[rotT.r, qn[k].r], writes=[R.r])
                                self.tt('pool', t1[k].t[:, 0:n], qn[k].t[:, 0:n], ct.t[:, 0:n], ALU.mult, reads=[qn[k].r, ct.r], writes=[t1[k].r])
                                self.tt('dve', qn[k].t[:, 0:n], R.t[:, 0:n], stt_.t[:, 0:n], ALU.mult, reads=[R.r, stt_.r, qn[k].r], writes=[qn[k].r])
                                self.tt('dve', qo[k].t[:, 0:n], t1[k].t[:, 0:n], qn[k].t[:, 0:n], ALU.add, reads=[t1[k].r, qn[k].r], writes=[qo[k].r])
                            else:
                                self.cp('dve', qo[k].t[:, 0:n], qn[k].t[:, 0:n], reads=[qn[k].r], writes=[qo[k].r])
                            self.dma(QT['ap'][head, :, tok0:tok0 + n], qo[k].t[:, 0:n], reads=[qo[k].r], pwrites=[QT['r']])
                    else:
                        for i in range(n // 128):
                            k = ch % 2
                            ch += 1
                            X = self.pb[k]
                            for kc in range(16):
                                self.mm(X.t[:], uT.t[:, kc, i * 128:(i + 1) * 128], w.t[:, kc, :], kc == 0, kc == 15,
                                        reads=[w.r, uT.r], writes=[X.r] if kc == 0 else [], pwrites=[] if kc == 0 else [X.r], inc=(kc == 15))
                            self.cp('act', vs[k].t[:], X.t[:], reads=[X.r], writes=[vs[k].r])
                            ti = tok0 // 128 + i
                            self.dma(V['ap'][ti * 128:(ti + 1) * 128, :], vs[k].t[:], reads=[vs[k].r], pwrites=[V['r']])
        self.P.barrier()

    def stage_attn(self, QT, V, OT, with_ctx=True):
        scale = 1.0 / math.sqrt(128.0)
        with contextlib.ExitStack() as sc:
            KT = self.sb(sc, [128, T], BF16, 'KT')
            Vg = self.sb(sc, [128, NT, 128], BF16, 'Vg')
            Qb = [self.sb(sc, [128, 512], BF16, 'Qb') for _ in range(2)]
            Pb = [self.sb(sc, [128, 512], BF16, 'Pb') for _ in range(3)]
            rec = self.sb(sc, [128, 512], F32, 'rec')
            ot = [self.sb(sc, [128, 512], BF16, 'ot') for _ in range(2)]
            cq = 0
            cs = 0
            for g in range(4):
                self.dma(KT.t[:], QT['ap'][16 + g], reads=[QT['r']], writes=[KT.r])
                self.dma(Vg.t[:], V['ap'][:, g * 128:(g + 1) * 128].rearrange("(t p) d -> p t d", p=128), reads=[V['r']], writes=[Vg.r])
                for hq in range(4):
                    h = g * 4 + hq
                    for blk in BLOCKS:
                        tok0, n, col0, which = blk
                        if which and not with_ctx:
                            continue
                        keys = [32, 33] if which else list(range(NT))
                        q = Qb[cq % 2]
                        O = self.pb[4 + cq % 2]
                        Dn = self.pb[6 + cq % 2]
                        o = ot[cq % 2]
                        cq += 1
                        self.dma(q.t[:, 0:n], QT['ap'][h, :, tok0:tok0 + n], reads=[QT['r']], writes=[q.r])
                        LAG = 2
                        plist = []
                        for idx in range(len(keys) + LAG):
                            if idx < len(keys):
                                kt = keys[idx]
                                Sb = self.pb[cs % 4]
                                p = Pb[cs % 3]
                                cs += 1
                                plist.append(p)
                                self.mm(Sb.t[:, 0:n], KT.t[:, kt * 128:(kt + 1) * 128], q.t[:, 0:n], True, True, reads=[KT.r, q.r], writes=[Sb.r])
                                self.act(p.t[:, 0:n], Sb.t[:, 0:n], AF.Exp, reads=[Sb.r], writes=[p.r], scale=scale)
                            j2 = idx - LAG
                            if j2 >= 0:
                                kt2 = keys[j2]
                                p2 = plist[j2]
                                first, last = j2 == 0, j2 == len(keys) - 1
                                self.mm(O.t[:, 0:n], Vg.t[:, kt2, :], p2.t[:, 0:n], first, last, reads=[Vg.r, p2.r],
                                        writes=[O.r] if first else [], pwrites=[] if first else [O.r], inc=False)
                                self.mm(Dn.t[:, 0:n], self.ones16.t[:], p2.t[:, 0:n], first, last, reads=[self.ones16.r, p2.r],
                                        writes=[Dn.r] if first else [], pwrites=[] if first else [Dn.r], inc=True)
                        self.recip(rec.t[:, 0:n], Dn.t[:, 0:n], reads=[Dn.r], writes=[rec.r])
                        self.tt('dve', o.t[:, 0:n], O.t[:, 0:n], rec.t[:, 0:n], ALU.mult, reads=[O.r, rec.r], writes=[o.r])
                        self.dma(OT['ap'][h, :, tok0:tok0 + n], o.t[:, 0:n], reads=[o.r], pwrites=[OT['r']])
        self.P.barrier()

    def stage_proj_T(self, XT, W_ap, Y, with_ctx=True):
        with contextlib.ExitStack() as sc:
            xT = [self.sb(sc, [128, 16, 512], BF16, 'xT') for _ in range(2)]
            wsl = [self.sb(sc, [128, 16, 512], BF16, 'wsl') for _ in range(2)]
            stg = [self.sb(sc, [128, 512], F32, 'stg') for _ in range(4)]
            cnt = [0, 0]
            for bi, blk in enumerate(BLOCKS):
                tok0, n, col0, which = blk
                if which and not with_ctx:
                    continue
                x = xT[bi % 2]
                self.dma(x.t[:, :, 0:n], XT['ap'][:, :, tok0:tok0 + n].rearrange("h p t -> p h t"), reads=[XT['r']], writes=[x.r])
                self.gemm_tok(sc, x, n, W_ap, 2048, Y, tok0, wsl, stg, cnt)
        self.P.barrier()

    def stage_zpad(self, PT):
        with contextlib.ExitStack() as sc:
            z = self.sb(sc, [128, 45, 32], F32, 'zpad')
            self.memset('pool', z.t[:], 0.0, writes=[z.r])
            pt = PT['ap']
            for (c0, w) in ((0, 16), (4112, 32), (4400, 16)):
                self.dma(pt[:, :, c0:c0 + w].rearrange("g p c -> p g c"), z.t[:, :, 0:w], reads=[z.r], pwrites=[PT['r']])
        self.P.barrier()

    def stage_ein(self, H, MODB, l, w_in, PT):
        with contextlib.ExitStack() as sc:
            mods = self.load_mods(sc, MODB, l, [2048, 0])
            msc, msh = mods[2048], mods[0]
            bufs = self.modT_bufs(sc, pbase=6)
            uT = self.sb(sc, [128, 16, 512], BF16, 'uT')
            wsl = [self.sb(sc, [128, 16, 512], BF16, 'wsl') for _ in range(2)]
            stg = [self.sb(sc, [128, 512], F32, 'stg') for _ in range(4)]
            cw = 0
            cg = 0
            for blk in BLOCKS:
                tok0, n, col0, which = blk
                self.make_modT(bufs, H, blk, msc, msh, uT)
                for slab in ESLABS:
                    c0 = EGROUPS[slab[0]][0]
                    c1 = EGROUPS[slab[-1]][0] + EGROUPS[slab[-1]][1]
                    w = wsl[cw % 2]
                    cw += 1
                    self.dma(w.t[:, :, 0:c1 - c0], w_in[:, c0:c1].rearrange("(c p) n -> p c n", p=128), writes=[w.r], q='pool')
                    for g in slab:
                        gc0, M = EGROUPS[g]
                        X = self.pb[cg % 4]
                        s = stg[cg % 4]
                        for kc in range(16):
                            self.mm(X.t[0:M, 0:n], w.t[:, kc, gc0 - c0:gc0 - c0 + M], uT.t[:, kc, 0:n], kc == 0, kc == 15,
                                    reads=[w.r, uT.r], writes=[X.r] if kc == 0 else [], pwrites=[] if kc == 0 else [X.r], inc=(kc == 15))
                        self.cp('act' if cg % 2 == 0 else 'dve', s.t[0:M, 0:n], X.t[0:M, 0:n], reads=[X.r], writes=[s.r])
                        self.dma(PT['ap'][g, 0:M, col0:col0 + n], s.t[0:M, 0:n], reads=[s.r], pwrites=[PT['r']])
                        cg += 1
        self.P.barrier()

    def stage_efeat(self, j, W, PT, SCN, VH, V32, G, RK):
        with contextlib.ExitStack() as sc:
            MU = self.sb(sc, [128, 29, 3], F32, 'MU')
            W0 = self.sb(sc, [128, 2, 8], F32, 'W0')
            A0 = self.sb(sc, [128, 2, 8], F32, 'A0')
            KKp = self.sb(sc, [128, 8], F32, 'KKp')
            KAp = self.sb(sc, [128, 8], F32, 'KAp')
            RKp = self.sb(sc, [128, 8], F32, 'RKp')
            wup = self.sb(sc, [96, 2, 1024], BF16, 'wup')
            aup = self.sb(sc, [96, 2, 1024], BF16, 'aup')
            gup = self.sb(sc, [64, 1024], BF16, 'gup')
            self.memset('pool', MU.t[:], 0.0, writes=[MU.r])
            for d in range(2):
                self.dma(MU.t[:, 0:24, d], W['mu'][d, 0:3072].rearrange("(g p) -> p g", p=128), pwrites=[MU.r], allow_slow_non_contiguous=True)
                for g in range(24, 29):
                    gc0, M = EGROUPS[g]
                    self.dma(MU.t[0:M, g, d:d + 1], W['mu'][d, gc0:gc0 + M].rearrange("(p o) -> p o", o=1), pwrites=[MU.r], allow_slow_non_contiguous=True)
                self.dma(W0.t[:, d, :], W['w0'][d].rearrange("(g p) -> p g", p=128), pwrites=[W0.r], allow_slow_non_contiguous=True)
                self.dma(A0.t[:, d, :], W['a0'][d].rearrange("(g p) -> p g", p=128), pwrites=[A0.r], allow_slow_non_contiguous=True)
                self.dma(wup.t[:, d, :], W['w_up'][d], pwrites=[wup.r], q='pool')
                self.dma(aup.t[:, d, :], W['a_up'][d], pwrites=[aup.r], q='pool')
            self.dma(gup.t[:], W['g_up'], writes=[gup.r], q='pool')
            self.dma(KKp.t[:], W['kk'].rearrange("(g p) -> p g", p=128), writes=[KKp.r], allow_slow_non_contiguous=True)
            self.dma(KAp.t[:], W['ka'].rearrange("(g p) -> p g", p=128), writes=[KAp.r], allow_slow_non_contiguous=True)
            self.dma(RKp.t[:], W['rk'].rearrange("h k -> (h k)").rearrange("(g p) -> p g", p=128), writes=[RKp.r], allow_slow_non_contiguous=True)
            self.tt('dve', MU.t[:, :, 2], MU.t[:, :, 0], MU.t[:, :, 1], ALU.add, reads=[MU.r], writes=[MU.r])
            self.ts('dve', MU.t[:, :, 2], MU.t[:, :, 2], -1.0, 1.0, ALU.mult, ALU.add, reads=[MU.r], writes=[MU.r])

            Xb = [self.sb(sc, [128, 514], F32, 'Xb') for _ in range(4)]
            mt = [self.sb(sc, [128, 512], F32, 'mt') for _ in range(2)]
            sg = self.sb(sc, [64, 512], BF16, 'sg')
            sgf = self.sb(sc, [64, 512], F32, 'sgf')
            twd = [self.sb(sc, [96, 512], BF16, 'twd') for _ in range(2)]
            tad = [self.sb(sc, [96, 512], BF16, 'tad') for _ in range(2)]
            tf = self.sb(sc, [96, 512], F32, 'tf')
            rT = [self.sb(sc, [128, 512], F32, 'rT') for _ in range(2)]
            kT = [self.sb(sc, [128, 512], F32, 'kT') for _ in range(2)]
            vT = [self.sb(sc, [128, 512], F32, 'vT') for _ in range(2)]
            kkr = [self.sb(sc, [128, 512], F32, 'kkr') for _ in range(2)]
            sq = self.sb(sc, [128, 512], F32, 'sq')
            rn = self.sb(sc, [128, 512], F32, 'rn')
            kk = [self.sb(sc, [128, 512], F32, 'kk') for _ in range(2)]
            sig = [self.sb(sc, [128, 512], F32, 'sig') for _ in range(2)]
            wdec = [self.sb(sc, [128, 512], F32, 'wdec') for _ in range(2)]
            aa = [self.sb(sc, [128, 512], F32, 'aa') for _ in range(2)]
            tm = [self.sb(sc, [128, 512], F32, 'tm') for _ in range(2)]
            kd = [self.sb(sc, [128, 512], F32, 'kd') for _ in range(2)]
            bd = [self.sb(sc, [128, 512], F32, 'bd') for _ in range(2)]
            prod = [self.sb(sc, [128, 512], F32, 'prod') for _ in range(2)]
            vtok = self.sb(sc, [128, 4, 1024], F32, 'vtok')
            vb16 = self.sb(sc, [128, 4, 1024], BF16, 'vb16')
            gt = [self.sb(sc, [128, 1024], F32, 'gt') for _ in range(2)]
            rkt = self.sb(sc, [128, 4, 32], F32, 'rkt')
            Sx, LW, LA, PRK, PV, PG0, PG1 = (self.pb[i] for i in range(7))
            cx = [0]

            def mix(g, M, col0, n, out_ap, out_res, pw=False):
                X = Xb[cx[0] % 4]
                t = mt[cx[0] % 2]
                cx[0] += 1
                self.dma(X.t[0:M, 0:n + 2], PT['ap'][g, 0:M, col0 - 1:col0 + n + 1], reads=[PT['r']], writes=[X.r])
                self.ts('dve', t.t[0:M, 0:n], X.t[0:M, 1:n + 1], MU.t[0:M, g, 2:3], None, ALU.mult, reads=[X.r, MU.r], writes=[t.r])
                self.stt('dve', t.t[0:M, 0:n], X.t[0:M, 0:n], MU.t[0:M, g, 0:1], t.t[0:M, 0:n], ALU.mult, ALU.add, reads=[X.r, MU.r, t.r], writes=[t.r])
                kw = dict(pwrites=[out_res]) if pw else dict(writes=[out_res])
                self.stt('dve', out_ap, X.t[0:M, 2:n + 2], MU.t[0:M, g, 1:2], t.t[0:M, 0:n], ALU.mult, ALU.add, reads=[X.r, MU.r, t.r], **kw)

            cj = 0
            for blk in BLOCKS:
                tok0, n, col0, which = blk
                nt = n // 128
                mix(24, 64, col0, n, sgf.t[:, 0:n], sgf.r)
                self.act(sg.t[:, 0:n], sgf.t[:, 0:n], AF.Sigmoid, reads=[sgf.r], writes=[sg.r])
                for d in range(2):
                    mix(25 + d, 96, col0, n, tf.t[:, 0:n], tf.r)
                    self.act(twd[d].t[:, 0:n], tf.t[:, 0:n], AF.Tanh, reads=[tf.r], writes=[twd[d].r])
                    mix(27 + d, 96, col0, n, tad[d].t[:, 0:n], tad[d].r)
                for i in range(nt):
                    for hf, PGx in enumerate((PG0, PG1)):
                        self.mm(PGx.t[:], sg.t[:, i * 128:(i + 1) * 128], gup.t[:, hf * 512:(hf + 1) * 512], True, True, reads=[sg.r, gup.r], writes=[PGx.r])
                    g_ = gt[i % 2]
                    self.cp('act', g_.t[:, 0:512], PG0.t[:], reads=[PG0.r], writes=[g_.r])
                    self.cp('act', g_.t[:, 512:1024], PG1.t[:], reads=[PG1.r], pwrites=[g_.r])
                    ti = tok0 // 128 + i
                    self.dma(G['ap'][ti * 128:(ti + 1) * 128, :], g_.t[:], reads=[g_.r], pwrites=[G['r']])
                for jj in range(8):
                    k2 = cj % 2
                    cj += 1
                    r_, k_, v_ = rT[k2], kT[k2], vT[k2]
                    mix(jj, 128, col0, n, r_.t[:, 0:n], r_.r)
                    mix(8 + jj, 128, col0, n, k_.t[:, 0:n], k_.r)
                    mix(16 + jj, 128, col0, n, v_.t[:, 0:n], v_.r)
                    cs = slice(col0, col0 + n)
                    self.dma(SCN['ap'][0, jj, :, cs], r_.t[:, 0:n], reads=[r_.r], pwrites=[SCN['r']])
                    kr = kkr[k2]
                    self.ts('dve', kr.t[:, 0:n], k_.t[:, 0:n], KKp.t[:, jj:jj + 1], None, ALU.mult, reads=[k_.r, KKp.r], writes=[kr.r])
                    self.act(sq.t[:, 0:n], kr.t[:, 0:n], AF.Square, reads=[kr.r], writes=[sq.r])
                    self.mm(Sx.t[:, 0:n], self.bones32.t[:], sq.t[:, 0:n], True, True, reads=[self.bones32.r, sq.r], writes=[Sx.r])
                    self.act(rn.t[:, 0:n], Sx.t[:, 0:n], AF.Sqrt, reads=[Sx.r], writes=[rn.r], bias=self.eps12.t[:, 0:1], scale=1.0)
                    self.recip(rn.t[:, 0:n], rn.t[:, 0:n], reads=[rn.r], writes=[rn.r])
                    kk_ = kk[k2]
                    self.tt('dve', kk_.t[:, 0:n], kr.t[:, 0:n], rn.t[:, 0:n], ALU.mult, reads=[kr.r, rn.r], writes=[kk_.r])
                    self.dma(SCN['ap'][1, jj, :, cs], kk_.t[:, 0:n], reads=[kk_.r], pwrites=[SCN['r']])
                    for d in range(2):
                        self.mm(LW.t[:, 0:n], wup.t[:, d, jj * 128:(jj + 1) * 128], twd[d].t[:, 0:n], True, True, reads=[wup.r, twd[d].r], writes=[LW.r])
                        self.act(sig[d].t[:, 0:n], LW.t[:, 0:n], AF.Sigmoid, reads=[LW.r, W0.r], writes=[sig[d].r], bias=W0.t[:, d, jj:jj + 1], scale=1.0)
                        self.act(wdec[d].t[:, 0:n], sig[d].t[:, 0:n], AF.Exp, reads=[sig[d].r], writes=[wdec[d].r], scale=-math.exp(-0.5))
                        self.dma(SCN['ap'][2 + 3 * d, jj, :, cs], wdec[d].t[:, 0:n], reads=[wdec[d].r], pwrites=[SCN['r']])
                        self.mm(LA.t[:, 0:n], aup.t[:, d, jj * 128:(jj + 1) * 128], tad[d].t[:, 0:n], True, True, reads=[aup.r, tad[d].r], writes=[LA.r])
                        self.act(aa[d].t[:, 0:n], LA.t[:, 0:n], AF.Sigmoid, reads=[LA.r, A0.r], writes=[aa[d].r], bias=A0.t[:, d, jj:jj + 1], scale=1.0)
                        self.ts('dve', tm[d].t[:, 0:n], aa[d].t[:, 0:n], -1.0, KAp.t[:, jj:jj + 1], ALU.add, ALU.mult, reads=[aa[d].r, KAp.r], writes=[tm[d].r])
                        self.stt('dve', kd[d].t[:, 0:n], tm[d].t[:, 0:n], 1.0, k_.t[:, 0:n], ALU.add, ALU.mult, reads=[tm[d].r, k_.r], writes=[kd[d].r])
                        self.dma(SCN['ap'][4 + 3 * d, jj, :, cs], kd[d].t[:, 0:n], reads=[kd[d].r], pwrites=[SCN['r']])
                        self.tt('pool', bd[d].t[:, 0:n], kk_.t[:, 0:n], aa[d].t[:, 0:n], ALU.mult, reads=[kk_.r, aa[d].r], writes=[bd[d].r])
                        self.dma(SCN['ap'][3 + 3 * d, jj, :, cs], bd[d].t[:, 0:n], reads=[bd[d].r], pwrites=[SCN['r']])
                        self.stt('dve', prod[d].t[:, 0:n], r_.t[:, 0:n], RKp.t[:, jj:jj + 1], kd[d].t[:, 0:n], ALU.mult, ALU.mult, reads=[r_.r, RKp.r, kd[d].r], writes=[prod[d].r])
                        for i in range(nt):
                            c_ = i * 32 + d * 16 + 2 * jj
                            self.mm(PRK.t[:, c_:c_ + 2], prod[d].t[:, i * 128:(i + 1) * 128], self.bind32.t[:], True, True,
                                    reads=[prod[d].r, self.bind32.r], pwrites=[PRK.r])
                    for i in range(nt):
                        self.tr(PV.t[:, i * 128:(i + 1) * 128], v_.t[:, i * 128:(i + 1) * 128], self.ident32.t[:],
                                reads=[v_.r, self.ident32.r], writes=[PV.r] if i == 0 else [], pwrites=[] if i == 0 else [PV.r], inc=(i == nt - 1))
                    self.cp('act', vtok.t[:, 0:nt, jj * 128:(jj + 1) * 128], PV.t[:, 0:nt * 128].rearrange("p (i c) -> p i c", i=nt),
                            reads=[PV.r], pwrites=[vtok.r])
                self.cp('act', rkt.t[:, 0:nt, :], PRK.t[:, 0:nt * 32].rearrange("p (i c) -> p i c", i=nt), reads=[PRK.r], writes=[rkt.r])
                rows = slice(tok0, tok0 + n)
                self.dma(RK['ap'][rows, :].rearrange("(i p) c -> p i c", p=128), rkt.t[:, 0:nt, :], reads=[rkt.r], pwrites=[RK['r']])
                self.dma(V32['ap'][rows, :].rearrange("(i p) c -> p i c", p=128), vtok.t[:, 0:nt, :], reads=[vtok.r], pwrites=[V32['r']])
                self.cp('pool', vb16.t[:, 0:nt, :], vtok.t[:, 0:nt, :], reads=[vtok.r], writes=[vb16.r])
                for h2 in range(2):
                    for i in range(nt):
                        src = vb16.t[:, i, :].rearrange("p (j h v) -> p j h v", j=8, h=2)[:, :, h2, :]
                        dst = VH['ap'][h2, tok0 + i * 128:tok0 + (i + 1) * 128, :].rearrange("p (j v) -> p j v", j=8)
                        self.dma(dst, src, reads=[vb16.r], pwrites=[VH['r']])
        self.P.barrier()

    def stage_escan(self, SCN, VH, YS, nsteps=T):
        CH = 64
        with contextlib.ExitStack() as sc:
            S = self.sb(sc, [128, 2, 8, 64], F32, 'S')
            Sw = self.sb(sc, [128, 2, 8, 64], F32, 'Sw')
            tmpB = self.sb(sc, [128, 2, 8, 64], F32, 'tmpB')
            tmpV = self.sb(sc, [128, 2, 8, 64], F32, 'tmpV')
            tmpK = self.sb(sc, [128, 2, 512], BF16, 'tmpK')
            tmpR = self.sb(sc, [128, 2, 512], BF16, 'tmpR')
            q2 = [self.sb(sc, [128, 2, 5, 8, CH], F32, 'q2') for _ in range(2)]
            vbb = [self.sb(sc, [128, 2, 16, 512], BF16, 'vbb') for _ in range(2)]
            Z = self.sb(sc, [128, 256], BF16, 'Z')
            ysb = [self.sb(sc, [128, 2, 512], F32, 'ysb') for _ in range(2)]
            self.memset('pool', Z.t[:], 0.0, writes=[Z.r])
            self.memset('pool', Z.t[0:64, 127:128], 1.0, pwrites=[Z.r])
            self.memset('pool', Z.t[64:128, 191:192], 1.0, pwrites=[Z.r])
            self.memset('pool', S.t[:], 0.0, writes=[S.r])
            SK = Tl(self.pp[0][:, :])
            SK.r = self.pb[0].r

            def chunk_ranges(c):
                if c < 4:
                    return (4144 + 64 * c, 4144 + 192 - 64 * c, 4096 + 64 * c, 4096 + 192 - 64 * c)
                cc = c - 4
                return (16 + 64 * cc, 16 + 4096 - 64 * (cc + 1), 64 * cc, 4096 - 64 * (cc + 1))

            def load_chunk(c):
                qb = q2[c % 2]
                cf0, cb0, _, _ = chunk_ranges(c)
                for d, c0 in ((0, cf0), (1, cb0)):
                    self.dma(qb.t[:, d, 0:2, :, :], SCN['ap'][0:2, :, :, c0:c0 + CH].rearrange("q j p c -> p q j c"), reads=[SCN['r']], pwrites=[qb.r])
                    self.dma(qb.t[:, d, 2:5, :, :], SCN['ap'][2 + 3 * d:5 + 3 * d, :, :, c0:c0 + CH].rearrange("q j p c -> p q j c"), reads=[SCN['r']], pwrites=[qb.r])

            def load_sub(c, sub):
                vb = vbb[(c * 4 + sub) % 2]
                _, _, tf0, tb0 = chunk_ranges(c)
                rf = tf0 + 16 * sub
                rb_ = tb0 + 48 - 16 * sub
                for d, r0 in ((0, rf), (1, rb_)):
                    for h2 in range(2):
                        self.dma(vb.t[h2 * 64:(h2 + 1) * 64, d, :, :], VH['ap'][h2, r0:r0 + 16, :].partition_broadcast(64), reads=[VH['r']], pwrites=[vb.r])

            def opnd(qb, qi, sl):
                a0 = qb.t[:, 0, qi, :, sl]
                a1 = qb.t[:, 1, qi, :, CH - 1 - sl]
                return bass.AP(a0.tensor, a0.offset, [list(a0.ap[0]), [a1.offset - a0.offset, 2], [CH, 8], [0, 64]])

            def vopnd(vb, ss):
                a0 = vb.t[:, 0, ss, :]
                a1 = vb.t[:, 1, 15 - ss, :]
                return bass.AP(a0.tensor, a0.offset, [list(a0.ap[0]), [a1.offset - a0.offset, 2], [64, 8], [1, 64]])

            nch = nsteps // CH
            load_chunk(0)
            load_sub(0, 0)
            for c in range(nch):
                qb = q2[c % 2]
                if c + 1 < nch:
                    load_chunk(c + 1)
                Y0 = self.pb[2 + (c % 2) * 2]
                Y1 = self.pb[3 + (c % 2) * 2]
                cf0, cb0, tf0, tb0 = chunk_ranges(c)
                for sl in range(CH):
                    sub, ss = sl // 16, sl % 16
                    if ss == 0:
                        if sub < 3:
                            load_sub(c, sub + 1)
                        elif c + 1 < nch:
                            load_sub(c + 1, 0)
                    vb = vbb[(c * 4 + sub) % 2]
                    Sf = S.t[:]
                    self.tt('dve', tmpK.t[:].rearrange("p d (j v) -> p d j v", j=8), Sf, opnd(qb, 1, sl), ALU.mult, reads=[S.r, qb.r], writes=[tmpK.r])
                    self.mm(SK.t[:, 0:512], self.bones16.t[:], tmpK.t[:, 0, :], True, True, reads=[self.bones16.r, tmpK.r], writes=[SK.r], inc=False)
                    self.mm(SK.t[:, 512:1024], self.bones16.t[:], tmpK.t[:, 1, :], True, True, reads=[self.bones16.r, tmpK.r], pwrites=[SK.r])
                    self.tt('pool', Sw.t[:], Sf, opnd(qb, 2, sl), ALU.mult, reads=[S.r, qb.r], writes=[Sw.r])
                    self.tt('pool', tmpV.t[:], vopnd(vb, ss), opnd(qb, 4, sl), ALU.mult, reads=[vb.r, qb.r], writes=[tmpV.r])
                    self.tt('dve', tmpB.t[:], SK.t[:, :].rearrange("p (d j v) -> p d j v", d=2, j=8), opnd(qb, 3, sl), ALU.mult, reads=[SK.r, qb.r], writes=[tmpB.r])
                    self.tt('dve', Sw.t[:], Sw.t[:], tmpB.t[:], ALU.subtract, reads=[Sw.r, tmpB.r], writes=[Sw.r])
                    self.tt('dve', S.t[:], Sw.t[:], tmpV.t[:], ALU.add, reads=[Sw.r, tmpV.r], writes=[S.r])
                    self.tt('dve', tmpR.t[:].rearrange("p d (j v) -> p d j v", j=8), S.t[:], opnd(qb, 0, sl), ALU.mult, reads=[S.r, qb.r], writes=[tmpR.r])
                    first, last = sl == 0, sl == CH - 1
                    self.mm(Y0.t[:], Z.t[:, 127 - sl:255 - sl], tmpR.t[:, 0, :], first, last, reads=[Z.r, tmpR.r],
                            writes=[Y0.r] if first else [], pwrites=[] if first else [Y0.r], inc=False)
                    self.mm(Y1.t[:], Z.t[:, 64 + sl:192 + sl], tmpR.t[:, 1, :], first, last, reads=[Z.r, tmpR.r],
                            writes=[Y1.r] if first else [], pwrites=[] if first else [Y1.r])
                yb = ysb[c % 2]
                self.cp('act', yb.t[:, 0, :], Y0.t[:], reads=[Y0.r], writes=[yb.r])
                self.cp('act', yb.t[:, 1, :], Y1.t[:], reads=[Y1.r], pwrites=[yb.r])
                for d, r0 in ((0, tf0), (1, tb0)):
                    for h2 in range(2):
                        dst = YS['ap'][d, r0:r0 + CH, :].rearrange("t (j h v) -> t j h v", j=8, h=2)[:, :, h2, :]
                        self.dma(dst, yb.t[h2 * 64:(h2 + 1) * 64, d, :].rearrange("p (j v) -> p j v", j=8), reads=[yb.r], pwrites=[YS['r']])
        self.P.barrier()

    def stage_eepi(self, W, YS, V32, G, RK, CAT):
        with contextlib.ExitStack() as sc:
            gng = self.load_bcast(sc, W['gn_g'], 1024, 'gng')
            gnb = self.load_bcast(sc, W['gn_b'], 1024, 'gnb')
            y0 = [self.sb(sc, [128, 16, 64], F32, 'y0') for _ in range(2)]
            y1 = [self.sb(sc, [128, 16, 64], F32, 'y1') for _ in range(2)]
            vv = [self.sb(sc, [128, 16, 64], F32, 'vv') for _ in range(2)]
            gg = [self.sb(sc, [128, 16, 64], F32, 'gg') for _ in range(2)]
            rk = [self.sb(sc, [128, 32], F32, 'rk') for _ in range(2)]
            sq = self.sb(sc, [128, 16, 64], F32, 'sq')
            st = [self.sb(sc, [128, 5, 16], F32, 'st') for _ in range(2)]
            ob = [self.sb(sc, [128, 1024], BF16, 'ob') for _ in range(2)]
            for ti in range(NT):
                k = ti % 2
                rows = slice(ti * 128, (ti + 1) * 128)
                a, b, v, g, r, s, o = y0[k], y1[k], vv[k], gg[k], rk[k], st[k], ob[k]
                self.dma(a.t[:], YS['ap'][0, rows, :].rearrange("p (h v) -> p h v", h=16), reads=[YS['r']], writes=[a.r])
                self.dma(b.t[:], YS['ap'][1, rows, :].rearrange("p (h v) -> p h v", h=16), reads=[YS['r']], writes=[b.r])
                self.dma(v.t[:], V32['ap'][rows, :].rearrange("p (h v) -> p h v", h=16), reads=[V32['r']], writes=[v.r])
                self.dma(g.t[:], G['ap'][rows, :].rearrange("p (h v) -> p h v", h=16), reads=[G['r']], writes=[g.r])
                self.dma(r.t[:], RK['ap'][rows, :], reads=[RK['r']], writes=[r.r])
                self.tt('dve', a.t[:], a.t[:], b.t[:], ALU.add, reads=[a.r, b.r], writes=[a.r])
                self.P.add('dve', lambda e, a=a, s=s: e.reduce_sum(out=s.t[:, 0, :], in_=a.t[:], axis=AX.X), reads=[a.r], writes=[s.r])
                self.ts('dve', s.t[:, 0, :], s.t[:, 0, :], -1.0 / 64, None, ALU.mult, reads=[s.r], writes=[s.r])
                self.tt('dve', a.t[:], a.t[:], s.t[:, 0, :].unsqueeze(2).to_broadcast([128, 16, 64]), ALU.add, reads=[a.r, s.r], writes=[a.r])
                self.tt('pool', sq.t[:], a.t[:], a.t[:], ALU.mult, reads=[a.r], writes=[sq.r])
                self.P.add('dve', lambda e, s=s: e.reduce_sum(out=s.t[:, 1, :], in_=sq.t[:], axis=AX.X), reads=[sq.r], writes=[s.r])
                self.act(s.t[:, 2, :], s.t[:, 1, :], AF.Sqrt, reads=[s.r], writes=[s.r], bias=self.epsg.t[:, 0:1], scale=1.0 / 64)
                self.recip(s.t[:, 3, :], s.t[:, 2, :], reads=[s.r], writes=[s.r])
                self.tt('dve', a.t[:], a.t[:], s.t[:, 3, :].unsqueeze(2).to_broadcast([128, 16, 64]), ALU.mult, reads=[a.r, s.r], writes=[a.r])
                af = a.t[:].rearrange("p h v -> p (h v)")
                self.tt('pool', af, af, gng.t[:], ALU.mult, reads=[a.r, gng.r], writes=[a.r])
                self.tt('dve', af, af, gnb.t[:], ALU.add, reads=[a.r, gnb.r], writes=[a.r])
                self.tt('dve', s.t[:, 4, :], r.t[:, 0:16], r.t[:, 16:32], ALU.add, reads=[r.r], writes=[s.r])
                self.tt('pool', v.t[:], v.t[:], s.t[:, 4, :].unsqueeze(2).to_broadcast([128, 16, 64]), ALU.mult, reads=[v.r, s.r], writes=[v.r])
                self.tt('dve', a.t[:], a.t[:], v.t[:], ALU.add, reads=[a.r, v.r], writes=[a.r])
                self.tt('dve', o.t[:].rearrange("p (h v) -> p h v", h=16), a.t[:], g.t[:], ALU.mult, reads=[a.r, g.r], writes=[o.r])
                self.dma(CAT['ap'][rows, 0:1024], o.t[:], reads=[o.r], pwrites=[CAT['r']])
        self.P.barrier()

    def stage_econv(self, W, PT, CAT):
        with contextlib.ExitStack() as sc:
            CW = self.sb(sc, [128, 8, 31], F32, 'CW')
            CB = self.sb(sc, [128, 8], F32, 'CB')
            lg = self.load_bcast(sc, W['cv_ln_g'], 1024, 'cvg')
            lb = self.load_bcast(sc, W['cv_ln_b'], 1024, 'cvb')
            for c in range(8):
                self.dma(CW.t[:, c, :], W['cv_w'][:, c * 128:(c + 1) * 128].rearrange("k p -> p k"), pwrites=[CW.r], allow_slow_non_contiguous=True)
            self.dma(CB.t[:], W['cv_b'].rearrange("(c p) -> p c", p=128), writes=[CB.r], allow_slow_non_contiguous=True)
            val = [self.sb(sc, [128, 542], F32, 'val') for _ in range(2)]
            gat = [self.sb(sc, [128, 542], F32, 'gat') for _ in range(2)]
            acc = [self.sb(sc, [128, 512], F32, 'acc') for _ in range(2)]
            zc = self.sb(sc, [128, 4, 1024], F32, 'zc')
            sq = self.sb(sc, [128, 1024], F32, 'sq')
            st = [self.sb(sc, [128, 8], F32, 'st') for _ in range(2)]
            ob = [self.sb(sc, [128, 1024], BF16, 'ob') for _ in range(2)]
            cc = 0
            for blk in BLOCKS:
                tok0, n, col0, which = blk
                nt = n // 128
                for c in range(8):
                    k = cc % 2
                    cc += 1
                    v, g, a = val[k], gat[k], acc[k]
                    eng = 'dve'
                    self.dma(v.t[:, 0:n + 30], PT['ap'][29 + c, :, col0 - 15:col0 + n + 15], reads=[PT['r']], writes=[v.r])
                    self.dma(g.t[:, 0:n + 30], PT['ap'][37 + c, :, col0 - 15:col0 + n + 15], reads=[PT['r']], writes=[g.r])
                    self.act(g.t[:, 0:n + 30], g.t[:, 0:n + 30], AF.Sigmoid, reads=[g.r], writes=[g.r])
                    self.tt(eng, v.t[:, 0:n + 30], v.t[:, 0:n + 30], g.t[:, 0:n + 30], ALU.mult, reads=[v.r, g.r], writes=[v.r])
                    self.ts(eng, a.t[:, 0:n], v.t[:, 0:n], CW.t[:, c, 0:1], CB.t[:, c:c + 1], ALU.mult, ALU.add, reads=[v.r, CW.r, CB.r], writes=[a.r])
                    for kk_ in range(1, 31):
                        self.stt(eng, a.t[:, 0:n], v.t[:, kk_:kk_ + n], CW.t[:, c, kk_:kk_ + 1], a.t[:, 0:n], ALU.mult, ALU.add, reads=[v.r, CW.r, a.r], writes=[a.r])
                    PV = self.pb[k]
                    for i in range(nt):
                        self.tr(PV.t[:, i * 128:(i + 1) * 128], a.t[:, i * 128:(i + 1) * 128], self.ident32.t[:],
                                reads=[a.r, self.ident32.r], writes=[PV.r] if i == 0 else [], pwrites=[] if i == 0 else [PV.r], inc=(i == nt - 1))
                    self.cp('act', zc.t[:, 0:nt, c * 128:(c + 1) * 128], PV.t[:, 0:nt * 128].rearrange("p (i c) -> p i c", i=nt), reads=[PV.r], pwrites=[zc.r])
                for i in range(nt):
                    ti = tok0 // 128 + i
                    s, o = st[i % 2], ob[i % 2]
                    t = zc.t[:, i, :]
                    self.P.add('dve', lambda e, t=t, s=s: e.reduce_sum(out=s.t[:, 0:1], in_=t, axis=AX.X), reads=[zc.r], writes=[s.r])
                    self.ts('dve', s.t[:, 1:2], s.t[:, 0:1], -1.0 / 1024, None, ALU.mult, reads=[s.r], writes=[s.r])
                    self.act(sq.t[:], t, AF.Square, reads=[zc.r, s.r], writes=[sq.r], pwrites=[s.r], bias=s.t[:, 1:2], scale=1.0, accum_out=s.t[:, 2:3])
                    self.act(s.t[:, 3:4], s.t[:, 2:3], AF.Sqrt, reads=[s.r], writes=[s.r], bias=self.eps5.t[:, 0:1], scale=1.0 / 1024)
                    self.recip(s.t[:, 4:5], s.t[:, 3:4], reads=[s.r], writes=[s.r])
                    self.tt('dve', s.t[:, 5:6], s.t[:, 1:2], s.t[:, 4:5], ALU.mult, reads=[s.r], writes=[s.r])
                    self.act(sq.t[:], t, AF.Identity, reads=[zc.r, s.r], writes=[sq.r], bias=s.t[:, 5:6], scale=s.t[:, 4:5])
                    self.tt('pool', sq.t[:], sq.t[:], lg.t[:], ALU.mult, reads=[sq.r, lg.r], writes=[sq.r])
                    self.tt('dve', sq.t[:], sq.t[:], lb.t[:], ALU.add, reads=[sq.r, lb.r], writes=[sq.r])
                    self.act(o.t[:], sq.t[:], AF.Silu, reads=[sq.r], writes=[o.r])
                    self.dma(CAT['ap'][ti * 128:(ti + 1) * 128, 1024:2048], o.t[:], reads=[o.r], pwrites=[CAT['r']])
        self.P.barrier()

    def stage_eproj(self, CAT, W_ap, Y):
        with contextlib.ExitStack() as sc:
            cb = [self.sb(sc, [128, 2048], BF16, 'cb') for _ in range(2)]
            xT = self.sb(sc, [128, 16, 512], BF16, 'xT')
            wsl = [self.sb(sc, [128, 16, 512], BF16, 'wsl') for _ in range(2)]
            stg = [self.sb(sc, [128, 512], F32, 'stg') for _ in range(4)]
            cnt = [0, 0]
            kq = 0
            for blk in BLOCKS:
                tok0, n, col0, which = blk
                for i in range(n // 128):
                    ti = tok0 // 128 + i
                    c = cb[kq % 2]
                    kq += 1
                    self.dma(c.t[:], CAT['ap'][ti * 128:(ti + 1) * 128, :], reads=[CAT['r']], writes=[c.r])
                    for half in range(2):
                        pbk = self.pb[6 + half]
                        pv = pbk.t[:].bitcast(BF16)
                        for q in range(8):
                            kc = half * 8 + q
                            self.tr(pv[:, q * 128:(q + 1) * 128], c.t[:, kc * 128:(kc + 1) * 128], self.ident16.t[:],
                                    reads=[c.r, self.ident16.r], writes=[pbk.r] if q == 0 else [], pwrites=[] if q == 0 else [pbk.r], inc=(q == 7))
                        self.cp('act', xT.t[:, half * 8:(half + 1) * 8, i * 128:(i + 1) * 128], pv.rearrange("p (q k) -> p q k", q=8),
                                reads=[pbk.r], pwrites=[xT.r])
                self.gemm_tok(sc, xT, n, W_ap, 2048, Y, tok0, wsl, stg, cnt)
        self.P.barrier()

    def stage_moe_conv(self, l, w1, w3, w2, sw1, sw3, sw2, WB):
        with contextlib.ExitStack() as sc:
            wt = [self.sb(sc, [128, 18432], BF16, 'wcv') for _ in range(2)]
            for e in range(65):
                if e < 64:
                    a1, a3, a2 = w1[l, e], w3[l, e], w2[l, e]
                else:
                    a1, a3, a2 = sw1[l], sw3[l], sw2[l]
                t = wt[e % 2]
                self.dma(t.t[:, 0:6144].rearrange("p (c f) -> p c f", c=16), a1.rearrange("(c p) f -> p c f", p=128), pwrites=[t.r], q='pool')
                self.dma(t.t[:, 6144:12288].rearrange("p (c f) -> p c f", c=16), a3.rearrange("(c p) f -> p c f", p=128), pwrites=[t.r], q='pool')
                self.dma(t.t[:, 12288:18432].rearrange("p (c n) -> p c n", c=3), a2.rearrange("(c p) n -> p c n", p=128), pwrites=[t.r], q='pool')
                self.dma(WB['ap'][e * 128:(e + 1) * 128, :], t.t[:, 0:12288], reads=[t.r], pwrites=[WB['r']])
                self.dma(WB['ap2'][e * 128:(e + 1) * 128, :], t.t[:, 12288:18432], reads=[t.r], pwrites=[WB['r']])
        self.P.barrier()

    def stage_moe_sparse(self, H, F, MODB, l, router, rbias, WB, VF, TOKBUF, YB, with_ctx=True):
        BS = 512
        ntile = NT if with_ctx else 32
        ntok = ntile * 128
        NBLK = (ntok * 6 + 64 * (BS - 1) + BS - 1) // BS
        MAXB = (ntok + BS - 1) // BS
        NSH = (ntile + 3) // 4
        with contextlib.ExitStack() as sc0:
            Mall = self.sb(sc0, [128, NT, 64], F32, 'Mall')
            Gall = self.sb(sc0, [128, NT, 64], F32, 'Gall')
            Rall = self.sb(sc0, [128, NT, 64], F32, 'Rall')
            DKu = self.sb(sc0, [128, NT, 6], U32, 'DKu')
            GK = self.sb(sc0, [128, NT, 6], F32, 'GK')
            idxW = self.sb(sc0, [128, 128], U32, 'idxW')
            base = self.sb(sc0, [128, 64], F32, 'base')
            dbase = self.sb(sc0, [128, 64], F32, 'dbase')
            UT = self.sb(sc0, [128, 128], BF16, 'UT')
            ipf = self.sb(sc0, [128, 1], F32, 'ipf')
            with contextlib.ExitStack() as sc:
                ut32 = self.sb(sc, [128, 128], F32, 'ut32')
                self.memset('pool', ut32.t[:], 1.0, writes=[ut32.r])
                self.P.add('pool', lambda e: e.affine_select(out=ut32.t[:], in_=ut32.t[:], pattern=[[1, 128]], compare_op=ALU.is_gt,
                                                             fill=0.0, base=0, channel_multiplier=-1), reads=[ut32.r], writes=[ut32.r])
                self.cp('dve', UT.t[:], ut32.t[:], reads=[ut32.r], writes=[UT.r])
                ip = self.sb(sc, [128, 1], I32, 'ip')
                self.P.add('pool', lambda e: e.iota(ip.t[:], pattern=[[0, 1]], base=0, channel_multiplier=1), writes=[ip.r])
                self.cp('dve', ipf.t[:], ip.t[:], reads=[ip.r], writes=[ipf.r])
                self.memset('pool', base.t[:], 0.0, writes=[base.r])
                rt32 = self.sb(sc, [128, 16, 64], F32, 'rt32')
                rb = self.load_bcast(sc, rbias, 64, 'rb')
                self.dma(rt32.t[:], router.rearrange("(c p) e -> p c e", p=128), writes=[rt32.r])
                mods = self.load_mods(sc, MODB, l, [8192, 6144])
                msc, msh = mods[8192], mods[6144]
                hb = [self.sb(sc, [128, 2048], F32, 'hb') for _ in range(2)]
                vf32 = [self.sb(sc, [128, 2048], F32, 'vf32') for _ in range(2)]
                ub = [self.sb(sc, [128, 2048], BF16, 'ub') for _ in range(2)]
                vT32 = self.sb(sc, [128, 16, 128], F32, 'vT32')
                g1 = self.sb(sc, [128, 64], F32, 'g1')
                g2 = self.sb(sc, [128, 64], F32, 'g2')
                g3 = self.sb(sc, [128, 16], F32, 'g3')
                mb = self.sb(sc, [128, 64], BF16, 'mb')
                for ti in range(ntile):
                    which = 0 if ti < 32 else 1
                    h, v, u = hb[ti % 2], vf32[ti % 2], ub[ti % 2]
                    rows = slice(ti * 128, (ti + 1) * 128)
                    self.dma(h.t[:], H['ap'][rows, :], reads=[H['r'][ti]], writes=[h.r])
                    self.tt('dve', v.t[:], h.t[:], msc.t[:, which, :], ALU.mult, reads=[h.r, msc.r], writes=[v.r])
                    self.tt('dve', v.t[:], v.t[:], msh.t[:, which, :], ALU.add, reads=[v.r, msh.r], writes=[v.r])
                    self.cp('act', u.t[:], v.t[:], reads=[v.r], writes=[u.r])
                    self.dma(VF['ap'][rows, :], u.t[:], reads=[u.r], pwrites=[VF['r']])
                    for q4 in range(4):
                        pbk = self.pb[q4 % 2]
                        for q in range(4):
                            kc = q4 * 4 + q
                            self.tr(pbk.t[:, q * 128:(q + 1) * 128], v.t[:, kc * 128:(kc + 1) * 128], self.ident32.t[:],
                                    reads=[v.r, self.ident32.r], writes=[pbk.r] if q == 0 else [], pwrites=[] if q == 0 else [pbk.r], inc=(q == 3))
                        self.cp('act' if q4 % 2 else 'dve', vT32.t[:, q4 * 4:(q4 + 1) * 4, :], pbk.t[:].rearrange("p (q k) -> p q k", q=4),
                                reads=[pbk.r], pwrites=[vT32.r])
                    pr = self.pb[2]
                    for kc in range(16):
                        self.mm(pr.t[:, 0:64], vT32.t[:, kc, :], rt32.t[:, kc, :], kc == 0, kc == 15,
                                reads=[vT32.r, rt32.r], writes=[pr.r] if kc == 0 else [], pwrites=[] if kc == 0 else [pr.r], inc=(kc == 15))
                    self.act(g1.t[:], pr.t[:, 0:64], AF.Sigmoid, reads=[pr.r], writes=[g1.r])
                    self.tt('dve', g2.t[:], g1.t[:], rb.t[:], ALU.add, reads=[g1.r, rb.r], writes=[g2.r])
                    self.P.add('dve', lambda e: e.max(out=g3.t[:, 0:8], in_=g2.t[:]), reads=[g2.r], writes=[g3.r])
                    self.ts('dve', Mall.t[:, ti, :], g2.t[:], g3.t[:, 5:6], None, ALU.is_ge, reads=[g2.r, g3.r], pwrites=[Mall.r])
                    self.tt('dve', g1.t[:], g1.t[:], Mall.t[:, ti, :], ALU.mult, reads=[g1.r, Mall.r], writes=[g1.r])
                    self.P.add('dve', lambda e: e.reduce_sum(out=g3.t[:, 8:9], in_=g1.t[:], axis=AX.X), reads=[g1.r], writes=[g3.r])
                    self.recip(g3.t[:, 9:10], g3.t[:, 8:9], reads=[g3.r], writes=[g3.r])
                    self.ts('dve', Gall.t[:, ti, :], g1.t[:], g3.t[:, 9:10], 2.5, ALU.mult, ALU.mult, reads=[g1.r, g3.r], pwrites=[Gall.r])
                    self.cp('dve', mb.t[:], Mall.t[:, ti, :], reads=[Mall.r], writes=[mb.r])
                    pk = self.pb[3]
                    self.mm(pk.t[:, 0:64], UT.t[:], mb.t[:], True, True, reads=[UT.r, mb.r], writes=[pk.r], inc=False)
                    self.mm(pk.t[:, 64:128], self.ones16.t[:], mb.t[:], True, True, reads=[self.ones16.r, mb.r], pwrites=[pk.r])
                    self.tt('dve', Rall.t[:, ti, :], pk.t[:, 0:64], base.t[:], ALU.add, reads=[pk.r, base.r], pwrites=[Rall.r])
                    self.tt('dve', base.t[:], base.t[:], pk.t[:, 64:128], ALU.add, reads=[pk.r, base.r], writes=[base.r])
            self.P.barrier()
            with contextlib.ExitStack() as sc:
                fi = self.sb(sc, [128, 128], I32, 'fi')
                ff = self.sb(sc, [128, 128], F32, 'ff')
                self.P.add('pool', lambda e: e.iota(fi.t[:], pattern=[[1, 128]], base=0, channel_multiplier=0), writes=[fi.r])
                self.cp('dve', ff.t[:], fi.t[:], reads=[fi.r], writes=[ff.r])
                thr = self.sb(sc, [128, 16], F32, 'thr')
                self.ts('dve', thr.t[:], ff.t[:, 0:16], float(BS), None, ALU.mult, reads=[ff.r], writes=[thr.r])
                cmp = self.sb(sc, [128, 64, MAXB], F32, 'cmp')
                self.tt('dve', cmp.t[:], base.t[:].unsqueeze(2).to_broadcast([128, 64, MAXB]),
                        thr.t[:, 0:MAXB].unsqueeze(1).to_broadcast([128, 64, MAXB]), ALU.is_gt, reads=[base.r, thr.r], writes=[cmp.r])
                nblk = self.sb(sc, [128, 64], F32, 'nblk')
                self.P.add('dve', lambda e: e.reduce_sum(out=nblk.t[:], in_=cmp.t[:], axis=AX.X), reads=[cmp.r], writes=[nblk.r])
                xa = self.sb(sc, [128, 64], F32, 'xa')
                xb = self.sb(sc, [128, 64], F32, 'xb')
                self.cp('dve', xa.t[:], nblk.t[:], reads=[nblk.r], writes=[xa.r])
                cur, oth = xa, xb
                for s in (1, 2, 4, 8, 16, 32):
                    self.cp('dve', oth.t[:, 0:s], cur.t[:, 0:s], reads=[cur.r], writes=[oth.r])
                    self.tt('dve', oth.t[:, s:64], cur.t[:, s:64], cur.t[:, 0:64 - s], ALU.add, reads=[cur.r], pwrites=[oth.r])
                    cur, oth = oth, cur
                pend = cur
                self.tt('dve', dbase.t[:], pend.t[:], nblk.t[:], ALU.subtract, reads=[pend.r, nblk.r], writes=[dbase.r])
                self.ts('dve', dbase.t[:], dbase.t[:], float(BS), None, ALU.mult, reads=[dbase.r], writes=[dbase.r])
                cmp2 = self.sb(sc, [128, 128, 64], F32, 'cmp2')
                self.tt('dve', cmp2.t[:], pend.t[:].unsqueeze(1).to_broadcast([128, 128, 64]),
                        ff.t[:].unsqueeze(2).to_broadcast([128, 128, 64]), ALU.is_le, reads=[pend.r, ff.r], writes=[cmp2.r])
                be = self.sb(sc, [128, 128], F32, 'be')
                self.P.add('dve', lambda e: e.reduce_sum(out=be.t[:], in_=cmp2.t[:], axis=AX.X), reads=[cmp2.r], writes=[be.r])
                self.ts('dve', be.t[:], be.t[:], 63.0, 128.0, ALU.min, ALU.mult, reads=[be.r], writes=[be.r])
                self.ts('dve', be.t[:], be.t[:], ipf.t[:, 0:1], None, ALU.add, reads=[be.r, ipf.r], writes=[be.r])
                self.cp('dve', idxW.t[:], be.t[:], reads=[be.r], writes=[idxW.r])
                zt = self.sb(sc, [128, NBLK, 16], I32, 'zt')
                self.memset('pool', zt.t[:], 0, writes=[zt.r])
                for i in range(4):
                    self.dma(TOKBUF['ap'][i * NBLK * 128:(i + 1) * NBLK * 128, :].rearrange("(a p) c -> p a c", p=128), zt.t[:], reads=[zt.r], pwrites=[TOKBUF['r']])
            self.P.barrier()
            with contextlib.ExitStack() as sc:
                dest = [self.sb(sc, [128, 64], F32, 'dest') for _ in range(2)]
                ca = [self.sb(sc, [128, 64], F32, 'ca') for _ in range(2)]
                cb_ = [self.sb(sc, [128, 64], F32, 'cb') for _ in range(2)]
                oh = [self.sb(sc, [128, 64], F32, 'oh') for _ in range(2)]
                o2 = [self.sb(sc, [128, 64], F32, 'o2') for _ in range(2)]
                dk = [self.sb(sc, [128, 8], F32, 'dk') for _ in range(2)]
                src = [self.sb(sc, [128, 16], I32, 'src') for _ in range(2)]
                for ti in range(ntile):
                    k2 = ti % 2
                    d_, s_ = dest[k2], src[k2]
                    self.tt('dve', d_.t[:], Rall.t[:, ti, :], dbase.t[:], ALU.add, reads=[Rall.r, dbase.r], writes=[d_.r])
                    cur, oth = ca[k2], cb_[k2]
                    self.cp('dve', cur.t[:], Mall.t[:, ti, :], reads=[Mall.r], writes=[cur.r])
                    for s in (1, 2, 4, 8, 16, 32):
                        self.cp('dve', oth.t[:, 0:s], cur.t[:, 0:s], reads=[cur.r], writes=[oth.r])
                        self.tt('dve', oth.t[:, s:64], cur.t[:, s:64], cur.t[:, 0:64 - s], ALU.add, reads=[cur.r], pwrites=[oth.r])
                        cur, oth = oth, cur
                    for k in range(6):
                        o_, p_ = oh[k % 2], o2[k % 2]
                        self.ts('dve', o_.t[:], cur.t[:], float(k + 1), None, ALU.is_equal, reads=[cur.r], writes=[o_.r])
                        self.tt('dve', o_.t[:], o_.t[:], Mall.t[:, ti, :], ALU.mult, reads=[o_.r, Mall.r], writes=[o_.r])
                        self.tt('dve', p_.t[:], o_.t[:], d_.t[:], ALU.mult, reads=[o_.r, d_.r], writes=[p_.r])
                        self.P.add('dve', lambda e, p_=p_, dkt=dk[k2], k=k: e.reduce_sum(out=dkt.t[:, k:k + 1], in_=p_.t[:], axis=AX.X), reads=[p_.r], pwrites=[dk[k2].r])
                        self.tt('dve', p_.t[:], o_.t[:], Gall.t[:, ti, :], ALU.mult, reads=[o_.r, Gall.r], writes=[p_.r])
                        self.P.add('dve', lambda e, p_=p_, ti=ti, k=k: e.reduce_sum(out=GK.t[:, ti, k:k + 1], in_=p_.t[:], axis=AX.X), reads=[p_.r], pwrites=[GK.r])
                    self.cp('dve', DKu.t[:, ti, :], dk[k2].t[:, 0:6], reads=[dk[k2].r], pwrites=[DKu.r])
                    self.P.add('pool', lambda e, s_=s_, ti=ti: e.iota(s_.t[:], pattern=[[0, 16]], base=ti * 128, channel_multiplier=1), writes=[s_.r])
                    for k in range(6):
                        self.P.add('pool', lambda e, s_=s_, ti=ti, k=k: e.indirect_dma_start(
                            out=TOKBUF['ap'], out_offset=bass.IndirectOffsetOnAxis(ap=DKu.t[:, ti, k:k + 1], axis=0), in_=s_.t[:], in_offset=None),
                            reads=[DKu.r, s_.r], pwrites=[TOKBUF['r']], dma=True)
            self.P.barrier()
            with contextlib.ExitStack() as sc:
                tb = [self.sb(sc, [128, 4, 16], I32, 'tb') for _ in range(2)]
                xg = [self.sb(sc, [128, 2048], BF16, 'xg') for _ in range(3)]
                xgT = [self.sb(sc, [128, 16, 512], BF16, 'xgT') for _ in range(2)]
                wt = [self.sb(sc, [128, 18432], BF16, 'wt') for _ in range(2)]
                aT = [self.sb(sc, [128, 3, 512], BF16, 'aT') for _ in range(2)]
                silt = [self.sb(sc, [128, 512], F32, 'silt') for _ in range(2)]
                yb = [self.sb(sc, [128, 2048], BF16, 'yb') for _ in range(2)]
                cx = 0
                cA = 0
                cY = 0
                cT = 0
                for b in range(NBLK + NSH):
                    routed = b < NBLK
                    w = wt[b % 2] if routed else wt[NBLK % 2]
                    xT = xgT[b % 2]
                    if routed:
                        t_ = tb[b % 2]
                        self.dma(t_.t[:], TOKBUF['ap'][b * BS:(b + 1) * BS, :].rearrange("(i p) c -> p i c", p=128), reads=[TOKBUF['r']], writes=[t_.r])
                        self.P.add('pool', lambda e, w=w, b=b: e.indirect_dma_start(
                            out=w.t[:, 0:12288], out_offset=None, in_=WB['ap'], in_offset=bass.IndirectOffsetOnAxis(ap=idxW.t[:, b:b + 1], axis=0)),
                            reads=[idxW.r, WB['r']], writes=[w.r], dma=True)
                        self.P.add('pool', lambda e, w=w, b=b: e.indirect_dma_start(
                            out=w.t[:, 12288:18432], out_offset=None, in_=WB['ap2'], in_offset=bass.IndirectOffsetOnAxis(ap=idxW.t[:, b:b + 1], axis=0)),
                            reads=[idxW.r, WB['r']], pwrites=[w.r], dma=True)
                        nsub = 4
                    else:
                        sbi = b - NBLK
                        if sbi == 0:
                            self.dma(w.t[:, 0:12288], WB['ap'][64 * 128:65 * 128, :], reads=[WB['r']], writes=[w.r])
                            self.dma(w.t[:, 12288:18432], WB['ap2'][64 * 128:65 * 128, :], reads=[WB['r']], pwrites=[w.r])
                        nsub = min(4, ntile - sbi * 4)
                    ns = nsub * 128
                    for i in range(nsub):
                        x = xg[cx % 3]
                        cx += 1
                        if routed:
                            self.P.add('pool', lambda e, x=x, t_=t_, i=i: e.indirect_dma_start(
                                out=x.t[:], out_offset=None, in_=VF['ap'], in_offset=bass.IndirectOffsetOnAxis(ap=t_.t[:, i, 0:1].bitcast(U32), axis=0)),
                                reads=[t_.r, VF['r']], writes=[x.r], dma=True)
                        else:
                            ti = (b - NBLK) * 4 + i
                            self.dma(x.t[:], VF['ap'][ti * 128:(ti + 1) * 128, :], reads=[VF['r']], writes=[x.r])
                        for half in range(2):
                            pbk = self.pb[4 + cT % 2]
                            cT += 1
                            pv = pbk.t[:].bitcast(BF16)
                            for q in range(8):
                                kc = half * 8 + q
                                self.tr(pv[:, q * 128:(q + 1) * 128], x.t[:, kc * 128:(kc + 1) * 128], self.ident16.t[:],
                                        reads=[x.r, self.ident16.r], writes=[pbk.r] if q == 0 else [], pwrites=[] if q == 0 else [pbk.r], inc=(q == 7))
                            self.cp('act' if half else 'dve', xT.t[:, half * 8:(half + 1) * 8, i * 128:(i + 1) * 128], pv.rearrange("p (q k) -> p q k", q=8),
                                    reads=[pbk.r], pwrites=[xT.r])
                    at = aT[b % 2]
                    for fc in range(3):
                        A = self.pb[(cA * 2) % 4]
                        B = self.pb[(cA * 2 + 1) % 4]
                        sl = silt[cA % 2]
                        cA += 1
                        for kc in range(16):
                            o1 = kc * 384 + fc * 128
                            self.mm(A.t[:, 0:ns], w.t[:, o1:o1 + 128], xT.t[:, kc, 0:ns], kc == 0, kc == 15,
                                    reads=[w.r, xT.r], writes=[A.r] if kc == 0 else [], pwrites=[] if kc == 0 else [A.r], inc=(kc == 15))
                        for kc in range(16):
                            o3 = 6144 + kc * 384 + fc * 128
                            self.mm(B.t[:, 0:ns], w.t[:, o3:o3 + 128], xT.t[:, kc, 0:ns], kc == 0, kc == 15,
                                    reads=[w.r, xT.r], writes=[B.r] if kc == 0 else [], pwrites=[] if kc == 0 else [B.r], inc=(kc == 15))
                        self.act(sl.t[:, 0:ns], A.t[:, 0:ns], AF.Silu, reads=[A.r], writes=[sl.r])
                        self.tt('dve', at.t[:, fc, 0:ns], sl.t[:, 0:ns], B.t[:, 0:ns], ALU.mult, reads=[sl.r, B.r], pwrites=[at.r])
                    for i in range(nsub):
                        y = yb[cY % 2]
                        for cb in range(4):
                            Yp = self.pb[4 + cY % 4] if False else self.pb[4 + (cY * 4 + cb) % 4]
                            for fc in range(3):
                                o2_ = 12288 + fc * 2048 + cb * 512
                                self.mm(Yp.t[:], at.t[:, fc, i * 128:(i + 1) * 128], w.t[:, o2_:o2_ + 512], fc == 0, fc == 2,
                                        reads=[at.r, w.r], writes=[Yp.r] if fc == 0 else [], pwrites=[] if fc == 0 else [Yp.r], inc=(fc == 2))
                            self.cp('act' if cb % 2 else 'dve', y.t[:, cb * 512:(cb + 1) * 512], Yp.t[:], reads=[Yp.r], pwrites=[y.r])
                        cY += 1
                        if routed:
                            r0 = b * BS + i * 128
                            self.dma(YB['ap'][r0:r0 + 128, :], y.t[:], reads=[y.r], pwrites=[YB['r']])
                        else:
                            r0 = ((b - NBLK) * 4 + i) * 128
                            self.dma(YB['ap2'][r0:r0 + 128, :], y.t[:], reads=[y.r], pwrites=[YB['r']])
            self.P.barrier()
            with contextlib.ExitStack() as sc:
                acc = [self.sb(sc, [128, 2048], F32, 'acc') for _ in range(2)]
                ysh = [self.sb(sc, [128, 2048], BF16, 'ysh') for _ in range(2)]
                yk = [self.sb(sc, [128, 2048], BF16, 'yk') for _ in range(4)]
                cg = 0
                for ti in range(ntile):
                    a, s = acc[ti % 2], ysh[ti % 2]
                    r0 = ti * 128
                    self.dma(s.t[:], YB['ap2'][r0:r0 + 128, :], reads=[YB['r']], writes=[s.r])
                    for k in range(6):
                        y = yk[cg % 4]
                        cg += 1
                        self.P.add('pool', lambda e, y=y, ti=ti, k=k: e.indirect_dma_start(
                            out=y.t[:], out_offset=None, in_=YB['ap'], in_offset=bass.IndirectOffsetOnAxis(ap=DKu.t[:, ti, k:k + 1], axis=0)),
                            reads=[DKu.r, YB['r']], writes=[y.r], dma=True)
                        self.stt('dve', a.t[:], y.t[:], GK.t[:, ti, k:k + 1], s.t[:] if k == 0 else a.t[:], ALU.mult, ALU.add,
                                 reads=[y.r, GK.r, s.r, a.r] if k == 0 else [y.r, GK.r, a.r], writes=[a.r])
                    self.dma(F['ap'][ti * 128:(ti + 1) * 128, :], a.t[:], reads=[a.r], writes=[F['r'][ti]])
        self.P.barrier()

    def stage_escan_chunked(self, SCN, V32, YS, nch=68):
        C = 64
        with contextlib.ExitStack() as sc:
            slots = [Tl(self.pp[i][:, s * 512:s * 512 + 128]) for i in range(4) for s in range(2)]
            ia_i = self.sb(sc, [128, 1], I32, 'ia_i')
            ia = self.sb(sc, [128, 1], F32, 'ia')
            ib_i = self.sb(sc, [128, 128], I32, 'ib_i')
            ib = self.sb(sc, [128, 128], F32, 'ib')
            tq = self.sb(sc, [128, 128], F32, 'tq')
            self.P.add('pool', lambda e: e.iota(ia_i.t[:], pattern=[[0, 1]], base=0, channel_multiplier=1), writes=[ia_i.r])
            self.cp('dve', ia.t[:], ia_i.t[:], reads=[ia_i.r], writes=[ia.r])
            self.ts('dve', tq.t[:, 0:1], ia.t[:], 64.0, -64.0, ALU.is_ge, ALU.mult, reads=[ia.r], writes=[tq.r])
            self.tt('dve', ia.t[:], ia.t[:], tq.t[:, 0:1], ALU.add, reads=[ia.r, tq.r], writes=[ia.r])
            self.P.add('pool', lambda e: e.iota(ib_i.t[:], pattern=[[1, 128]], base=0, channel_multiplier=0), writes=[ib_i.r])
            self.cp('dve', ib.t[:], ib_i.t[:], reads=[ib_i.r], writes=[ib.r])
            self.ts('dve', tq.t[:], ib.t[:], 64.0, -64.0, ALU.is_ge, ALU.mult, reads=[ib.r, tq.r], writes=[tq.r])
            self.tt('dve', ib.t[:], ib.t[:], tq.t[:], ALU.add, reads=[ib.r, tq.r], writes=[ib.r])
            masks = {}
            for nm, op, val in (('US', ALU.is_gt, 1.0), ('LS', ALU.is_lt, 1.0), ('UI', ALU.is_ge, 1.0), ('LI', ALU.is_le, 1.0),
                                ('nUI', ALU.is_ge, -1.0), ('nLI', ALU.is_le, -1.0)):
                m = self.sb(sc, [128, 128], F32, 'mask' + nm)
                self.ts('dve', m.t[:], ib.t[:], ia.t[:, 0:1], val, op, ALU.mult, reads=[ib.r, ia.r], writes=[m.r])
                masks[nm] = m
            zeros = self.sb(sc, [128, C], F32, 'zeros')
            self.memset('pool', zeros.t[:], 0.0, writes=[zeros.r])
            G32 = [[self.sb(sc, [128, C], F32, 'G32') for j in range(8)] for d in range(2)]
            G16 = [[self.sb(sc, [128, C], BF16, 'G16') for j in range(8)] for d in range(2)]
            for d in range(2):
                for j in range(8):
                    self.memset('pool', G32[d][j].t[:], 0.0, writes=[G32[d][j].r])
                    self.memset('pool', G16[d][j].t[:], 0.0, writes=[G16[d][j].r])
            qd = [[self.sb(sc, [128, 5, 8, C], F32, 'qd') for _ in range(2)] for d in range(2)]
            v32 = [[self.sb(sc, [128, 8, C], F32, 'v32') for _ in range(2)] for d in range(2)]
            v16 = [[self.sb(sc, [128, 8, C], BF16, 'v16') for _ in range(2)] for d in range(2)]
            ych = [[self.sb(sc, [128, 8, C], F32, 'ych') for _ in range(2)] for d in range(2)]
            NB = 2

            def mat(dt, name):
                return [[[self.sb(sc, [128, 128], dt, name) for _ in range(NB)] for j in range(8)] for d in range(2)]
            KKe, REe, BBe, KEe = mat(BF16, 'KKe'), mat(BF16, 'REe'), mat(BF16, 'BBe'), mat(BF16, 'KEe')
            LkT, nMrbT, MrkT = mat(BF16, 'LkT'), mat(BF16, 'nMrbT'), mat(BF16, 'MrkT')
            KEt, nBBt = mat(BF16, 'KEt'), mat(BF16, 'nBBt')
            InvT = mat(F32, 'InvT')
            tot = [[[self.sb(sc, [128, 1], F32, 'tot') for _ in range(NB)] for j in range(8)] for d in range(2)]
            for arr in (KKe, REe, BBe, KEe):
                for d in range(2):
                    for j in range(8):
                        for b in range(NB):
                            self.memset('pool', arr[d][j][b].t[:], 0.0, writes=[arr[d][j][b].r])
            Qt = [self.sb(sc, [128, C + 1], F32, 'Qt') for _ in range(2)]
            Qi = [self.sb(sc, [128, C + 1], F32, 'Qi') for _ in range(2)]
            Pb = [self.sb(sc, [128, C + 1], F32, 'Pb') for _ in range(2)]
            Pv = [self.sb(sc, [128, C], F32, 'Pv') for _ in range(2)]
            Nm = [self.sb(sc, [128, 128], F32, 'Nm') for _ in range(2)]
            NTm = [self.sb(sc, [128, 128], F32, 'NTm') for _ in range(2)]
            Np = [self.sb(sc, [128, 128], F32, 'Np') for _ in range(4)]
            NTp = [self.sb(sc, [128, 128], F32, 'NTp') for _ in range(4)]
            Xs = [self.sb(sc, [128, 128], F32, 'Xs') for _ in range(4)]
            rhs_sb = [self.sb(sc, [128, C], F32, 'rhs_sb') for _ in range(4)]
            u16 = [self.sb(sc, [128, C], BF16, 'u16') for _ in range(4)]
            gtmp = [self.sb(sc, [128, C], F32, 'gtmp') for _ in range(4)]
            for q in Qt + Qi:
                self.memset('pool', q.t[:, 0:1], 1.0, writes=[q.r])
            cnt = {'s': 0, 'e': 0, 't': 0}

            def slot():
                s = slots[cnt['s'] % 8]
                cnt['s'] += 1
                return s

            def ev():
                cnt['e'] += 1
                return 'act' if cnt['e'] % 2 else 'dve'

            def chunk_ranges(c):
                if c < 4:
                    return (4144 + 64 * c, 4144 + 192 - 64 * c, 4096 + 64 * c, 4096 + 192 - 64 * c)
                cc = c - 4
                return (16 + 64 * cc, 16 + 4096 - 64 * (cc + 1), 64 * cc, 4096 - 64 * (cc + 1))

            def load_chunk(c):
                cf0, cb0, tf0, tb0 = chunk_ranges(c)
                for d, c0, t0 in ((0, cf0, tf0), (1, cb0, tb0)):
                    qb = qd[d][c % 2]
                    self.dma(qb.t[:, 0:2, :, :], SCN['ap'][0:2, :, :, c0:c0 + C].rearrange("q j p c -> p q j c"), reads=[SCN['r']], pwrites=[qb.r])
                    self.dma(qb.t[:, 2:5, :, :], SCN['ap'][2 + 3 * d:5 + 3 * d, :, :, c0:c0 + C].rearrange("q j p c -> p q j c"), reads=[SCN['r']], pwrites=[qb.r])
                    vb = v32[d][c % 2]
                    for h2 in range(2):
                        src = V32['ap'][t0:t0 + C, :].rearrange("s (j h v) -> s j h v", j=8, h=2)[:, :, h2, :]
                        self.dma(vb.t[h2 * 64:(h2 + 1) * 64, :, :], src, reads=[V32['r']], pwrites=[vb.r])
                    self.cp('act', v16[d][c % 2].t[:], vb.t[:], reads=[vb.r], writes=[v16[d][c % 2].r])

            def precompute(c, d, j):
                b = c % NB
                qb = qd[d][c % 2]
                k2 = cnt['t'] % 2
                cnt['t'] += 1
                qt, qi, pb_, pv = Qt[k2], Qi[k2], Pb[k2], Pv[k2]
                r_, kk_, w_, b_, k_ = (qb.t[:, qi_, j, :] for qi_ in range(5))
                self.P.add('dve', lambda e: e.tensor_tensor_scan(out=qt.t[:, 1:C + 1], data0=w_, data1=zeros.t[:], initial=1.0, op0=ALU.mult, op1=ALU.add),
                           reads=[qb.r, zeros.r], pwrites=[qt.r])
                self.recip(qi.t[:, 1:C + 1], qt.t[:, 1:C + 1], reads=[qt.r], pwrites=[qi.r])
                tt_ = tot[d][j][b]
                self.cp('dve', tt_.t[:], qt.t[:, C:C + 1], reads=[qt.r], writes=[tt_.r])
                if d == 0:
                    P_, Pp_, Pi_ = qt.t[:, 1:C + 1], qt.t[:, 0:C], qi.t[:, 1:C + 1]
                    rd = [qt.r, qi.r]
                else:
                    self.ts('dve', pb_.t[:], qi.t[:], qt.t[:, C:C + 1], None, ALU.mult, reads=[qi.r, qt.r], writes=[pb_.r])
                    self.ts('dve', pv.t[:], qt.t[:, 0:C], qi.t[:, C:C + 1], None, ALU.mult, reads=[qi.r, qt.r], writes=[pv.r])
                    P_, Pp_, Pi_ = pb_.t[:, 0:C], pb_.t[:, 1:C + 1], pv.t[:]
                    rd = [pb_.r, pv.r]
                n_ = 0
                for (dst, src, fac) in ((REe, r_, P_), (KKe, kk_, Pp_), (BBe, b_, Pi_), (KEe, k_, Pi_)):
                    for h2 in range(2):
                        ps_ = slice(h2 * 64, (h2 + 1) * 64)
                        eng = 'pool' if n_ % 2 else 'dve'
                        n_ += 1
                        self.tt(eng, dst[d][j][b].t[ps_, h2 * 64:(h2 + 1) * 64], src[ps_], fac[ps_], ALU.mult, reads=[qb.r] + rd, pwrites=[dst[d][j][b].r])
                kke, ree, bbe, kee = KKe[d][j][b], REe[d][j][b], BBe[d][j][b], KEe[d][j][b]
                mS_T, mS, mI = (('US', 'LS', 'UI') if d == 0 else ('LS', 'US', 'LI'))
                nm, ntm = Nm[k2], NTm[k2]
                for (lh, rh, mk_, dst_) in ((bbe, kke, masks[mS_T], nm), (kke, bbe, masks[mS], ntm), (kee, kke, masks[mS_T], LkT[d][j][b]),
                                            (bbe, ree, masks['n' + mI], nMrbT[d][j][b]), (kee, ree, masks[mI], MrkT[d][j][b])):
                    s = slot()
                    self.mm(s.t[:], lh.t[:], rh.t[:], True, True, reads=[lh.r, rh.r], writes=[s.r])
                    self.tt('dve', dst_.t[:], s.t[:], mk_.t[:], ALU.mult, reads=[s.r, mk_.r], writes=[dst_.r])
                for (src_, dst_, neg) in ((kee, KEt[d][j][b], False), (bbe, nBBt[d][j][b], True)):
                    s = slot()
                    pv_ = s.t[:].bitcast(BF16)[:, 0:128]
                    self.tr(pv_, src_.t[:], self.ident16.t[:], reads=[src_.r, self.ident16.r], writes=[s.r])
                    if neg:
                        self.act(dst_.t[:], pv_, AF.Copy, reads=[s.r], writes=[dst_.r], scale=-1.0)
                    else:
                        self.cp('act', dst_.t[:], pv_, reads=[s.r], writes=[dst_.r])
                x = Xs[(cnt['t'] * 2) % 4]
                self.tt('dve', x.t[:], self.ident32.t[:], nm.t[:], ALU.subtract, reads=[self.ident32.r, nm.r], writes=[x.r])
                curN, curNT = nm, ntm
                for lvl in range(5):
                    nNT = NTp[lvl % 4] if lvl < 4 else NTp[0]
                    s = slot()
                    self.mm(s.t[:], curN.t[:], curNT.t[:], True, True, reads=[curN.r, curNT.r], writes=[s.r])
                    self.cp(ev(), nNT.t[:], s.t[:], reads=[s.r], writes=[nNT.r])
                    if lvl < 4:
                        nN = Np[lvl % 4]
                        s = slot()
                        self.mm(s.t[:], curNT.t[:], curN.t[:], True, True, reads=[curN.r, curNT.r], writes=[s.r])
                        self.cp(ev(), nN.t[:], s.t[:], reads=[s.r], writes=[nN.r])
                    s = slot()
                    self.mm(s.t[:], nNT.t[:], x.t[:], True, True, reads=[nNT.r, x.r], writes=[s.r])
                    xn = InvT[d][j][b] if lvl == 4 else Xs[(cnt['t'] * 2 + 1 + lvl) % 4]
                    if xn is x:
                        xn = Xs[(cnt['t'] * 2 + 2 + lvl) % 4]
                    self.tt('dve', xn.t[:], s.t[:], x.t[:], ALU.add, reads=[s.r, x.r], writes=[xn.r])
                    x = xn
                    curNT = nNT
                    if lvl < 4:
                        curN = nN

            def sequential(c, d, j):
                b = c % NB
                g32, g16 = G32[d][j], G16[d][j]
                vj = v16[d][c % 2]
                k4 = cnt['e'] % 4
                rs, u_, gt_ = rhs_sb[k4], u16[k4], gtmp[k4]
                s = slot()
                self.mm(s.t[:, 0:C], KKe[d][j][b].t[:], g16.t[:], True, False, reads=[KKe[d][j][b].r, g16.r], writes=[s.r], inc=False)
                self.mm(s.t[:, 0:C], LkT[d][j][b].t[:], vj.t[:, j, :], False, True, reads=[LkT[d][j][b].r, vj.r], pwrites=[s.r])
                self.cp(ev(), rs.t[:], s.t[:, 0:C], reads=[s.r], writes=[rs.r])
                s = slot()
                self.mm(s.t[:, 0:C], InvT[d][j][b].t[:], rs.t[:], True, True, reads=[InvT[d][j][b].r, rs.r], writes=[s.r])
                self.cp(ev(), u_.t[:], s.t[:, 0:C], reads=[s.r], writes=[u_.r])
                s = slot()
                self.mm(s.t[:, 0:C], REe[d][j][b].t[:], g16.t[:], True, False, reads=[REe[d][j][b].r, g16.r], writes=[s.r], inc=False)
                self.mm(s.t[:, 0:C], nMrbT[d][j][b].t[:], u_.t[:], False, False, reads=[nMrbT[d][j][b].r, u_.r], pwrites=[s.r], inc=False)
                self.mm(s.t[:, 0:C], MrkT[d][j][b].t[:], vj.t[:, j, :], False, True, reads=[MrkT[d][j][b].r, vj.r], pwrites=[s.r])
                yc = ych[d][c % 2]
                self.cp(ev(), yc.t[:, j, :], s.t[:, 0:C], reads=[s.r], pwrites=[yc.r])
                s = slot()
                self.mm(s.t[:, 0:C], KEt[d][j][b].t[:], vj.t[:, j, :], True, False, reads=[KEt[d][j][b].r, vj.r], writes=[s.r], inc=False)
                self.mm(s.t[:, 0:C], nBBt[d][j][b].t[:], u_.t[:], False, True, reads=[nBBt[d][j][b].r, u_.r], pwrites=[s.r])
                self.tt('dve', gt_.t[:], s.t[:, 0:C], g32.t[:], ALU.add, reads=[s.r, g32.r], writes=[gt_.r])
                self.ts('dve', g32.t[:], gt_.t[:], tot[d][j][b].t[:, 0:1], None, ALU.mult, reads=[gt_.r, tot[d][j][b].r], writes=[g32.r])
                self.cp('act', g16.t[:], g32.t[:], reads=[g32.r], writes=[g16.r])

            load_chunk(0)
            for c in range(nch):
                if c + 1 < nch:
                    load_chunk(c + 1)
                for d in range(2):
                    for j in range(8):
                        precompute(c, d, j)
                for d in range(2):
                    for j in range(8):
                        sequential(c, d, j)
                cf0, cb0, tf0, tb0 = chunk_ranges(c)
                for d, t0 in ((0, tf0), (1, tb0)):
                    yc = ych[d][c % 2]
                    for h2 in range(2):
                        dst = YS['ap'][d, t0:t0 + C, :].rearrange("t (j h v) -> t j h v", j=8, h=2)[:, :, h2, :]
                        self.dma(dst, yc.t[h2 * 64:(h2 + 1) * 64, :, :], reads=[yc.r], pwrites=[YS['r']])
        self.P.barrier()

    def stage_escan_chunked2(self, SCN, V32, YS, nch=68):
        C = 64
        with contextlib.ExitStack() as sc:
            slots = [Tl(self.pp[i][:, s * 512:s * 512 + 128]) for i in range(4) for s in range(2)]
            ia_i = self.sb(sc, [128, 1], I32, 'ia_i')
            ia = self.sb(sc, [128, 1], F32, 'ia')
            ib_i = self.sb(sc, [128, 128], I32, 'ib_i')
            ib = self.sb(sc, [128, 128], F32, 'ib')
            tq = self.sb(sc, [128, 128], F32, 'tq')
            self.P.add('pool', lambda e: e.iota(ia_i.t[:], pattern=[[0, 1]], base=0, channel_multiplier=1), writes=[ia_i.r])
            self.cp('dve', ia.t[:], ia_i.t[:], reads=[ia_i.r], writes=[ia.r])
            self.ts('dve', tq.t[:, 0:1], ia.t[:], 64.0, -64.0, ALU.is_ge, ALU.mult, reads=[ia.r], writes=[tq.r])
            self.tt('dve', ia.t[:], ia.t[:], tq.t[:, 0:1], ALU.add, reads=[ia.r, tq.r], writes=[ia.r])
            self.P.add('pool', lambda e: e.iota(ib_i.t[:], pattern=[[1, 128]], base=0, channel_multiplier=0), writes=[ib_i.r])
            self.cp('dve', ib.t[:], ib_i.t[:], reads=[ib_i.r], writes=[ib.r])
            self.ts('dve', tq.t[:], ib.t[:], 64.0, -64.0, ALU.is_ge, ALU.mult, reads=[ib.r, tq.r], writes=[tq.r])
            self.tt('dve', ib.t[:], ib.t[:], tq.t[:], ALU.add, reads=[ib.r, tq.r], writes=[ib.r])
            masks = {}
            for nm, op, val in (('US', ALU.is_gt, 1.0), ('LS', ALU.is_lt, 1.0), ('UI', ALU.is_ge, 1.0), ('LI', ALU.is_le, 1.0),
                                ('nUI', ALU.is_ge, -1.0), ('nLI', ALU.is_le, -1.0)):
                m = self.sb(sc, [128, 128], F32, 'mask' + nm)
                self.ts('dve', m.t[:], ib.t[:], ia.t[:, 0:1], val, op, ALU.mult, reads=[ib.r, ia.r], writes=[m.r])
                masks[nm] = m
            zeros = self.sb(sc, [128, C], F32, 'zeros')
            self.memset('pool', zeros.t[:], 0.0, writes=[zeros.r])
            units = [(d, j) for d in range(2) for j in range(8)]

            def per_unit(shape, dt, name):
                return {u: self.sb(sc, shape, dt, name) for u in units}
            G32 = per_unit([128, C], F32, 'G32')
            G16 = per_unit([128, C], BF16, 'G16')
            KKe, REe, BBe, KEe = (per_unit([128, 128], BF16, n) for n in ('KKe', 'REe', 'BBe', 'KEe'))
            LkT, nMrbT, MrkT, KEt, nBBt, InvT = (per_unit([128, 128], BF16, n) for n in ('LkT', 'nMrbT', 'MrkT', 'KEt', 'nBBt', 'InvT'))
            Na, NTa, Nb, NTb, Xa, Xb = (per_unit([128, 128], BF16, n) for n in ('Na', 'NTa', 'Nb', 'NTb', 'Xa', 'Xb'))
            tot = per_unit([128, 1], F32, 'tot')
            Qt = per_unit([128, C + 1], F32, 'Qt')
            Qi = per_unit([128, C + 1], F32, 'Qi')
            Pb = {u: self.sb(sc, [128, C + 1], F32, 'Pb') for u in units if u[0] == 1}
            Pv = {u: self.sb(sc, [128, C], F32, 'Pv') for u in units if u[0] == 1}
            rs16 = per_unit([128, C], BF16, 'rs16')
            u16 = per_unit([128, C], BF16, 'u16')
            gtmp = per_unit([128, C], F32, 'gtmp')
            for u in units:
                self.memset('pool', G32[u].t[:], 0.0, writes=[G32[u].r])
                self.memset('pool', G16[u].t[:], 0.0, writes=[G16[u].r])
                for arr in (KKe, REe, BBe, KEe):
                    self.memset('pool', arr[u].t[:], 0.0, writes=[arr[u].r])
                self.memset('pool', Qt[u].t[:, 0:1], 1.0, writes=[Qt[u].r])
                self.memset('pool', Qi[u].t[:, 0:1], 1.0, writes=[Qi[u].r])
            qd = [[self.sb(sc, [128, 5, 8, C], F32, 'qd') for _ in range(2)] for d in range(2)]
            v32 = [[self.sb(sc, [128, 8, C], F32, 'v32') for _ in range(2)] for d in range(2)]
            v16 = [[self.sb(sc, [128, 8, C], BF16, 'v16') for _ in range(2)] for d in range(2)]
            ych = [[self.sb(sc, [128, 8, C], F32, 'ych') for _ in range(2)] for d in range(2)]
            cnt = {'s': 0, 'e': 0}

            def slot():
                s = slots[cnt['s'] % 8]
                cnt['s'] += 1
                return s

            def ev():
                cnt['e'] += 1
                return 'act' if cnt['e'] % 2 else 'dve'

            def chunk_ranges(c):
                if c < 4:
                    return (4144 + 64 * c, 4144 + 192 - 64 * c, 4096 + 64 * c, 4096 + 192 - 64 * c)
                cc = c - 4
                return (16 + 64 * cc, 16 + 4096 - 64 * (cc + 1), 64 * cc, 4096 - 64 * (cc + 1))

            def load_chunk(c):
                cf0, cb0, tf0, tb0 = chunk_ranges(c)
                for d, c0, t0 in ((0, cf0, tf0), (1, cb0, tb0)):
                    qb = qd[d][c % 2]
                    self.dma(qb.t[:, 0:2, :, :], SCN['ap'][0:2, :, :, c0:c0 + C].rearrange("q j p c -> p q j c"), reads=[SCN['r']], pwrites=[qb.r])
                    self.dma(qb.t[:, 2:5, :, :], SCN['ap'][2 + 3 * d:5 + 3 * d, :, :, c0:c0 + C].rearrange("q j p c -> p q j c"), reads=[SCN['r']], pwrites=[qb.r])
                    vb = v32[d][c % 2]
                    for h2 in range(2):
                        src = V32['ap'][t0:t0 + C, :].rearrange("s (j h v) -> s j h v", j=8, h=2)[:, :, h2, :]
                        self.dma(vb.t[h2 * 64:(h2 + 1) * 64, :, :], src, reads=[V32['r']], pwrites=[vb.r])
                    self.cp('act', v16[d][c % 2].t[:], vb.t[:], reads=[vb.r], writes=[v16[d][c % 2].r])

            def mm1(dst_slot, lh, rh, n=128):
                self.mm(dst_slot.t[:, 0:n], lh.t[:], rh if not isinstance(rh, Tl) else rh.t[:], True, True,
                        reads=[lh.r] + ([rh.r] if isinstance(rh, Tl) else []), writes=[dst_slot.r])

            load_chunk(0)
            for c in range(nch):
                if c + 1 < nch:
                    load_chunk(c + 1)
                for u in units:
                    d, j = u
                    qb = qd[d][c % 2]
                    qt, qi = Qt[u], Qi[u]
                    r_, kk_, w_, b_, k_ = (qb.t[:, qi_, j, :] for qi_ in range(5))
                    self.P.add('dve', lambda e, qt=qt, w_=w_: e.tensor_tensor_scan(out=qt.t[:, 1:C + 1], data0=w_, data1=zeros.t[:], initial=1.0, op0=ALU.mult, op1=ALU.add),
                               reads=[qb.r, zeros.r], pwrites=[qt.r])
                    self.recip(qi.t[:, 1:C + 1], qt.t[:, 1:C + 1], reads=[qt.r], pwrites=[qi.r])
                    self.cp('act', tot[u].t[:], qt.t[:, C:C + 1], reads=[qt.r], writes=[tot[u].r])
                    if d == 0:
                        P_, Pp_, Pi_ = qt.t[:, 1:C + 1], qt.t[:, 0:C], qi.t[:, 1:C + 1]
                        rd = [qt.r, qi.r]
                    else:
                        self.ts('dve', Pb[u].t[:], qi.t[:], qt.t[:, C:C + 1], None, ALU.mult, reads=[qi.r, qt.r], writes=[Pb[u].r])
                        self.ts('dve', Pv[u].t[:], qt.t[:, 0:C], qi.t[:, C:C + 1], None, ALU.mult, reads=[qi.r, qt.r], writes=[Pv[u].r])
                        P_, Pp_, Pi_ = Pb[u].t[:, 0:C], Pb[u].t[:, 1:C + 1], Pv[u].t[:]
                        rd = [Pb[u].r, Pv[u].r]
                    n_ = 0
                    for (dst, src, fac) in ((REe, r_, P_), (KKe, kk_, Pp_), (BBe, b_, Pi_), (KEe, k_, Pi_)):
                        for h2 in range(2):
                            ps_ = slice(h2 * 64, (h2 + 1) * 64)
                            eng = 'pool' if n_ % 2 else 'dve'
                            n_ += 1
                            self.tt(eng, dst[u].t[ps_, h2 * 64:(h2 + 1) * 64], src[ps_], fac[ps_], ALU.mult, reads=[qb.r] + rd, pwrites=[dst[u].r])
                for u in units:
                    d, j = u
                    mS_T, mS, mI = (('US', 'LS', 'UI') if d == 0 else ('LS', 'US', 'LI'))
                    for (lh, rh, mk_, dst_) in ((BBe[u], KKe[u], masks[mS_T], Na[u]), (KKe[u], BBe[u], masks[mS], NTa[u]), (KEe[u], KKe[u], masks[mS_T], LkT[u]),
                                                (BBe[u], REe[u], masks['n' + mI], nMrbT[u]), (KEe[u], REe[u], masks[mI], MrkT[u])):
                        s = slot()
                        mm1(s, lh, rh)
                        self.tt('dve', dst_.t[:], s.t[:, 0:128], mk_.t[:], ALU.mult, reads=[s.r, mk_.r], writes=[dst_.r])
                for u in units:
                    for (src_, dst_, neg) in ((KEe[u], KEt[u], False), (BBe[u], nBBt[u], True)):
                        s = slot()
                        pv_ = s.t[:, 0:128].bitcast(BF16)[:, 0:128]
                        self.tr(pv_, src_.t[:], self.ident16.t[:], reads=[src_.r, self.ident16.r], writes=[s.r])
                        if neg:
                            self.act(dst_.t[:], pv_, AF.Copy, reads=[s.r], writes=[dst_.r], scale=-1.0)
                        else:
                            self.cp('act', dst_.t[:], pv_, reads=[s.r], writes=[dst_.r])
                curN = {u: Na[u] for u in units}
                curNT = {u: NTa[u] for u in units}
                othN = {u: Nb[u] for u in units}
                othNT = {u: NTb[u] for u in units}
                X = {u: Xa[u] for u in units}
                Xo = {u: Xb[u] for u in units}
                for u in units:
                    self.tt('pool', X[u].t[:], self.ident16.t[:], curN[u].t[:], ALU.subtract, reads=[self.ident16.r, curN[u].r], writes=[X[u].r])
                for lvl in range(5):
                    for u in units:
                        s = slot()
                        mm1(s, curN[u], curNT[u])
                        self.cp(ev(), othNT[u].t[:], s.t[:, 0:128], reads=[s.r], writes=[othNT[u].r])
                        if lvl < 4:
                            s = slot()
                            mm1(s, curNT[u], curN[u])
                            self.cp(ev(), othN[u].t[:], s.t[:, 0:128], reads=[s.r], writes=[othN[u].r])
                    for u in units:
                        s = slot()
                        self.mm(s.t[:, 0:128], othNT[u].t[:], X[u].t[:], True, False, reads=[othNT[u].r, X[u].r], writes=[s.r], inc=False)
                        self.mm(s.t[:, 0:128], self.ident16.t[:], X[u].t[:], False, True, reads=[self.ident16.r, X[u].r], pwrites=[s.r])
                        xn = InvT[u] if lvl == 4 else Xo[u]
                        self.cp(ev(), xn.t[:], s.t[:, 0:128], reads=[s.r], writes=[xn.r])
                        if lvl < 4:
                            X[u], Xo[u] = Xo[u], X[u]
                    for u in units:
                        curN[u], othN[u] = othN[u], curN[u]
                        curNT[u], othNT[u] = othNT[u], curNT[u]
                for u in units:
                    d, j = u
                    vj = v16[d][c % 2]
                    s = slot()
                    self.mm(s.t[:, 0:C], KKe[u].t[:], G16[u].t[:], True, False, reads=[KKe[u].r, G16[u].r], writes=[s.r], inc=False)
                    self.mm(s.t[:, 0:C], LkT[u].t[:], vj.t[:, j, :], False, True, reads=[LkT[u].r, vj.r], pwrites=[s.r])
                    self.cp(ev(), rs16[u].t[:], s.t[:, 0:C], reads=[s.r], writes=[rs16[u].r])
                for u in units:
                    s = slot()
                    self.mm(s.t[:, 0:C], InvT[u].t[:], rs16[u].t[:], True, True, reads=[InvT[u].r, rs16[u].r], writes=[s.r])
                    self.cp(ev(), u16[u].t[:], s.t[:, 0:C], reads=[s.r], writes=[u16[u].r])
                for u in units:
                    d, j = u
                    vj = v16[d][c % 2]
                    s = slot()
                    self.mm(s.t[:, 0:C], REe[u].t[:], G16[u].t[:], True, False, reads=[REe[u].r, G16[u].r], writes=[s.r], inc=False)
                    self.mm(s.t[:, 0:C], nMrbT[u].t[:], u16[u].t[:], False, False, reads=[nMrbT[u].r, u16[u].r], pwrites=[s.r], inc=False)
                    self.mm(s.t[:, 0:C], MrkT[u].t[:], vj.t[:, j, :], False, True, reads=[MrkT[u].r, vj.r], pwrites=[s.r])
                    yc = ych[d][c % 2]
                    self.cp('act', yc.t[:, j, :], s.t[:, 0:C], reads=[s.r], pwrites=[yc.r])
                    s = slot()
                    self.mm(s.t[:, 0:C], KEt[u].t[:], vj.t[:, j, :], True, False, reads=[KEt[u].r, vj.r], writes=[s.r], inc=False)
                    self.mm(s.t[:, 0:C], nBBt[u].t[:], u16[u].t[:], False, True, reads=[nBBt[u].r, u16[u].r], pwrites=[s.r])
                    self.tt('dve', gtmp[u].t[:], s.t[:, 0:C], G32[u].t[:], ALU.add, reads=[s.r, G32[u].r], writes=[gtmp[u].r])
                    self.ts('dve', G32[u].t[:], gtmp[u].t[:], tot[u].t[:, 0:1], None, ALU.mult, reads=[gtmp[u].r, tot[u].r], writes=[G32[u].r])
                    self.cp('act', G16[u].t[:], G32[u].t[:], reads=[G32[u].r], writes=[G16[u].r])
                cf0, cb0, tf0, tb0 = chunk_ranges(c)
                for d, t0 in ((0, tf0), (1, tb0)):
                    yc = ych[d][c % 2]
                    for h2 in range(2):
                        dst = YS['ap'][d, t0:t0 + C, :].rearrange("t (j h v) -> t j h v", j=8, h=2)[:, :, h2, :]
                        self.dma(dst, yc.t[h2 * 64:(h2 + 1) * 64, :, :], reads=[yc.r], pwrites=[YS['r']])
        self.P.barrier()


WNAMES = ['ada_w', 'ada_b', 'ln1_g', 'ln1_b', 'ln2_g', 'ln2_b', 'even_w_in', 'even_w_out',
          'rw_mu', 'rw_w0', 'rw_w_up', 'rw_a0', 'rw_a_up', 'rw_g_up', 'rw_kk', 'rw_ka', 'rw_rk', 'rw_gn_g', 'rw_gn_b',
          'cv_w', 'cv_b', 'cv_ln_g', 'cv_ln_b', 'odd_w_in', 'odd_w_out', 'q_norm', 'k_norm',
          'moe_router', 'moe_bias', 'moe_w1', 'moe_w3', 'moe_w2', 'sh_w1', 'sh_w3', 'sh_w2']
WSHAPES = {'ada_w': (4, 2048, 12288), 'ada_b': (4, 12288), 'ln1_g': (4, 2048), 'ln1_b': (4, 2048), 'ln2_g': (4, 2048), 'ln2_b': (4, 2048),
           'even_w_in': (2, 2048, 5568), 'even_w_out': (2, 2048, 2048), 'rw_mu': (2, 2, 3520), 'rw_w0': (2, 2, 1024),
           'rw_w_up': (2, 2, 96, 1024), 'rw_a0': (2, 2, 1024), 'rw_a_up': (2, 2, 96, 1024), 'rw_g_up': (2, 64, 1024),
           'rw_kk': (2, 1024), 'rw_ka': (2, 1024), 'rw_rk': (2, 16, 64), 'rw_gn_g': (2, 1024), 'rw_gn_b': (2, 1024),
           'cv_w': (2, 31, 1024), 'cv_b': (2, 1024), 'cv_ln_g': (2, 1024), 'cv_ln_b': (2, 1024),
           'odd_w_in': (2, 2048, 3072), 'odd_w_out': (2, 2048, 2048), 'q_norm': (2, 128), 'k_norm': (2, 128),
           'moe_router': (4, 2048, 64), 'moe_bias': (4, 64), 'moe_w1': (4, 64, 2048, 384), 'moe_w3': (4, 64, 2048, 384),
           'moe_w2': (4, 64, 384, 2048), 'sh_w1': (4, 2048, 384), 'sh_w3': (4, 2048, 384), 'sh_w2': (4, 384, 2048)}


def host_consts():
    tok = np.arange(4096)
    row = tok // 64
    col = tok % 64
    inv = 10000.0 ** (-np.arange(32, dtype=np.float32) / 32)
    ang = np.zeros((128, 4096), np.float32)
    for a, pos in enumerate((row, col)):
        for half in range(2):
            ang[a * 64 + half * 32:a * 64 + half * 32 + 32, :] = inv[:, None] * pos[None, :].astype(np.float32)
    cosT = np.cos(ang).astype(np.float32)
    sinT = np.sin(ang).astype(np.float32)
    rotT = np.zeros((128, 128), np.float32)
    for m in range(128):
        half = (m % 64) // 32
        if half == 0:
            rotT[m + 32, m] = -1.0
        else:
            rotT[m - 32, m] = 1.0
    return {'cosT': cosT, 'sinT': sinT, 'rotT': rotT}


def build_program(plan=None, dbg_out=(), dbg_in=(), declare=None):
    nc = bass.Bass("TRN2", target_bir_lowering=False)
    mk = MK(nc, dbg_out)
    ins = {}
    for k in WNAMES:
        if declare is not None and k not in declare:
            continue
        ins[k] = nc.dram_tensor(k, list(WSHAPES[k]), F32, kind="ExternalInput").ap()
    xin = nc.dram_tensor("xin", [T, D], F32, kind="ExternalInput").ap()
    cvec = nc.dram_tensor("cvec", [2, D], F32, kind="ExternalInput").ap()
    cosT = nc.dram_tensor("cosT", [128, 4096], F32, kind="ExternalInput").ap()
    sinT = nc.dram_tensor("sinT", [128, 4096], F32, kind="ExternalInput").ap()
    rotT = nc.dram_tensor("rotT", [128, 128], F32, kind="ExternalInput").ap()
    out = nc.dram_tensor("out", [4096, D], F32, kind="ExternalOutput").ap()

    def scratch(name, shape, dt, nres=None):
        kind = "ExternalInput" if name in dbg_in else None
        ap = mk.dram(name, shape, dt, kind=kind)
        if nres is None:
            return {'ap': ap, 'r': Res(name)}
        return {'ap': ap, 'r': [Res(name) for _ in range(nres)]}
    with mk.top:
        mk.setup()
        H = {'ap': xin, 'r': [Res() for _ in range(NT)]}
        Hs = scratch('H', [T, D], F32, NT)
        Y = scratch('Y', [T, D], F32, NT)
        Fm = scratch('F', [T, D], F32, NT)
        MODB = scratch('MODB', [4, 2, 128, 12288], F32)
        QT = scratch('QT', [20, 128, T], BF16)
        Vd = scratch('V', [T, 512], BF16)
        OT = scratch('OT', [16, 128, T], BF16)
        OUT = {'ap': out, 'r': [Res() for _ in range(NT)]}
        PT = scratch('PT', [45, 128, TP], F32)
        WB = scratch('WB', [65 * 128, 12288], BF16)
        WB['ap2'] = mk.dram('WB2', [65 * 128, 6144], BF16)
        VF = scratch('VF', [T, D], BF16)
        TOKBUF = scratch('TOKBUF', [115 * 512, 16], I32)
        YB = scratch('YB', [115 * 512, D], BF16)
        YB['ap2'] = mk.dram('YSHB', [T, D], BF16)
        SCN = scratch('SCN', [8, 8, 128, TP], F32)
        VH = scratch('VH', [2, T, 512], BF16)
        V32 = scratch('V32', [T, 1024], F32)
        G = scratch('G', [T, 1024], F32)
        RK = scratch('RK', [T, 32], F32)
        YS = scratch('YS', [2, T, 1024], F32)
        CAT = scratch('CAT', [T, 2048], BF16)

        def EW(j):
            return {'mu': ins['rw_mu'][j], 'w0': ins['rw_w0'][j], 'w_up': ins['rw_w_up'][j], 'a0': ins['rw_a0'][j], 'a_up': ins['rw_a_up'][j],
                    'g_up': ins['rw_g_up'][j], 'kk': ins['rw_kk'][j], 'ka': ins['rw_ka'][j], 'rk': ins['rw_rk'][j],
                    'gn_g': ins['rw_gn_g'][j], 'gn_b': ins['rw_gn_b'][j], 'cv_w': ins['cv_w'][j], 'cv_b': ins['cv_b'][j],
                    'cv_ln_g': ins['cv_ln_g'][j], 'cv_ln_b': ins['cv_ln_b'][j]}
        if plan is None:
            plan = full_plan()
        for (stg, l) in plan:
            j = l // 2 if isinstance(l, int) else 0
            last = (l == 3)
            if stg == 'copyin':
                with contextlib.ExitStack() as sc:
                    hb = [mk.sb(sc, [128, D], F32, 'cp') for _ in range(2)]
                    for ti in range(NT):
                        mk.dma(hb[ti % 2].t[:], xin[ti * 128:(ti + 1) * 128, :], writes=[hb[ti % 2].r])
                        mk.dma(Hs['ap'][ti * 128:(ti + 1) * 128, :], hb[ti % 2].t[:], reads=[hb[ti % 2].r], writes=[Hs['r'][ti]])
                mk.P.barrier()
            elif stg == 'mod':
                mk.stage_mod(cvec, ins['ada_w'], ins['ada_b'], MODB, layers=l if isinstance(l, (list, range)) else [l])
            elif stg == 'moe':
                mk.stage_moe(Hs, Fm, MODB, l, ins['moe_router'][l], ins['moe_bias'][l], ins['moe_w1'], ins['moe_w3'], ins['moe_w2'],
                             ins['sh_w1'], ins['sh_w3'], ins['sh_w2'], with_ctx=not last)
            elif stg == 'moeconv':
                mk.stage_moe_conv(l, ins['moe_w1'], ins['moe_w3'], ins['moe_w2'], ins['sh_w1'], ins['sh_w3'], ins['sh_w2'], WB)
            elif stg == 'moes':
                mk.stage_moe_sparse(Hs, Fm, MODB, l, ins['moe_router'][l], ins['moe_bias'][l], WB, VF, TOKBUF, YB, with_ctx=not last)
            elif stg == 'ln1':
                tiles = range(32) if last else range(NT)
                mk.stage_ln(Hs, Y, MODB, l, 4096, ins['ln1_g'][l], ins['ln1_b'][l], tiles)
            elif stg == 'ln2':
                tiles = range(32) if last else range(NT)
                mk.stage_ln(Hs, Fm, MODB, l, 10240, ins['ln2_g'][l], ins['ln2_b'][l], tiles, OUTF=OUT if last else None)
            elif stg == 'qkv':
                mk.stage_qkv(Hs, MODB, l, ins['odd_w_in'][j], ins['q_norm'][j], ins['k_norm'][j], cosT, sinT, rotT, QT, Vd)
            elif stg == 'attn':
                mk.stage_attn(QT, Vd, OT, with_ctx=not last)
            elif stg == 'oproj':
                mk.stage_proj_T(OT, ins['odd_w_out'][j], Y, with_ctx=not last)
            elif stg == 'zpad':
                mk.stage_zpad(PT)
            elif stg == 'ein':
                mk.stage_ein(Hs, MODB, l, ins['even_w_in'][j], PT)
            elif stg == 'efeat':
                mk.stage_efeat(j, EW(j), PT, SCN, VH, V32, G, RK)
            elif stg == 'escan':
                mk.stage_escan(SCN, VH, YS)
            elif stg == 'escanc':
                mk.stage_escan_chunked(SCN, V32, YS)
            elif stg == 'escanc2':
                mk.stage_escan_chunked2(SCN, V32, YS)
            elif stg == 'eepi':
                mk.stage_eepi(EW(j), YS, V32, G, RK, CAT)
            elif stg == 'econv':
                mk.stage_econv(EW(j), PT, CAT)
            elif stg == 'eproj':
                mk.stage_eproj(CAT, ins['even_w_out'][j], Y)
            else:
                raise ValueError(stg)
        fin = [r for r in OUT['r']]
        for d in (Hs, Y, Fm):
            fin += d['r']
        fin += [WB['r'], VF['r'], TOKBUF['r'], YB['r'], MODB['r'], QT['r'], Vd['r'], OT['r'], PT['r'], SCN['r'], VH['r'], V32['r'], G['r'], RK['r'], YS['r'], CAT['r']]
        mk.P.finish(fin)
        mk.P.emit()
    return nc


def full_plan():
    plan = [('copyin', 0), ('zpad', 0), ('mod', range(4))]
    for l in range(4):
        if l % 2 == 0:
            plan += [('ein', l), ('efeat', l), ('escanc2', l), ('eepi', l), ('econv', l), ('eproj', l)]
        else:
            plan += [('qkv', l), ('attn', l), ('oproj', l)]
        plan += [('ln1', l), ('moeconv', l), ('moes', l), ('ln2', l)]
    return plan


_CACHE = {}


def kernel(**inputs):
    n = 8
    if 'nc' not in _CACHE:
        _CACHE['nc'] = build_program()
    nc = _CACHE['nc']
    consts = host_consts()
    shared = {k: np.ascontiguousarray(np.asarray(inputs[k], dtype=np.float32)) for k in WNAMES}
    x = np.asarray(inputs['x'], dtype=np.float32)
    ctx = np.asarray(inputs['ctx'], dtype=np.float32)
    c = np.asarray(inputs['c'], dtype=np.float32)
    c_ctx = np.asarray(inputs['c_ctx'], dtype=np.float32)
    in_maps = []
    for b in range(n):
        m = dict(shared)
        m.update(consts)
        m['xin'] = np.ascontiguousarray(np.concatenate([x[b], ctx[b]], axis=0))
        m['cvec'] = np.ascontiguousarray(np.stack([c[b], c_ctx], axis=0))
        in_maps.append(m)
    res = run_bass_kernel_spmd(nc, in_maps, core_ids=list(range(n)))
    return np.stack([res.results[b]['out'] for b in range(n)], axis=0).astype(np.float32)
```

```python
import numpy as np
import concourse.bass as bass
import concourse.mybir as mybir
from concourse.bass_utils import run_bass_kernel_spmd

F32 = mybir.dt.float32
BF16 = mybir.dt.bfloat16
I32 = mybir.dt.int32
U32 = mybir.dt.uint32
ALU = mybir.AluOpType
AF = mybir.ActivationFunctionType
AX = mybir.AxisListType

ENGS = ['pe', 'act', 'dve', 'pool', 'sp']
NSLOT = 8


class Res:
    __slots__ = ('name', 'w', 'r')

    def __init__(self, name=''):
        self.name = name
        self.w = {}
        self.r = {}


def _merge(dst, src):
    for k, v in src.items():
        if dst.get(k, 0) < v:
            dst[k] = v


class Prog:
    def __init__(self, nc):
        self.nc = nc
        self.ops = {e: [] for e in ENGS}
        self.cnt = {e: 0 for e in ENGS}
        self.known = {e: {} for e in ENGS}
        self.ndma = {e: 0 for e in ENGS}
        self.sems = {}
        self.nops = 0

    def add(self, eng, fn, reads=(), writes=(), pwrites=(), inc=True, dma=False):
        waits = {}
        for r in reads:
            _merge(waits, r.w)
        for w in writes:
            _merge(waits, w.w)
            _merge(waits, w.r)
        for w in pwrites:
            _merge(waits, w.w)
            _merge(waits, w.r)
        own = 'c_' + eng
        if own in waits and waits[own] > self.cnt[eng]:
            waits[own] = self.cnt[eng]
        if dma:
            n = self.ndma[eng]
            self.ndma[eng] = n + 1
            slot = 'd_%s_%d' % (eng, n % NSLOT)
            rnd = n // NSLOT
            if rnd > 0:
                _merge(waits, {slot: 16 * rnd})
            ev = (slot, 16 * (rnd + 1))
            incv = 16
        else:
            key = 'c_' + eng
            if inc:
                self.cnt[eng] += 1
                ev = (key, self.cnt[eng])
            else:
                ev = (key, self.cnt[eng] + 1)
            incv = 1
        kn = self.known[eng]
        wl = []
        for k, v in waits.items():
            if kn.get(k, 0) < v:
                kn[k] = v
                wl.append((k, v))
        for r in reads:
            if r.r.get(ev[0], 0) < ev[1]:
                r.r[ev[0]] = ev[1]
        for w in writes:
            w.w = {ev[0]: ev[1]}
            w.r = {}
        for w in pwrites:
            if w.w.get(ev[0], 0) < ev[1]:
                w.w[ev[0]] = ev[1]
        self.ops[eng].append((wl, fn, ev if (inc or dma) else None, incv))
        self.nops += 1

    def finish(self, reslist, eng='sp'):
        waits = {}
        for r in reslist:
            _merge(waits, r.w)
        self.ops[eng].append((list(waits.items()), None, None, 0))

    def emit(self):
        nc = self.nc
        import contextlib
        names = set()
        for e in ENGS:
            for wl, fn, ev, incv in self.ops[e]:
                for k, v in wl:
                    names.add(k)
                if ev is not None:
                    names.add(ev[0])
        with contextlib.ExitStack() as st:
            sems = {k: st.enter_context(nc.semaphore(k)) for k in sorted(names)}
            block = st.enter_context(nc.Block())

            def run(e):
                def body(eng):
                    for wl, fn, ev, incv in self.ops[e]:
                        for k, v in wl:
                            eng.wait_ge(sems[k], v)
                        if fn is not None:
                            ins = fn(eng)
                            if ev is not None:
                                ins.then_inc(sems[ev[0]], incv)
                return body
            block.tensor(run('pe'))
            block.scalar(run('act'))
            block.vector(run('dve'))
            block.gpsimd(run('pool'))
            block.sync(run('sp'))

    def barrier(self):
        tgt = {}
        for e in ENGS:
            if self.cnt[e] > 0:
                tgt['c_' + e] = self.cnt[e]
            n = self.ndma[e]
            for s in range(min(n, NSLOT)):
                last = n - 1 - ((n - 1 - s) % NSLOT)
                tgt['d_%s_%d' % (e, s)] = 16 * (last // NSLOT + 1)
        for e in ENGS:
            kn = self.known[e]
            wl = []
            for k, v in tgt.items():
                if kn.get(k, 0) < v:
                    kn[k] = v
                    wl.append((k, v))
            if wl:
                self.ops[e].append((wl, None, None, 0))


import contextlib
import math

T = 4352
NT = 34
D = 2048
TP = 4416
BLOCKS = [(512 * b, 512, 16 + 512 * b, 0) for b in range(8)] + [(4096, 256, 4144, 1)]
ALPHA = 8 ** 0.25
EGROUPS = [(i * 128, 128) for i in range(24)] + [(3072, 64), (3136, 96), (3232, 96), (3328, 96), (3424, 96)] \
    + [(3520 + i * 128, 128) for i in range(16)]
ESLABS = [list(range(4 * i, 4 * i + 4)) for i in range(6)] + [[24, 25, 26, 27, 28]] \
    + [list(range(29 + 4 * i, 33 + 4 * i)) for i in range(4)]


class Tl:
    __slots__ = ('t', 'r')

    def __init__(self, t):
        self.t = t
        self.r = Res()


class MK:
    def __init__(self, nc, dbg_out=()):
        self.nc = nc
        self.P = Prog(nc)
        self.uid = 0
        self.dbg_out = set(dbg_out)
        self.top = contextlib.ExitStack()
        self.outs = []

    def sb(self, scope, shape, dt, name='t'):
        self.uid += 1
        return Tl(scope.enter_context(self.nc.sbuf_tensor('%s_%d' % (name, self.uid), list(shape), dt)))

    def dram(self, name, shape, dt, kind=None):
        if kind is None:
            kind = "ExternalOutput" if name in self.dbg_out else "Internal"
        return self.nc.dram_tensor(name, list(shape), dt, kind=kind).ap()

    def dma(self, out, in_, reads=(), writes=(), pwrites=(), q='sp', **kw):
        self.P.add(q, lambda e: e.dma_start(out=out, in_=in_, **kw), reads=reads, writes=writes, pwrites=pwrites, dma=True)

    def mm(self, out, lhsT, rhs, start, stop, reads=(), writes=(), pwrites=(), inc=True):
        self.P.add('pe', lambda e: e.matmul(out, lhsT=lhsT, rhs=rhs, start=start, stop=stop),
                   reads=reads, writes=writes, pwrites=pwrites, inc=inc)

    def tr(self, out, in_, ident, reads=(), writes=(), pwrites=(), inc=True):
        self.P.add('pe', lambda e: e.transpose(out=out, in_=in_, identity=ident),
                   reads=reads, writes=writes, pwrites=pwrites, inc=inc)

    def act(self, out, in_, func, reads=(), writes=(), pwrites=(), **kw):
        self.P.add('act', lambda e: e.activation(out=out, in_=in_, func=func, **kw), reads=reads, writes=writes, pwrites=pwrites)

    def tt(self, eng, out, in0, in1, op, reads=(), writes=(), pwrites=()):
        self.P.add(eng, lambda e: e.tensor_tensor(out=out, in0=in0, in1=in1, op=op), reads=reads, writes=writes, pwrites=pwrites)

    def ts(self, eng, out, in0, s1, s2, op0, op1=None, reads=(), writes=(), pwrites=()):
        if op1 is None:
            self.P.add(eng, lambda e: e.tensor_scalar(out=out, in0=in0, scalar1=s1, scalar2=None, op0=op0),
                       reads=reads, writes=writes, pwrites=pwrites)
        else:
            self.P.add(eng, lambda e: e.tensor_scalar(out=out, in0=in0, scalar1=s1, scalar2=s2, op0=op0, op1=op1),
                       reads=reads, writes=writes, pwrites=pwrites)

    def stt(self, eng, out, in0, scalar, in1, op0, op1, reads=(), writes=(), pwrites=()):
        self.P.add(eng, lambda e: e.scalar_tensor_tensor(out=out, in0=in0, scalar=scalar, in1=in1, op0=op0, op1=op1),
                   reads=reads, writes=writes, pwrites=pwrites)

    def cp(self, eng, out, in_, reads=(), writes=(), pwrites=()):
        if eng == 'act':
            self.P.add(eng, lambda e: e.copy(out=out, in_=in_), reads=reads, writes=writes, pwrites=pwrites)
        else:
            self.P.add(eng, lambda e: e.tensor_copy(out=out, in_=in_), reads=reads, writes=writes, pwrites=pwrites)

    def memset(self, eng, ap, val, writes=(), pwrites=()):
        self.P.add(eng, lambda e: e.memset(ap, val), writes=writes, pwrites=pwrites)

    def recip(self, out, in_, reads=(), writes=(), pwrites=()):
        self.P.add('dve', lambda e: e.reciprocal(out=out, in_=in_), reads=reads, writes=writes, pwrites=pwrites)

    def setup(self):
        nc = self.nc
        top = self.top
        self.pb = []
        self.pp = []
        for i in range(4):
            pair = top.enter_context(nc.psum_tensor('pp%d' % i, [128, 1024], F32))
            self.pp.append(pair)
            for hh in range(2):
                self.pb.append(Tl(pair[:, hh * 512:(hh + 1) * 512]))
        self.ident32 = self.sb(top, [128, 128], F32, 'ident32')
        self.ident16 = self.sb(top, [128, 128], BF16, 'ident16')
        self.ones16 = self.sb(top, [128, 128], BF16, 'ones16')
        self.ones32 = self.sb(top, [128, 128], F32, 'ones32')
        self.bones32 = self.sb(top, [128, 128], F32, 'bones32')
        self.bones16 = self.sb(top, [128, 128], BF16, 'bones16')
        self.bind32 = self.sb(top, [128, 2], F32, 'bind32')
        i32, i16 = self.ident32, self.ident16
        self.memset('pool', i32.t[:], 1.0, writes=[i32.r])
        self.P.add('pool', lambda e: e.affine_select(out=i32.t[:], in_=i32.t[:], pattern=[[-1, 128]], compare_op=ALU.is_equal,
                                                     fill=0.0, base=0, channel_multiplier=1), reads=[i32.r], writes=[i32.r])
        self.cp('dve', i16.t[:], i32.t[:], reads=[i32.r], writes=[i16.r])
        self.memset('pool', self.ones16.t[:], 1.0, writes=[self.ones16.r])
        self.memset('pool', self.ones32.t[:], 1.0, writes=[self.ones32.r])
        b32 = self.bones32
        self.memset('pool', b32.t[:], 0.0, writes=[b32.r])
        self.memset('pool', b32.t[0:64, 0:64], 1.0, pwrites=[b32.r])
        self.memset('pool', b32.t[64:128, 64:128], 1.0, pwrites=[b32.r])
        self.cp('dve', self.bones16.t[:], b32.t[:], reads=[b32.r], writes=[self.bones16.r])
        self.eps5 = self.sb(top, [128, 1], F32, 'eps5')
        self.epsq = self.sb(top, [128, 1], F32, 'epsq')
        self.epsg = self.sb(top, [128, 1], F32, 'epsg')
        self.eps12 = self.sb(top, [128, 1], F32, 'eps12')
        self.memset('pool', self.eps5.t[:], 1e-5, writes=[self.eps5.r])
        self.memset('pool', self.epsq.t[:], 1e-6, writes=[self.epsq.r])
        self.memset('pool', self.epsg.t[:], 64e-5, writes=[self.epsg.r])
        self.memset('pool', self.eps12.t[:], 1e-12, writes=[self.eps12.r])
        bi = self.bind32
        self.memset('pool', bi.t[:], 0.0, writes=[bi.r])
        self.memset('pool', bi.t[0:64, 0:1], 1.0, pwrites=[bi.r])
        self.memset('pool', bi.t[64:128, 1:2], 1.0, pwrites=[bi.r])

    def stage_mod(self, cvec, ada_w, ada_b, MODB, layers=range(4)):
        with contextlib.ExitStack() as sc:
            cT = self.sb(sc, [128, 2, 16], F32, 'cT')
            sil = self.sb(sc, [128, 2, 16], F32, 'sil')
            silb = self.sb(sc, [128, 2, 16, 128], BF16, 'silb')
            self.dma(cT.t[:], cvec.rearrange("w (c p) -> p w c", p=128), writes=[cT.r], allow_slow_non_contiguous=True)
            self.act(sil.t[:], cT.t[:], AF.Silu, reads=[cT.r], writes=[sil.r])
            self.cp('dve', silb.t[:], sil.t[:].unsqueeze(3).to_broadcast([128, 2, 16, 128]), reads=[sil.r], writes=[silb.r])
            wsl = [self.sb(sc, [128, 16, 512], BF16, 'wsl') for _ in range(2)]
            bias = [self.sb(sc, [128, 512], F32, 'bias') for _ in range(2)]
            ot = [self.sb(sc, [128, 512], F32, 'ot') for _ in range(4)]
            it = 0
            for l in layers:
                for nb in range(24):
                    w = wsl[it % 2]
                    bt = bias[it % 2]
                    self.dma(w.t[:], ada_w[l, :, nb * 512:(nb + 1) * 512].rearrange("(c p) n -> p c n", p=128), writes=[w.r], q='pool')
                    self.dma(bt.t[:], ada_b[l, nb * 512:(nb + 1) * 512].partition_broadcast(128), writes=[bt.r])
                    for which in range(2):
                        pbk = self.pb[(it * 2 + which) % 4]
                        for kc in range(16):
                            self.mm(pbk.t[:], silb.t[:, which, kc, :], w.t[:, kc, :], kc == 0, kc == 15,
                                    reads=[silb.r, w.r], writes=[pbk.r] if kc == 0 else [], pwrites=[] if kc == 0 else [pbk.r], inc=(kc == 15))
                        o = ot[(it * 2 + which) % 4]
                        is_sc = (nb // 4) in (1, 4)
                        self.stt('dve', o.t[:], pbk.t[:], 1.0 if is_sc else 0.0, bt.t[:], ALU.add, ALU.add,
                                 reads=[pbk.r, bt.r], writes=[o.r])
                        self.dma(MODB['ap'][l, which, :, nb * 512:(nb + 1) * 512], o.t[:], reads=[o.r], pwrites=[MODB['r']])
                    it += 1
        self.P.barrier()

    def load_mods(self, sc, MODB, l, offs):
        res = {}
        for off in offs:
            t = self.sb(sc, [128, 2, 2048], F32, 'mod')
            for which in range(2):
                self.dma(t.t[:, which, :], MODB['ap'][l, which, :, off:off + 2048], reads=[MODB['r']], pwrites=[t.r])
            res[off] = t
        return res

    def load_bcast(self, sc, vec_ap, n, name='bc'):
        t = self.sb(sc, [128, n], F32, name)
        self.dma(t.t[:], vec_ap.partition_broadcast(128), writes=[t.r])
        return t

    def make_modT(self, sc_bufs, H, blk, mods_sc, mods_sh, uT, tile_off=0, also32=None):
        tok0, ntok, col0, which = blk
        for i in range(ntok // 128):
            ti = tok0 // 128 + i
            k = sc_bufs['n']
            sc_bufs['n'] += 1
            hb = sc_bufs['hb'][k % 2]
            tmp = sc_bufs['tmp'][k % 2]
            ub = sc_bufs['ub'][k % 2]
            self.dma(hb.t[:], H['ap'][ti * 128:(ti + 1) * 128, :], reads=[H['r'][ti]], writes=[hb.r])
            self.tt('dve', tmp.t[:], hb.t[:], mods_sc.t[:, which, :], ALU.mult, reads=[hb.r, mods_sc.r], writes=[tmp.r])
            self.tt('pool', ub.t[:], tmp.t[:], mods_sh.t[:, which, :], ALU.add, reads=[tmp.r, mods_sh.r], writes=[ub.r])
            if also32 is not None:
                also32(i, ti, tmp, mods_sh, which)
            for half in range(2):
                pbk = self.pb[sc_bufs['pbase'] + half]
                pv = pbk.t[:].bitcast(BF16)
                for q in range(8):
                    kc = half * 8 + q
                    self.tr(pv[:, q * 128:(q + 1) * 128], ub.t[:, kc * 128:(kc + 1) * 128], self.ident16.t[:],
                            reads=[ub.r, self.ident16.r], writes=[pbk.r] if q == 0 else [], pwrites=[] if q == 0 else [pbk.r], inc=(q == 7))
                c0 = tile_off + i * 128
                self.cp('act', uT.t[:, half * 8:(half + 1) * 8, c0:c0 + 128], pv.rearrange("p (q k) -> p q k", q=8),
                        reads=[pbk.r], pwrites=[uT.r])

    def modT_bufs(self, sc, pbase=6):
        return {'n': 0, 'pbase': pbase,
                'hb': [self.sb(sc, [128, 2048], F32, 'hb') for _ in range(2)],
                'tmp': [self.sb(sc, [128, 2048], F32, 'tmp') for _ in range(2)],
                'ub': [self.sb(sc, [128, 2048], BF16, 'ub') for _ in range(2)]}

    def gemm_tok(self, sc, xT, ntok, W_ap, N, OUT, tok0, wsl, stg, cnt, pbanks=(0, 1, 2, 3)):
        for nb in range((N + 511) // 512):
            n0 = nb * 512
            nn = min(512, N - n0)
            w = wsl[cnt[0] % 2]
            self.dma(w.t[:, :, 0:nn], W_ap[:, n0:n0 + nn].rearrange("(c p) n -> p c n", p=128), writes=[w.r], q='pool')
            for i in range(ntok // 128):
                pbk = self.pb[pbanks[cnt[1] % len(pbanks)]]
                for kc in range(16):
                    self.mm(pbk.t[:, 0:nn], xT.t[:, kc, i * 128:(i + 1) * 128], w.t[:, kc, 0:nn], kc == 0, kc == 15,
                            reads=[xT.r, w.r], writes=[pbk.r] if kc == 0 else [], pwrites=[] if kc == 0 else [pbk.r], inc=(kc == 15))
                s = stg[cnt[1] % len(stg)]
                eng = 'act' if cnt[1] % 2 == 0 else 'dve'
                self.cp(eng, s.t[:, 0:nn], pbk.t[:, 0:nn], reads=[pbk.r], writes=[s.r])
                ti = tok0 // 128 + i
                self.dma(OUT['ap'][ti * 128:(ti + 1) * 128, n0:n0 + nn], s.t[:, 0:nn], reads=[s.r], pwrites=[OUT['r'][ti]])
                cnt[1] += 1
            cnt[0] += 1

    def stage_ln(self, H, Y, MODB, l, gate_off, g_ap, b_ap, tiles, OUTF=None):
        with contextlib.ExitStack() as sc:
            gate = self.load_mods(sc, MODB, l, [gate_off])[gate_off]
            gt = self.load_bcast(sc, g_ap, 2048, 'lng')
            bt = self.load_bcast(sc, b_ap, 2048, 'lnb')
            hb = [self.sb(sc, [128, 2048], F32, 'hb') for _ in range(2)]
            yb = [self.sb(sc, [128, 2048], F32, 'yb') for _ in range(2)]
            t1 = [self.sb(sc, [128, 2048], F32, 't1') for _ in range(2)]
            sq = self.sb(sc, [128, 2048], F32, 'sq')
            st = [self.sb(sc, [128, 8], F32, 'st') for _ in range(2)]
            for n, ti in enumerate(tiles):
                which = 0 if ti < 32 else 1
                h, y, t, s = hb[n % 2], yb[n % 2], t1[n % 2], st[n % 2]
                rows = slice(ti * 128, (ti + 1) * 128)
                self.dma(h.t[:], H['ap'][rows, :], reads=[H['r'][ti]], writes=[h.r])
                self.dma(y.t[:], Y['ap'][rows, :], reads=[Y['r'][ti]], writes=[y.r])
                self.tt('pool', y.t[:], y.t[:], gate.t[:, which, :], ALU.mult, reads=[y.r, gate.r], writes=[y.r])
                self.stt('dve', t.t[:], h.t[:], ALPHA, y.t[:], ALU.mult, ALU.add, reads=[h.r, y.r], writes=[t.r])
                self.P.add('dve', lambda e, t=t, s=s: e.reduce_sum(out=s.t[:, 0:1], in_=t.t[:], axis=AX.X), reads=[t.r], writes=[s.r])
                self.ts('dve', s.t[:, 1:2], s.t[:, 0:1], -1.0 / 2048, None, ALU.mult, reads=[s.r], writes=[s.r])
                self.act(sq.t[:], t.t[:], AF.Square, reads=[t.r, s.r], writes=[sq.r], pwrites=[s.r], bias=s.t[:, 1:2], scale=1.0, accum_out=s.t[:, 2:3])
                self.act(s.t[:, 3:4], s.t[:, 2:3], AF.Sqrt, reads=[s.r], writes=[s.r], bias=self.eps5.t[:, 0:1], scale=1.0 / 2048)
                self.recip(s.t[:, 4:5], s.t[:, 3:4], reads=[s.r], writes=[s.r])
                self.tt('dve', s.t[:, 5:6], s.t[:, 1:2], s.t[:, 4:5], ALU.mult, reads=[s.r], writes=[s.r])
                self.act(t.t[:], t.t[:], AF.Identity, reads=[t.r, s.r], writes=[t.r], bias=s.t[:, 5:6], scale=s.t[:, 4:5])
                self.tt('pool', t.t[:], t.t[:], gt.t[:], ALU.mult, reads=[t.r, gt.r], writes=[t.r])
                self.tt('dve', t.t[:], t.t[:], bt.t[:], ALU.add, reads=[t.r, bt.r], writes=[t.r])
                self.dma(H['ap'][rows, :], t.t[:], reads=[t.r], writes=[H['r'][ti]])
                if OUTF is not None and ti < 32:
                    self.dma(OUTF['ap'][rows, :], t.t[:], reads=[t.r], writes=[OUTF['r'][ti]])
        self.P.barrier()

    def stage_moe(self, H, F, MODB, l, router, rbias, w1, w3, w2, sw1, sw3, sw2, with_ctx=True, n_exp=64):
        sblocks = [(1024 * b, 1024, 0) for b in range(4)] + ([(4096, 256, 1)] if with_ctx else [])
        with contextlib.ExitStack() as sc0:
            vT = self.sb(sc0, [128, 16, 1024], BF16, 'vT')
            yacc = self.sb(sc0, [128, 8, 2048], F32, 'yacc')
            Gs = self.sb(sc0, [128, 8, 66], F32, 'Gs')
            rt32 = self.sb(sc0, [128, 16, 64], F32, 'rt32')
            rb = self.load_bcast(sc0, rbias, 64, 'rb')
            self.dma(rt32.t[:], router.rearrange("(c p) e -> p c e", p=128), writes=[rt32.r])
            for (tok0, ntok, which) in sblocks:
                ntile = ntok // 128
                with contextlib.ExitStack() as sc:
                    msc = self.sb(sc, [128, 1, 2048], F32, 'msc')
                    msh = self.sb(sc, [128, 1, 2048], F32, 'msh')
                    self.dma(msc.t[:, 0, :], MODB['ap'][l, which, :, 8192:10240], reads=[MODB['r']], writes=[msc.r])
                    self.dma(msh.t[:, 0, :], MODB['ap'][l, which, :, 6144:8192], reads=[MODB['r']], writes=[msh.r])
                    bufs = self.modT_bufs(sc, pbase=6)
                    vf32 = self.sb(sc, [128, 2048], F32, 'vf32')
                    vT32 = self.sb(sc, [128, 16, 128], F32, 'vT32')
                    g1 = self.sb(sc, [128, 64], F32, 'g1')
                    g2 = self.sb(sc, [128, 64], F32, 'g2')
                    g3 = self.sb(sc, [128, 16], F32, 'g3')
                    self.memset('pool', Gs.t[:, :, 64:65], 1.0, pwrites=[Gs.r])

                    def gates(i, ti, tmp, mods_sh, w_):
                        self.tt('dve', vf32.t[:], tmp.t[:], mods_sh.t[:, 0, :], ALU.add, reads=[tmp.r, mods_sh.r], writes=[vf32.r])
                        for q4 in range(4):
                            pbk = self.pb[q4 % 2]
                            for q in range(4):
                                kc = q4 * 4 + q
                                self.tr(pbk.t[:, q * 128:(q + 1) * 128], vf32.t[:, kc * 128:(kc + 1) * 128], self.ident32.t[:],
                                        reads=[vf32.r, self.ident32.r], writes=[pbk.r] if q == 0 else [], pwrites=[] if q == 0 else [pbk.r], inc=(q == 3))
                            self.cp('dve', vT32.t[:, q4 * 4:(q4 + 1) * 4, :], pbk.t[:].rearrange("p (q k) -> p q k", q=4),
                                    reads=[pbk.r], pwrites=[vT32.r])
                        pr = self.pb[2]
                        for kc in range(16):
                            self.mm(pr.t[:, 0:64], vT32.t[:, kc, :], rt32.t[:, kc, :], kc == 0, kc == 15,
                                    reads=[vT32.r, rt32.r], writes=[pr.r] if kc == 0 else [], pwrites=[] if kc == 0 else [pr.r], inc=(kc == 15))
                        self.act(g1.t[:], pr.t[:, 0:64], AF.Sigmoid, reads=[pr.r], writes=[g1.r])
                        self.tt('dve', g2.t[:], g1.t[:], rb.t[:], ALU.add, reads=[g1.r, rb.r], writes=[g2.r])
                        self.P.add('dve', lambda e: e.max(out=g3.t[:, 0:8], in_=g2.t[:]), reads=[g2.r], writes=[g3.r])
                        self.ts('dve', g2.t[:], g2.t[:], g3.t[:, 5:6], None, ALU.is_ge, reads=[g2.r, g3.r], writes=[g2.r])
                        self.tt('dve', g1.t[:], g1.t[:], g2.t[:], ALU.mult, reads=[g1.r, g2.r], writes=[g1.r])
                        self.P.add('dve', lambda e: e.reduce_sum(out=g3.t[:, 8:9], in_=g1.t[:], axis=AX.X), reads=[g1.r], writes=[g3.r])
                        self.recip(g3.t[:, 9:10], g3.t[:, 8:9], reads=[g3.r], writes=[g3.r])
                        self.ts('dve', Gs.t[:, i, 0:64], g1.t[:], g3.t[:, 9:10], 2.5, ALU.mult, ALU.mult, reads=[g1.r, g3.r], pwrites=[Gs.r])
                    self.make_modT(bufs, H, (tok0, ntok, 0, 0), msc, msh, vT, also32=gates)
                self.P.barrier()
                with contextlib.ExitStack() as sc:
                    W13 = [self.sb(sc, [128, 16, 768], BF16, 'W13') for _ in range(2)]
                    W2 = [self.sb(sc, [128, 3, 2048], BF16, 'W2') for _ in range(2)]
                    aT = [self.sb(sc, [128, 3, 512], BF16, 'aT') for _ in range(2)]
                    silt = [self.sb(sc, [128, 512], F32, 'silt') for _ in range(2)]
                    cA = 0
                    cY = 0
                    cS = 0
                    for e in range(n_exp + 1):
                        if e < n_exp:
                            a1, a3, a2 = w1[l, e], w3[l, e], w2[l, e]
                        else:
                            a1, a3, a2 = sw1[l], sw3[l], sw2[l]
                        gcol = e if e < n_exp else 64
                        wa = W13[e % 2]
                        wb = W2[e % 2]
                        self.dma(wa.t[:, :, 0:384], a1.rearrange("(c p) f -> p c f", p=128), pwrites=[wa.r], q='pool')
                        self.dma(wa.t[:, :, 384:768], a3.rearrange("(c p) f -> p c f", p=128), pwrites=[wa.r], q='pool')
                        self.dma(wb.t[:], a2.rearrange("(c p) n -> p c n", p=128), writes=[wb.r], q='pool')
                        for sub in range((ntok + 511) // 512):
                            ns = min(512, ntok - sub * 512)
                            at = aT[cS % 2]
                            cS += 1
                            for fc in range(3):
                                A = self.pb[(cA * 2) % 4]
                                B = self.pb[(cA * 2 + 1) % 4]
                                sl = silt[cA % 2]
                                cA += 1
                                for kc in range(16):
                                    self.mm(A.t[:, 0:ns], wa.t[:, kc, fc * 128:(fc + 1) * 128], vT.t[:, kc, sub * 512:sub * 512 + ns], kc == 0, kc == 15,
                                            reads=[wa.r, vT.r], writes=[A.r] if kc == 0 else [], pwrites=[] if kc == 0 else [A.r], inc=(kc == 15))
                                for kc in range(16):
                                    self.mm(B.t[:, 0:ns], wa.t[:, kc, 384 + fc * 128:384 + (fc + 1) * 128], vT.t[:, kc, sub * 512:sub * 512 + ns], kc == 0, kc == 15,
                                            reads=[wa.r, vT.r], writes=[B.r] if kc == 0 else [], pwrites=[] if kc == 0 else [B.r], inc=(kc == 15))
                                self.act(sl.t[:, 0:ns], A.t[:, 0:ns], AF.Silu, reads=[A.r], writes=[sl.r])
                                self.tt('dve', at.t[:, fc, 0:ns], sl.t[:, 0:ns], B.t[:, 0:ns], ALU.mult, reads=[sl.r, B.r], pwrites=[at.r])
                            for i in range(ns // 128):
                                tl = sub * 4 + i
                                for cb in range(4):
                                    Yp = self.pb[4 + cY % 4]
                                    cY += 1
                                    for fc in range(3):
                                        self.mm(Yp.t[:], at.t[:, fc, i * 128:(i + 1) * 128], wb.t[:, fc, cb * 512:(cb + 1) * 512], fc == 0, fc == 2,
                                                reads=[at.r, wb.r], writes=[Yp.r] if fc == 0 else [], pwrites=[] if fc == 0 else [Yp.r], inc=(fc == 2))
                                    ya = yacc.t[:, tl, cb * 512:(cb + 1) * 512]
                                    if e == 0:
                                        self.ts('dve', ya, Yp.t[:], Gs.t[:, tl, gcol:gcol + 1], None, ALU.mult, reads=[Yp.r, Gs.r], pwrites=[yacc.r])
                                    else:
                                        self.stt('dve', ya, Yp.t[:], Gs.t[:, tl, gcol:gcol + 1], ya, ALU.mult, ALU.add, reads=[Yp.r, Gs.r], pwrites=[yacc.r])
                    for i in range(ntile):
                        ti = tok0 // 128 + i
                        self.dma(F['ap'][ti * 128:(ti + 1) * 128, :], yacc.t[:, i, :], reads=[yacc.r], writes=[F['r'][ti]])
                self.P.barrier()

    def stage_qkv(self, H, MODB, l, w_in, qn_ap, kn_ap, cosT, sinT, rotT_ap, QT, V):
        with contextlib.ExitStack() as sc:
            mods = self.load_mods(sc, MODB, l, [2048, 0])
            msc, msh = mods[2048], mods[0]
            bufs = self.modT_bufs(sc, pbase=6)
            uT = self.sb(sc, [128, 16, 512], BF16, 'uT')
            wsl = [self.sb(sc, [128, 16, 512], BF16, 'wsl') for _ in range(2)]
            rotT = self.sb(sc, [128, 128], F32, 'rotT')
            gn = self.sb(sc, [128, 2], F32, 'gn')
            self.dma(rotT.t[:], rotT_ap, writes=[rotT.r])
            self.dma(gn.t[:, 0:1], qn_ap.rearrange("(p o) -> p o", o=1), pwrites=[gn.r])
            self.dma(gn.t[:, 1:2], kn_ap.rearrange("(p o) -> p o", o=1), pwrites=[gn.r])
            ct = self.sb(sc, [128, 512], F32, 'ct')
            stt_ = self.sb(sc, [128, 512], F32, 'st')
            sq = [self.sb(sc, [128, 512], F32, 'sq') for _ in range(2)]
            rn = [self.sb(sc, [128, 512], F32, 'rn') for _ in range(2)]
            qn = [self.sb(sc, [128, 512], F32, 'qn') for _ in range(2)]
            t1 = [self.sb(sc, [128, 512], F32, 't1') for _ in range(2)]
            qo = [self.sb(sc, [128, 512], BF16, 'qo') for _ in range(2)]
            vs = [self.sb(sc, [128, 512], BF16, 'vs') for _ in range(2)]
            cw = 0
            ch = 0
            for blk in BLOCKS:
                tok0, n, col0, which = blk
                self.make_modT(bufs, H, blk, msc, msh, uT)
                if not which:
                    self.dma(ct.t[:, 0:n], cosT[:, tok0:tok0 + n], writes=[ct.r])
                    self.dma(stt_.t[:, 0:n], sinT[:, tok0:tok0 + n], writes=[stt_.r])
                for s in range(6):
                    w = wsl[cw % 2]
                    cw += 1
                    self.dma(w.t[:], w_in[:, s * 512:(s + 1) * 512].rearrange("(c p) n -> p c n", p=128), writes=[w.r], q='pool')
                    if s < 5:
                        for hh in range(4):
                            head = s * 4 + hh
                            gcol = 0 if head < 16 else 1
                            k = ch % 2
                            ch += 1
                            X = self.pb[k]
                            Sx = self.pb[2 + k]
                            R = self.pb[4 + k]
                            for kc in range(16):
                                self.mm(X.t[:, 0:n], w.t[:, kc, hh * 128:(hh + 1) * 128], uT.t[:, kc, 0:n], kc == 0, kc == 15,
                                        reads=[w.r, uT.r], writes=[X.r] if kc == 0 else [], pwrites=[] if kc == 0 else [X.r], inc=(kc == 15))
                            self.act(sq[k].t[:, 0:n], X.t[:, 0:n], AF.Square, reads=[X.r], writes=[sq[k].r])
                            self.mm(Sx.t[:, 0:n], self.ones32.t[:], sq[k].t[:, 0:n], True, True, reads=[self.ones32.r, sq[k].r], writes=[Sx.r])
                            self.act(rn[k].t[:, 0:n], Sx.t[:, 0:n], AF.Sqrt, reads=[Sx.r], writes=[rn[k].r], bias=self.epsq.t[:, 0:1], scale=1.0 / 128)
                            self.recip(rn[k].t[:, 0:n], rn[k].t[:, 0:n], reads=[rn[k].r], writes=[rn[k].r])
                            self.stt('dve', qn[k].t[:, 0:n], X.t[:, 0:n], gn.t[:, gcol:gcol + 1], rn[k].t[:, 0:n], ALU.mult, ALU.mult,
                                     reads=[X.r, gn.r, rn[k].r], writes=[qn[k].r])
                            if not which:
                                self.mm(R.t[:, 0:n], rotT.t[:], qn[k].t[:, 0:n], True, True, reads=[rotT.r, qn[k].r], writes=[R.r])
                                self.tt('pool', t1[k].t[:, 0:n], qn[k].t[:, 0:n], ct.t[:, 0:n], ALU.mult, reads=[qn[k].r, ct.r], writes=[t1[k].r])
                                self.tt('dve', qn[k].t[:, 0:n], R.t[:, 0:n], stt_.t[:, 0:n], ALU.mult, reads=[R.r, stt_.r, qn[k].r], writes=[qn[k].r])
                                self.tt('dve', qo[k].t[:, 0:n], t1[k].t[:, 0:n], qn[k].t[:, 0:n], ALU.add, reads=[t1[k].r, qn[k].r], writes=[qo[k].r])
                            else:
                                self.cp('dve', qo[k].t[:, 0:n], qn[k].t[:, 0:n], reads=[qn[k].r], writes=[qo[k].r])
                            self.dma(QT['ap'][head, :, tok0:tok0 + n], qo[k].t[:, 0:n], reads=[qo[k].r], pwrites=[QT['r']])
                    else:
                        for i in range(n // 128):
                            k = ch % 2
                            ch += 1
                            X = self.pb[k]
                            for kc in range(16):
                                self.mm(X.t[:], uT.t[:, kc, i * 128:(i + 1) * 128], w.t[:, kc, :], kc == 0, kc == 15,
                                        reads=[w.r, uT.r], writes=[X.r] if kc == 0 else [], pwrites=[] if kc == 0 else [X.r], inc=(kc == 15))
                            self.cp('act', vs[k].t[:], X.t[:], reads=[X.r], writes=[vs[k].r])
                            ti = tok0 // 128 + i
                            self.dma(V['ap'][ti * 128:(ti + 1) * 128, :], vs[k].t[:], reads=[vs[k].r], pwrites=[V['r']])
        self.P.barrier()

    def stage_attn(self, QT, V, OT, with_ctx=True, conv=None):
        scale = 1.0 / math.sqrt(128.0)
        with contextlib.ExitStack() as sc:
            hook = self.make_conv_hook(sc, *conv, total_calls=16 * (9 if with_ctx else 8)) if conv else None
            KT = self.sb(sc, [128, T], BF16, 'KT')
            Vg = self.sb(sc, [128, NT, 128], BF16, 'Vg')
            Qb = [self.sb(sc, [128, 512], BF16, 'Qb') for _ in range(2)]
            Pb = [self.sb(sc, [128, 512], BF16, 'Pb') for _ in range(3)]
            rec = self.sb(sc, [128, 512], F32, 'rec')
            ot = [self.sb(sc, [128, 512], BF16, 'ot') for _ in range(2)]
            cq = 0
            cs = 0
            for g in range(4):
                self.dma(KT.t[:], QT['ap'][16 + g], reads=[QT['r']], writes=[KT.r])
                self.dma(Vg.t[:], V['ap'][:, g * 128:(g + 1) * 128].rearrange("(t p) d -> p t d", p=128), reads=[V['r']], writes=[Vg.r])
                for hq in range(4):
                    h = g * 4 + hq
                    for blk in BLOCKS:
                        tok0, n, col0, which = blk
                        if which and not with_ctx:
                            continue
                        keys = [32, 33] if which else list(range(NT))
                        if hook:
                            hook()
                        q = Qb[cq % 2]
                        O = self.pb[4 + cq % 2]
                        Dn = self.pb[6 + cq % 2]
                        o = ot[cq % 2]
                        cq += 1
                        self.dma(q.t[:, 0:n], QT['ap'][h, :, tok0:tok0 + n], reads=[QT['r']], writes=[q.r])
                        LAG = 2
                        plist = []
                        for idx in range(len(keys) + LAG):
                            if idx < len(keys):
                                kt = keys[idx]
                                Sb = self.pb[cs % 4]
                                p = Pb[cs % 3]
                                cs += 1
                                plist.append(p)
                                self.mm(Sb.t[:, 0:n], KT.t[:, kt * 128:(kt + 1) * 128], q.t[:, 0:n], True, True, reads=[KT.r, q.r], writes=[Sb.r])
                                self.act(p.t[:, 0:n], Sb.t[:, 0:n], AF.Exp, reads=[Sb.r], writes=[p.r], scale=scale)
                            j2 = idx - LAG
                            if j2 >= 0:
                                kt2 = keys[j2]
                                p2 = plist[j2]
                                first, last = j2 == 0, j2 == len(keys) - 1
                                self.mm(O.t[:, 0:n], Vg.t[:, kt2, :], p2.t[:, 0:n], first, last, reads=[Vg.r, p2.r],
                                        writes=[O.r] if first else [], pwrites=[] if first else [O.r], inc=False)
                                self.mm(Dn.t[:, 0:n], self.ones16.t[:], p2.t[:, 0:n], first, last, reads=[self.ones16.r, p2.r],
                                        writes=[Dn.r] if first else [], pwrites=[] if first else [Dn.r], inc=True)
                        self.recip(rec.t[:, 0:n], Dn.t[:, 0:n], reads=[Dn.r], writes=[rec.r])
                        self.tt('dve', o.t[:, 0:n], O.t[:, 0:n], rec.t[:, 0:n], ALU.mult, reads=[O.r, rec.r], writes=[o.r])
                        self.dma(OT['ap'][h, :, tok0:tok0 + n], o.t[:, 0:n], reads=[o.r], pwrites=[OT['r']])
            if hook:
                hook(final=True)
        self.P.barrier()

    def stage_proj_T(self, XT, W_ap, Y, with_ctx=True):
        with contextlib.ExitStack() as sc:
            xT = [self.sb(sc, [128, 16, 512], BF16, 'xT') for _ in range(2)]
            wsl = [self.sb(sc, [128, 16, 512], BF16, 'wsl') for _ in range(2)]
            stg = [self.sb(sc, [128, 512], F32, 'stg') for _ in range(4)]
            cnt = [0, 0]
            for bi, blk in enumerate(BLOCKS):
                tok0, n, col0, which = blk
                if which and not with_ctx:
                    continue
                x = xT[bi % 2]
                self.dma(x.t[:, :, 0:n], XT['ap'][:, :, tok0:tok0 + n].rearrange("h p t -> p h t"), reads=[XT['r']], writes=[x.r])
                self.gemm_tok(sc, x, n, W_ap, 2048, Y, tok0, wsl, stg, cnt)
        self.P.barrier()

    def stage_zpad(self, PT):
        with contextlib.ExitStack() as sc:
            z = self.sb(sc, [128, 45, 32], F32, 'zpad')
            self.memset('pool', z.t[:], 0.0, writes=[z.r])
            pt = PT['ap']
            for (c0, w) in ((0, 16), (4112, 32), (4400, 16)):
                self.dma(pt[:, :, c0:c0 + w].rearrange("g p c -> p g c"), z.t[:, :, 0:w], reads=[z.r], pwrites=[PT['r']])
        self.P.barrier()

    def stage_ein(self, H, MODB, l, w_in, PT):
        with contextlib.ExitStack() as sc:
            mods = self.load_mods(sc, MODB, l, [2048, 0])
            msc, msh = mods[2048], mods[0]
            bufs = self.modT_bufs(sc, pbase=6)
            uT = self.sb(sc, [128, 16, 512], BF16, 'uT')
            wsl = [self.sb(sc, [128, 16, 512], BF16, 'wsl') for _ in range(2)]
            stg = [self.sb(sc, [128, 512], F32, 'stg') for _ in range(4)]
            cw = 0
            cg = 0
            for blk in BLOCKS:
                tok0, n, col0, which = blk
                self.make_modT(bufs, H, blk, msc, msh, uT)
                for slab in ESLABS:
                    c0 = EGROUPS[slab[0]][0]
                    c1 = EGROUPS[slab[-1]][0] + EGROUPS[slab[-1]][1]
                    w = wsl[cw % 2]
                    cw += 1
                    self.dma(w.t[:, :, 0:c1 - c0], w_in[:, c0:c1].rearrange("(c p) n -> p c n", p=128), writes=[w.r], q='pool')
                    for g in slab:
                        gc0, M = EGROUPS[g]
                        X = self.pb[cg % 4]
                        s = stg[cg % 4]
                        for kc in range(16):
                            self.mm(X.t[0:M, 0:n], w.t[:, kc, gc0 - c0:gc0 - c0 + M], uT.t[:, kc, 0:n], kc == 0, kc == 15,
                                    reads=[w.r, uT.r], writes=[X.r] if kc == 0 else [], pwrites=[] if kc == 0 else [X.r], inc=(kc == 15))
                        self.cp('act' if cg % 2 == 0 else 'dve', s.t[0:M, 0:n], X.t[0:M, 0:n], reads=[X.r], writes=[s.r])
                        self.dma(PT['ap'][g, 0:M, col0:col0 + n], s.t[0:M, 0:n], reads=[s.r], pwrites=[PT['r']])
                        cg += 1
        self.P.barrier()

    def stage_efeat(self, j, W, PT, SCN, VH, V32, G, RK):
        with contextlib.ExitStack() as sc:
            MU = self.sb(sc, [128, 29, 3], F32, 'MU')
            W0 = self.sb(sc, [128, 2, 8], F32, 'W0')
            A0 = self.sb(sc, [128, 2, 8], F32, 'A0')
            KKp = self.sb(sc, [128, 8], F32, 'KKp')
            KAp = self.sb(sc, [128, 8], F32, 'KAp')
            RKp = self.sb(sc, [128, 8], F32, 'RKp')
            wup = self.sb(sc, [96, 2, 1024], BF16, 'wup')
            aup = self.sb(sc, [96, 2, 1024], BF16, 'aup')
            gup = self.sb(sc, [64, 1024], BF16, 'gup')
            self.memset('pool', MU.t[:], 0.0, writes=[MU.r])
            for d in range(2):
                self.dma(MU.t[:, 0:24, d], W['mu'][d, 0:3072].rearrange("(g p) -> p g", p=128), pwrites=[MU.r], allow_slow_non_contiguous=True)
                for g in range(24, 29):
                    gc0, M = EGROUPS[g]
                    self.dma(MU.t[0:M, g, d:d + 1], W['mu'][d, gc0:gc0 + M].rearrange("(p o) -> p o", o=1), pwrites=[MU.r], allow_slow_non_contiguous=True)
                self.dma(W0.t[:, d, :], W['w0'][d].rearrange("(g p) -> p g", p=128), pwrites=[W0.r], allow_slow_non_contiguous=True)
                self.dma(A0.t[:, d, :], W['a0'][d].rearrange("(g p) -> p g", p=128), pwrites=[A0.r], allow_slow_non_contiguous=True)
                self.dma(wup.t[:, d, :], W['w_up'][d], pwrites=[wup.r], q='pool')
                self.dma(aup.t[:, d, :], W['a_up'][d], pwrites=[aup.r], q='pool')
            self.dma(gup.t[:], W['g_up'], writes=[gup.r], q='pool')
            self.dma(KKp.t[:], W['kk'].rearrange("(g p) -> p g", p=128), writes=[KKp.r], allow_slow_non_contiguous=True)
            self.dma(KAp.t[:], W['ka'].rearrange("(g p) -> p g", p=128), writes=[KAp.r], allow_slow_non_contiguous=True)
            self.dma(RKp.t[:], W['rk'].rearrange("h k -> (h k)").rearrange("(g p) -> p g", p=128), writes=[RKp.r], allow_slow_non_contiguous=True)
            self.tt('dve', MU.t[:, :, 2], MU.t[:, :, 0], MU.t[:, :, 1], ALU.add, reads=[MU.r], writes=[MU.r])
            self.ts('dve', MU.t[:, :, 2], MU.t[:, :, 2], -1.0, 1.0, ALU.mult, ALU.add, reads=[MU.r], writes=[MU.r])

            Xb = [self.sb(sc, [128, 514], F32, 'Xb') for _ in range(4)]
            mt = [self.sb(sc, [128, 512], F32, 'mt') for _ in range(2)]
            sg = self.sb(sc, [64, 512], BF16, 'sg')
            sgf = self.sb(sc, [64, 512], F32, 'sgf')
            twd = [self.sb(sc, [96, 512], BF16, 'twd') for _ in range(2)]
            tad = [self.sb(sc, [96, 512], BF16, 'tad') for _ in range(2)]
            tf = self.sb(sc, [96, 512], F32, 'tf')
            rT = [self.sb(sc, [128, 512], F32, 'rT') for _ in range(2)]
            kT = [self.sb(sc, [128, 512], F32, 'kT') for _ in range(2)]
            vT = [self.sb(sc, [128, 512], F32, 'vT') for _ in range(2)]
            kkr = [self.sb(sc, [128, 512], F32, 'kkr') for _ in range(2)]
            sq = self.sb(sc, [128, 512], F32, 'sq')
            rn = self.sb(sc, [128, 512], F32, 'rn')
            kk = [self.sb(sc, [128, 512], F32, 'kk') for _ in range(2)]
            sig = [self.sb(sc, [128, 512], F32, 'sig') for _ in range(2)]
            wdec = [self.sb(sc, [128, 512], F32, 'wdec') for _ in range(2)]
            aa = [self.sb(sc, [128, 512], F32, 'aa') for _ in range(2)]
            tm = [self.sb(sc, [128, 512], F32, 'tm') for _ in range(2)]
            kd = [self.sb(sc, [128, 512], F32, 'kd') for _ in range(2)]
            bd = [self.sb(sc, [128, 512], F32, 'bd') for _ in range(2)]
            prod = [self.sb(sc, [128, 512], F32, 'prod') for _ in range(2)]
            vtok = self.sb(sc, [128, 4, 1024], F32, 'vtok')
            vb16 = self.sb(sc, [128, 4, 1024], BF16, 'vb16')
            gt = [self.sb(sc, [128, 1024], F32, 'gt') for _ in range(2)]
            rkt = self.sb(sc, [128, 4, 32], F32, 'rkt')
            Sx, LW, LA, PRK, PV, PG0, PG1 = (self.pb[i] for i in range(7))
            cx = [0]

            def mix(g, M, col0, n, out_ap, out_res, pw=False):
                X = Xb[cx[0] % 4]
                t = mt[cx[0] % 2]
                cx[0] += 1
                self.dma(X.t[0:M, 0:n + 2], PT['ap'][g, 0:M, col0 - 1:col0 + n + 1], reads=[PT['r']], writes=[X.r])
                self.ts('dve', t.t[0:M, 0:n], X.t[0:M, 1:n + 1], MU.t[0:M, g, 2:3], None, ALU.mult, reads=[X.r, MU.r], writes=[t.r])
                self.stt('dve', t.t[0:M, 0:n], X.t[0:M, 0:n], MU.t[0:M, g, 0:1], t.t[0:M, 0:n], ALU.mult, ALU.add, reads=[X.r, MU.r, t.r], writes=[t.r])
                kw = dict(pwrites=[out_res]) if pw else dict(writes=[out_res])
                self.stt('dve', out_ap, X.t[0:M, 2:n + 2], MU.t[0:M, g, 1:2], t.t[0:M, 0:n], ALU.mult, ALU.add, reads=[X.r, MU.r, t.r], **kw)

            cj = 0
            for blk in BLOCKS:
                tok0, n, col0, which = blk
                nt = n // 128
                mix(24, 64, col0, n, sgf.t[:, 0:n], sgf.r)
                self.act(sg.t[:, 0:n], sgf.t[:, 0:n], AF.Sigmoid, reads=[sgf.r], writes=[sg.r])
                for d in range(2):
                    mix(25 + d, 96, col0, n, tf.t[:, 0:n], tf.r)
                    self.act(twd[d].t[:, 0:n], tf.t[:, 0:n], AF.Tanh, reads=[tf.r], writes=[twd[d].r])
                    mix(27 + d, 96, col0, n, tad[d].t[:, 0:n], tad[d].r)
                for i in range(nt):
                    for hf, PGx in enumerate((PG0, PG1)):
                        self.mm(PGx.t[:], sg.t[:, i * 128:(i + 1) * 128], gup.t[:, hf * 512:(hf + 1) * 512], True, True, reads=[sg.r, gup.r], writes=[PGx.r])
                    g_ = gt[i % 2]
                    self.cp('act', g_.t[:, 0:512], PG0.t[:], reads=[PG0.r], writes=[g_.r])
                    self.cp('act', g_.t[:, 512:1024], PG1.t[:], reads=[PG1.r], pwrites=[g_.r])
                    ti = tok0 // 128 + i
                    self.dma(G['ap'][ti * 128:(ti + 1) * 128, :], g_.t[:], reads=[g_.r], pwrites=[G['r']])
                for jj in range(8):
                    k2 = cj % 2
                    cj += 1
                    r_, k_, v_ = rT[k2], kT[k2], vT[k2]
                    mix(jj, 128, col0, n, r_.t[:, 0:n], r_.r)
                    mix(8 + jj, 128, col0, n, k_.t[:, 0:n], k_.r)
                    mix(16 + jj, 128, col0, n, v_.t[:, 0:n], v_.r)
                    cs = slice(col0, col0 + n)
                    self.dma(SCN['ap'][0, jj, :, cs], r_.t[:, 0:n], reads=[r_.r], pwrites=[SCN['r']])
                    kr = kkr[k2]
                    self.ts('dve', kr.t[:, 0:n], k_.t[:, 0:n], KKp.t[:, jj:jj + 1], None, ALU.mult, reads=[k_.r, KKp.r], writes=[kr.r])
                    self.act(sq.t[:, 0:n], kr.t[:, 0:n], AF.Square, reads=[kr.r], writes=[sq.r])
                    self.mm(Sx.t[:, 0:n], self.bones32.t[:], sq.t[:, 0:n], True, True, reads=[self.bones32.r, sq.r], writes=[Sx.r])
                    self.act(rn.t[:, 0:n], Sx.t[:, 0:n], AF.Sqrt, reads=[Sx.r], writes=[rn.r], bias=self.eps12.t[:, 0:1], scale=1.0)
                    self.recip(rn.t[:, 0:n], rn.t[:, 0:n], reads=[rn.r], writes=[rn.r])
                    kk_ = kk[k2]
                    self.tt('dve', kk_.t[:, 0:n], kr.t[:, 0:n], rn.t[:, 0:n], ALU.mult, reads=[kr.r, rn.r], writes=[kk_.r])
                    self.dma(SCN['ap'][1, jj, :, cs], kk_.t[:, 0:n], reads=[kk_.r], pwrites=[SCN['r']])
                    for d in range(2):
                        self.mm(LW.t[:, 0:n], wup.t[:, d, jj * 128:(jj + 1) * 128], twd[d].t[:, 0:n], True, True, reads=[wup.r, twd[d].r], writes=[LW.r])
                        self.act(sig[d].t[:, 0:n], LW.t[:, 0:n], AF.Sigmoid, reads=[LW.r, W0.r], writes=[sig[d].r], bias=W0.t[:, d, jj:jj + 1], scale=1.0)
                        self.act(wdec[d].t[:, 0:n], sig[d].t[:, 0:n], AF.Exp, reads=[sig[d].r], writes=[wdec[d].r], scale=-math.exp(-0.5))
                        self.dma(SCN['ap'][2 + 3 * d, jj, :, cs], wdec[d].t[:, 0:n], reads=[wdec[d].r], pwrites=[SCN['r']])
                        self.mm(LA.t[:, 0:n], aup.t[:, d, jj * 128:(jj + 1) * 128], tad[d].t[:, 0:n], True, True, reads=[aup.r, tad[d].r], writes=[LA.r])
                        self.act(aa[d].t[:, 0:n], LA.t[:, 0:n], AF.Sigmoid, reads=[LA.r, A0.r], writes=[aa[d].r], bias=A0.t[:, d, jj:jj + 1], scale=1.0)
                        self.ts('dve', tm[d].t[:, 0:n], aa[d].t[:, 0:n], -1.0, KAp.t[:, jj:jj + 1], ALU.add, ALU.mult, reads=[aa[d].r, KAp.r], writes=[tm[d].r])
                        self.stt('dve', kd[d].t[:, 0:n], tm[d].t[:, 0:n], 1.0, k_.t[:, 0:n], ALU.add, ALU.mult, reads=[tm[d].r, k_.r], writes=[kd[d].r])
                        self.dma(SCN['ap'][4 + 3 * d, jj, :, cs], kd[d].t[:, 0:n], reads=[kd[d].r], pwrites=[SCN['r']])
                        self.tt('pool', bd[d].t[:, 0:n], kk_.t[:, 0:n], aa[d].t[:, 0:n], ALU.mult, reads=[kk_.r, aa[d].r], writes=[bd[d].r])
                        self.dma(SCN['ap'][3 + 3 * d, jj, :, cs], bd[d].t[:, 0:n], reads=[bd[d].r], pwrites=[SCN['r']])
                        self.stt('dve', prod[d].t[:, 0:n], r_.t[:, 0:n], RKp.t[:, jj:jj + 1], kd[d].t[:, 0:n], ALU.mult, ALU.mult, reads=[r_.r, RKp.r, kd[d].r], writes=[prod[d].r])
                        for i in range(nt):
                            c_ = i * 32 + d * 16 + 2 * jj
                            self.mm(PRK.t[:, c_:c_ + 2], prod[d].t[:, i * 128:(i + 1) * 128], self.bind32.t[:], True, True,
                                    reads=[prod[d].r, self.bind32.r], pwrites=[PRK.r])
                    for i in range(nt):
                        self.tr(PV.t[:, i * 128:(i + 1) * 128], v_.t[:, i * 128:(i + 1) * 128], self.ident32.t[:],
                                reads=[v_.r, self.ident32.r], writes=[PV.r] if i == 0 else [], pwrites=[] if i == 0 else [PV.r], inc=(i == nt - 1))
                    self.cp('act', vtok.t[:, 0:nt, jj * 128:(jj + 1) * 128], PV.t[:, 0:nt * 128].rearrange("p (i c) -> p i c", i=nt),
                            reads=[PV.r], pwrites=[vtok.r])
                self.cp('act', rkt.t[:, 0:nt, :], PRK.t[:, 0:nt * 32].rearrange("p (i c) -> p i c", i=nt), reads=[PRK.r], writes=[rkt.r])
                rows = slice(tok0, tok0 + n)
                self.dma(RK['ap'][rows, :].rearrange("(i p) c -> p i c", p=128), rkt.t[:, 0:nt, :], reads=[rkt.r], pwrites=[RK['r']])
                self.dma(V32['ap'][rows, :].rearrange("(i p) c -> p i c", p=128), vtok.t[:, 0:nt, :], reads=[vtok.r], pwrites=[V32['r']])
                self.cp('pool', vb16.t[:, 0:nt, :], vtok.t[:, 0:nt, :], reads=[vtok.r], writes=[vb16.r])
                for h2 in range(2):
                    for i in range(nt):
                        src = vb16.t[:, i, :].rearrange("p (j h v) -> p j h v", j=8, h=2)[:, :, h2, :]
                        dst = VH['ap'][h2, tok0 + i * 128:tok0 + (i + 1) * 128, :].rearrange("p (j v) -> p j v", j=8)
                        self.dma(dst, src, reads=[vb16.r], pwrites=[VH['r']])
        self.P.barrier()

    def stage_escan(self, SCN, VH, YS, nsteps=T):
        CH = 64
        with contextlib.ExitStack() as sc:
            S = self.sb(sc, [128, 2, 8, 64], F32, 'S')
            Sw = self.sb(sc, [128, 2, 8, 64], F32, 'Sw')
            tmpB = self.sb(sc, [128, 2, 8, 64], F32, 'tmpB')
            tmpV = self.sb(sc, [128, 2, 8, 64], F32, 'tmpV')
            tmpK = self.sb(sc, [128, 2, 512], BF16, 'tmpK')
            tmpR = self.sb(sc, [128, 2, 512], BF16, 'tmpR')
            q2 = [self.sb(sc, [128, 2, 5, 8, CH], F32, 'q2') for _ in range(2)]
            vbb = [self.sb(sc, [128, 2, 16, 512], BF16, 'vbb') for _ in range(2)]
            Z = self.sb(sc, [128, 256], BF16, 'Z')
            ysb = [self.sb(sc, [128, 2, 512], F32, 'ysb') for _ in range(2)]
            self.memset('pool', Z.t[:], 0.0, writes=[Z.r])
            self.memset('pool', Z.t[0:64, 127:128], 1.0, pwrites=[Z.r])
            self.memset('pool', Z.t[64:128, 191:192], 1.0, pwrites=[Z.r])
            self.memset('pool', S.t[:], 0.0, writes=[S.r])
            SK = Tl(self.pp[0][:, :])
            SK.r = self.pb[0].r

            def chunk_ranges(c):
                if c < 4:
                    return (4144 + 64 * c, 4144 + 192 - 64 * c, 4096 + 64 * c, 4096 + 192 - 64 * c)
                cc = c - 4
                return (16 + 64 * cc, 16 + 4096 - 64 * (cc + 1), 64 * cc, 4096 - 64 * (cc + 1))

            def load_chunk(c):
                qb = q2[c % 2]
                cf0, cb0, _, _ = chunk_ranges(c)
                for d, c0 in ((0, cf0), (1, cb0)):
                    self.dma(qb.t[:, d, 0:2, :, :], SCN['ap'][0:2, :, :, c0:c0 + CH].rearrange("q j p c -> p q j c"), reads=[SCN['r']], pwrites=[qb.r])
                    self.dma(qb.t[:, d, 2:5, :, :], SCN['ap'][2 + 3 * d:5 + 3 * d, :, :, c0:c0 + CH].rearrange("q j p c -> p q j c"), reads=[SCN['r']], pwrites=[qb.r])

            def load_sub(c, sub):
                vb = vbb[(c * 4 + sub) % 2]
                _, _, tf0, tb0 = chunk_ranges(c)
                rf = tf0 + 16 * sub
                rb_ = tb0 + 48 - 16 * sub
                for d, r0 in ((0, rf), (1, rb_)):
                    for h2 in range(2):
                        self.dma(vb.t[h2 * 64:(h2 + 1) * 64, d, :, :], VH['ap'][h2, r0:r0 + 16, :].partition_broadcast(64), reads=[VH['r']], pwrites=[vb.r])

            def opnd(qb, qi, sl):
                a0 = qb.t[:, 0, qi, :, sl]
                a1 = qb.t[:, 1, qi, :, CH - 1 - sl]
                return bass.AP(a0.tensor, a0.offset, [list(a0.ap[0]), [a1.offset - a0.offset, 2], [CH, 8], [0, 64]])

            def vopnd(vb, ss):
                a0 = vb.t[:, 0, ss, :]
                a1 = vb.t[:, 1, 15 - ss, :]
                return bass.AP(a0.tensor, a0.offset, [list(a0.ap[0]), [a1.offset - a0.offset, 2], [64, 8], [1, 64]])

            nch = nsteps // CH
            load_chunk(0)
            load_sub(0, 0)
            for c in range(nch):
                qb = q2[c % 2]
                if c + 1 < nch:
                    load_chunk(c + 1)
                Y0 = self.pb[2 + (c % 2) * 2]
                Y1 = self.pb[3 + (c % 2) * 2]
                cf0, cb0, tf0, tb0 = chunk_ranges(c)
                for sl in range(CH):
                    sub, ss = sl // 16, sl % 16
                    if ss == 0:
                        if sub < 3:
                            load_sub(c, sub + 1)
                        elif c + 1 < nch:
                            load_sub(c + 1, 0)
                    vb = vbb[(c * 4 + sub) % 2]
                    Sf = S.t[:]
                    self.tt('dve', tmpK.t[:].rearrange("p d (j v) -> p d j v", j=8), Sf, opnd(qb, 1, sl), ALU.mult, reads=[S.r, qb.r], writes=[tmpK.r])
                    self.mm(SK.t[:, 0:512], self.bones16.t[:], tmpK.t[:, 0, :], True, True, reads=[self.bones16.r, tmpK.r], writes=[SK.r], inc=False)
                    self.mm(SK.t[:, 512:1024], self.bones16.t[:], tmpK.t[:, 1, :], True, True, reads=[self.bones16.r, tmpK.r], pwrites=[SK.r])
                    self.tt('pool', Sw.t[:], Sf, opnd(qb, 2, sl), ALU.mult, reads=[S.r, qb.r], writes=[Sw.r])
                    self.tt('pool', tmpV.t[:], vopnd(vb, ss), opnd(qb, 4, sl), ALU.mult, reads=[vb.r, qb.r], writes=[tmpV.r])
                    self.tt('dve', tmpB.t[:], SK.t[:, :].rearrange("p (d j v) -> p d j v", d=2, j=8), opnd(qb, 3, sl), ALU.mult, reads=[SK.r, qb.r], writes=[tmpB.r])
                    self.tt('dve', Sw.t[:], Sw.t[:], tmpB.t[:], ALU.subtract, reads=[Sw.r, tmpB.r], writes=[Sw.r])
                    self.tt('dve', S.t[:], Sw.t[:], tmpV.t[:], ALU.add, reads=[Sw.r, tmpV.r], writes=[S.r])
                    self.tt('dve', tmpR.t[:].rearrange("p d (j v) -> p d j v", j=8), S.t[:], opnd(qb, 0, sl), ALU.mult, reads=[S.r, qb.r], writes=[tmpR.r])
                    first, last = sl == 0, sl == CH - 1
                    self.mm(Y0.t[:], Z.t[:, 127 - sl:255 - sl], tmpR.t[:, 0, :], first, last, reads=[Z.r, tmpR.r],
                            writes=[Y0.r] if first else [], pwrites=[] if first else [Y0.r], inc=False)
                    self.mm(Y1.t[:], Z.t[:, 64 + sl:192 + sl], tmpR.t[:, 1, :], first, last, reads=[Z.r, tmpR.r],
                            writes=[Y1.r] if first else [], pwrites=[] if first else [Y1.r])
                yb = ysb[c % 2]
                self.cp('act', yb.t[:, 0, :], Y0.t[:], reads=[Y0.r], writes=[yb.r])
                self.cp('act', yb.t[:, 1, :], Y1.t[:], reads=[Y1.r], pwrites=[yb.r])
                for d, r0 in ((0, tf0), (1, tb0)):
                    for h2 in range(2):
                        dst = YS['ap'][d, r0:r0 + CH, :].rearrange("t (j h v) -> t j h v", j=8, h=2)[:, :, h2, :]
                        self.dma(dst, yb.t[h2 * 64:(h2 + 1) * 64, d, :].rearrange("p (j v) -> p j v", j=8), reads=[yb.r], pwrites=[YS['r']])
        self.P.barrier()

    def stage_eepi(self, W, YS, V32, G, RK, CAT):
        with contextlib.ExitStack() as sc:
            gng = self.load_bcast(sc, W['gn_g'], 1024, 'gng')
            gnb = self.load_bcast(sc, W['gn_b'], 1024, 'gnb')
            y0 = [self.sb(sc, [128, 16, 64], F32, 'y0') for _ in range(2)]
            y1 = [self.sb(sc, [128, 16, 64], F32, 'y1') for _ in range(2)]
            vv = [self.sb(sc, [128, 16, 64], F32, 'vv') for _ in range(2)]
            gg = [self.sb(sc, [128, 16, 64], F32, 'gg') for _ in range(2)]
            rk = [self.sb(sc, [128, 32], F32, 'rk') for _ in range(2)]
            sq = self.sb(sc, [128, 16, 64], F32, 'sq')
            st = [self.sb(sc, [128, 5, 16], F32, 'st') for _ in range(2)]
            ob = [self.sb(sc, [128, 1024], BF16, 'ob') for _ in range(2)]
            for ti in range(NT):
                k = ti % 2
                rows = slice(ti * 128, (ti + 1) * 128)
                a, b, v, g, r, s, o = y0[k], y1[k], vv[k], gg[k], rk[k], st[k], ob[k]
                self.dma(a.t[:], YS['ap'][0, rows, :].rearrange("p (h v) -> p h v", h=16), reads=[YS['r']], writes=[a.r])
                self.dma(b.t[:], YS['ap'][1, rows, :].rearrange("p (h v) -> p h v", h=16), reads=[YS['r']], writes=[b.r])
                self.dma(v.t[:], V32['ap'][rows, :].rearrange("p (h v) -> p h v", h=16), reads=[V32['r']], writes=[v.r])
                self.dma(g.t[:], G['ap'][rows, :].rearrange("p (h v) -> p h v", h=16), reads=[G['r']], writes=[g.r])
                self.dma(r.t[:], RK['ap'][rows, :], reads=[RK['r']], writes=[r.r])
                self.tt('dve', a.t[:], a.t[:], b.t[:], ALU.add, reads=[a.r, b.r], writes=[a.r])
                self.P.add('dve', lambda e, a=a, s=s: e.reduce_sum(out=s.t[:, 0, :], in_=a.t[:], axis=AX.X), reads=[a.r], writes=[s.r])
                self.ts('dve', s.t[:, 0, :], s.t[:, 0, :], -1.0 / 64, None, ALU.mult, reads=[s.r], writes=[s.r])
                self.tt('dve', a.t[:], a.t[:], s.t[:, 0, :].unsqueeze(2).to_broadcast([128, 16, 64]), ALU.add, reads=[a.r, s.r], writes=[a.r])
                self.tt('pool', sq.t[:], a.t[:], a.t[:], ALU.mult, reads=[a.r], writes=[sq.r])
                self.P.add('dve', lambda e, s=s: e.reduce_sum(out=s.t[:, 1, :], in_=sq.t[:], axis=AX.X), reads=[sq.r], writes=[s.r])
                self.act(s.t[:, 2, :], s.t[:, 1, :], AF.Sqrt, reads=[s.r], writes=[s.r], bias=self.epsg.t[:, 0:1], scale=1.0 / 64)
                self.recip(s.t[:, 3, :], s.t[:, 2, :], reads=[s.r], writes=[s.r])
                self.tt('dve', a.t[:], a.t[:], s.t[:, 3, :].unsqueeze(2).to_broadcast([128, 16, 64]), ALU.mult, reads=[a.r, s.r], writes=[a.r])
                af = a.t[:].rearrange("p h v -> p (h v)")
                self.tt('pool', af, af, gng.t[:], ALU.mult, reads=[a.r, gng.r], writes=[a.r])
                self.tt('dve', af, af, gnb.t[:], ALU.add, reads=[a.r, gnb.r], writes=[a.r])
                self.tt('dve', s.t[:, 4, :], r.t[:, 0:16], r.t[:, 16:32], ALU.add, reads=[r.r], writes=[s.r])
                self.tt('pool', v.t[:], v.t[:], s.t[:, 4, :].unsqueeze(2).to_broadcast([128, 16, 64]), ALU.mult, reads=[v.r, s.r], writes=[v.r])
                self.tt('dve', a.t[:], a.t[:], v.t[:], ALU.add, reads=[a.r, v.r], writes=[a.r])
                self.tt('dve', o.t[:].rearrange("p (h v) -> p h v", h=16), a.t[:], g.t[:], ALU.mult, reads=[a.r, g.r], writes=[o.r])
                self.dma(CAT['ap'][rows, 0:1024], o.t[:], reads=[o.r], pwrites=[CAT['r']])
        self.P.barrier()

    def stage_econv(self, W, PT, CAT):
        with contextlib.ExitStack() as sc:
            CW = self.sb(sc, [128, 8, 31], F32, 'CW')
            CB = self.sb(sc, [128, 8], F32, 'CB')
            lg = self.load_bcast(sc, W['cv_ln_g'], 1024, 'cvg')
            lb = self.load_bcast(sc, W['cv_ln_b'], 1024, 'cvb')
            for c in range(8):
                self.dma(CW.t[:, c, :], W['cv_w'][:, c * 128:(c + 1) * 128].rearrange("k p -> p k"), pwrites=[CW.r], allow_slow_non_contiguous=True)
            self.dma(CB.t[:], W['cv_b'].rearrange("(c p) -> p c", p=128), writes=[CB.r], allow_slow_non_contiguous=True)
            val = [self.sb(sc, [128, 542], F32, 'val') for _ in range(2)]
            gat = [self.sb(sc, [128, 542], F32, 'gat') for _ in range(2)]
            acc = [self.sb(sc, [128, 512], F32, 'acc') for _ in range(2)]
            zc = self.sb(sc, [128, 4, 1024], F32, 'zc')
            sq = self.sb(sc, [128, 1024], F32, 'sq')
            st = [self.sb(sc, [128, 8], F32, 'st') for _ in range(2)]
            ob = [self.sb(sc, [128, 1024], BF16, 'ob') for _ in range(2)]
            cc = 0
            for blk in BLOCKS:
                tok0, n, col0, which = blk
                nt = n // 128
                for c in range(8):
                    k = cc % 2
                    cc += 1
                    v, g, a = val[k], gat[k], acc[k]
                    eng = 'dve'
                    self.dma(v.t[:, 0:n + 30], PT['ap'][29 + c, :, col0 - 15:col0 + n + 15], reads=[PT['r']], writes=[v.r])
                    self.dma(g.t[:, 0:n + 30], PT['ap'][37 + c, :, col0 - 15:col0 + n + 15], reads=[PT['r']], writes=[g.r])
                    self.act(g.t[:, 0:n + 30], g.t[:, 0:n + 30], AF.Sigmoid, reads=[g.r], writes=[g.r])
                    self.tt(eng, v.t[:, 0:n + 30], v.t[:, 0:n + 30], g.t[:, 0:n + 30], ALU.mult, reads=[v.r, g.r], writes=[v.r])
                    self.ts(eng, a.t[:, 0:n], v.t[:, 0:n], CW.t[:, c, 0:1], CB.t[:, c:c + 1], ALU.mult, ALU.add, reads=[v.r, CW.r, CB.r], writes=[a.r])
                    for kk_ in range(1, 31):
                        self.stt(eng, a.t[:, 0:n], v.t[:, kk_:kk_ + n], CW.t[:, c, kk_:kk_ + 1], a.t[:, 0:n], ALU.mult, ALU.add, reads=[v.r, CW.r, a.r], writes=[a.r])
                    PV = self.pb[k]
                    for i in range(nt):
                        self.tr(PV.t[:, i * 128:(i + 1) * 128], a.t[:, i * 128:(i + 1) * 128], self.ident32.t[:],
                                reads=[a.r, self.ident32.r], writes=[PV.r] if i == 0 else [], pwrites=[] if i == 0 else [PV.r], inc=(i == nt - 1))
                    self.cp('act', zc.t[:, 0:nt, c * 128:(c + 1) * 128], PV.t[:, 0:nt * 128].rearrange("p (i c) -> p i c", i=nt), reads=[PV.r], pwrites=[zc.r])
                for i in range(nt):
                    ti = tok0 // 128 + i
                    s, o = st[i % 2], ob[i % 2]
                    t = zc.t[:, i, :]
                    self.P.add('dve', lambda e, t=t, s=s: e.reduce_sum(out=s.t[:, 0:1], in_=t, axis=AX.X), reads=[zc.r], writes=[s.r])
                    self.ts('dve', s.t[:, 1:2], s.t[:, 0:1], -1.0 / 1024, None, ALU.mult, reads=[s.r], writes=[s.r])
                    self.act(sq.t[:], t, AF.Square, reads=[zc.r, s.r], writes=[sq.r], pwrites=[s.r], bias=s.t[:, 1:2], scale=1.0, accum_out=s.t[:, 2:3])
                    self.act(s.t[:, 3:4], s.t[:, 2:3], AF.Sqrt, reads=[s.r], writes=[s.r], bias=self.eps5.t[:, 0:1], scale=1.0 / 1024)
                    self.recip(s.t[:, 4:5], s.t[:, 3:4], reads=[s.r], writes=[s.r])
                    self.tt('dve', s.t[:, 5:6], s.t[:, 1:2], s.t[:, 4:5], ALU.mult, reads=[s.r], writes=[s.r])
                    self.act(sq.t[:], t, AF.Identity, reads=[zc.r, s.r], writes=[sq.r], bias=s.t[:, 5:6], scale=s.t[:, 4:5])
                    self.tt('pool', sq.t[:], sq.t[:], lg.t[:], ALU.mult, reads=[sq.r, lg.r], writes=[sq.r])
                    self.tt('dve', sq.t[:], sq.t[:], lb.t[:], ALU.add, reads=[sq.r, lb.r], writes=[sq.r])
                    self.act(o.t[:], sq.t[:], AF.Silu, reads=[sq.r], writes=[o.r])
                    self.dma(CAT['ap'][ti * 128:(ti + 1) * 128, 1024:2048], o.t[:], reads=[o.r], pwrites=[CAT['r']])
        self.P.barrier()

    def stage_eproj(self, CAT, W_ap, Y):
        with contextlib.ExitStack() as sc:
            cb = [self.sb(sc, [128, 2048], BF16, 'cb') for _ in range(2)]
            xT = self.sb(sc, [128, 16, 512], BF16, 'xT')
            wsl = [self.sb(sc, [128, 16, 512], BF16, 'wsl') for _ in range(2)]
            stg = [self.sb(sc, [128, 512], F32, 'stg') for _ in range(4)]
            cnt = [0, 0]
            kq = 0
            for blk in BLOCKS:
                tok0, n, col0, which = blk
                for i in range(n // 128):
                    ti = tok0 // 128 + i
                    c = cb[kq % 2]
                    kq += 1
                    self.dma(c.t[:], CAT['ap'][ti * 128:(ti + 1) * 128, :], reads=[CAT['r']], writes=[c.r])
                    for half in range(2):
                        pbk = self.pb[6 + half]
                        pv = pbk.t[:].bitcast(BF16)
                        for q in range(8):
                            kc = half * 8 + q
                            self.tr(pv[:, q * 128:(q + 1) * 128], c.t[:, kc * 128:(kc + 1) * 128], self.ident16.t[:],
                                    reads=[c.r, self.ident16.r], writes=[pbk.r] if q == 0 else [], pwrites=[] if q == 0 else [pbk.r], inc=(q == 7))
                        self.cp('act', xT.t[:, half * 8:(half + 1) * 8, i * 128:(i + 1) * 128], pv.rearrange("p (q k) -> p q k", q=8),
                                reads=[pbk.r], pwrites=[xT.r])
                self.gemm_tok(sc, xT, n, W_ap, 2048, Y, tok0, wsl, stg, cnt)
        self.P.barrier()

    def stage_moe_conv(self, l, w1, w3, w2, sw1, sw3, sw2, WB):
        with contextlib.ExitStack() as sc:
            wt = [self.sb(sc, [128, 18432], BF16, 'wcv') for _ in range(2)]
            for e in range(65):
                if e < 64:
                    a1, a3, a2 = w1[l, e], w3[l, e], w2[l, e]
                else:
                    a1, a3, a2 = sw1[l], sw3[l], sw2[l]
                t = wt[e % 2]
                self.dma(t.t[:, 0:6144].rearrange("p (c f) -> p c f", c=16), a1.rearrange("(c p) f -> p c f", p=128), pwrites=[t.r], q='pool')
                self.dma(t.t[:, 6144:12288].rearrange("p (c f) -> p c f", c=16), a3.rearrange("(c p) f -> p c f", p=128), pwrites=[t.r], q='pool')
                self.dma(t.t[:, 12288:18432].rearrange("p (c n) -> p c n", c=3), a2.rearrange("(c p) n -> p c n", p=128), pwrites=[t.r], q='pool')
                self.dma(WB['ap'][e * 128:(e + 1) * 128, :], t.t[:, 0:12288], reads=[t.r], pwrites=[WB['r']])
                self.dma(WB['ap2'][e * 128:(e + 1) * 128, :], t.t[:, 12288:18432], reads=[t.r], pwrites=[WB['r']])
        self.P.barrier()

    def make_conv_hook(self, sc, l, w1, w3, w2, sw1, sw3, sw2, WB, total_calls):
        bufs = [self.sb(sc, [128, 6144], BF16, 'wcv') for _ in range(4)]
        state = {'e': 0, 'calls': 0, 'k': 0}

        def emit_expert(e):
            if e < 64:
                a1, a3, a2 = w1[l, e], w3[l, e], w2[l, e]
            else:
                a1, a3, a2 = sw1[l], sw3[l], sw2[l]
            rows = slice(e * 128, (e + 1) * 128)
            for part, src, dst in ((0, a1, WB['ap'][rows, 0:6144]), (1, a3, WB['ap'][rows, 6144:12288]), (2, a2, WB['ap2'][rows, :])):
                t = bufs[state['k'] % 4]
                state['k'] += 1
                if part < 2:
                    self.dma(t.t[:].rearrange("p (c f) -> p c f", c=16), src.rearrange("(c p) f -> p c f", p=128), writes=[t.r], q='pool')
                else:
                    self.dma(t.t[:].rearrange("p (c n) -> p c n", c=3), src.rearrange("(c p) n -> p c n", p=128), writes=[t.r], q='pool')
                self.dma(dst, t.t[:], reads=[t.r], pwrites=[WB['r']])

        def hook(final=False):
            state['calls'] += 1
            target = 65 if final else min(65, (state['calls'] * 65 + total_calls - 1) // total_calls)
            while state['e'] < target:
                emit_expert(state['e'])
                state['e'] += 1
        return hook

    def stage_moe_sparse(self, H, F, MODB, l, router, rbias, WB, VF, TOKBUF, YB, with_ctx=True):
        BS = 512
        ntile = NT if with_ctx else 32
        ntok = ntile * 128
        NBLK = (ntok * 6 + 64 * (BS - 1) + BS - 1) // BS
        MAXB = (ntok + BS - 1) // BS
        NSH = (ntile + 3) // 4
        with contextlib.ExitStack() as sc0:
            Mall = self.sb(sc0, [128, NT, 64], F32, 'Mall')
            Gall = self.sb(sc0, [128, NT, 64], F32, 'Gall')
            Rall = self.sb(sc0, [128, NT, 64], F32, 'Rall')
            DKu = self.sb(sc0, [128, NT, 6], U32, 'DKu')
            GK = self.sb(sc0, [128, NT, 6], F32, 'GK')
            idxW = self.sb(sc0, [128, 128], U32, 'idxW')
            base = self.sb(sc0, [128, 64], F32, 'base')
            dbase = self.sb(sc0, [128, 64], F32, 'dbase')
            UT = self.sb(sc0, [128, 128], BF16, 'UT')
            ipf = self.sb(sc0, [128, 1], F32, 'ipf')
            with contextlib.ExitStack() as sc:
                ut32 = self.sb(sc, [128, 128], F32, 'ut32')
                self.memset('pool', ut32.t[:], 1.0, writes=[ut32.r])
                self.P.add('pool', lambda e: e.affine_select(out=ut32.t[:], in_=ut32.t[:], pattern=[[1, 128]], compare_op=ALU.is_gt,
                                                             fill=0.0, base=0, channel_multiplier=-1), reads=[ut32.r], writes=[ut32.r])
                self.cp('dve', UT.t[:], ut32.t[:], reads=[ut32.r], writes=[UT.r])
                ip = self.sb(sc, [128, 1], I32, 'ip')
                self.P.add('pool', lambda e: e.iota(ip.t[:], pattern=[[0, 1]], base=0, channel_multiplier=1), writes=[ip.r])
                self.cp('dve', ipf.t[:], ip.t[:], reads=[ip.r], writes=[ipf.r])
                self.memset('pool', base.t[:], 0.0, writes=[base.r])
                rt32 = self.sb(sc, [128, 16, 64], F32, 'rt32')
                rb = self.load_bcast(sc, rbias, 64, 'rb')
                self.dma(rt32.t[:], router.rearrange("(c p) e -> p c e", p=128), writes=[rt32.r])
                mods = self.load_mods(sc, MODB, l, [8192, 6144])
                msc, msh = mods[8192], mods[6144]
                hb = [self.sb(sc, [128, 2048], F32, 'hb') for _ in range(2)]
                vf32 = [self.sb(sc, [128, 2048], F32, 'vf32') for _ in range(2)]
                ub = [self.sb(sc, [128, 2048], BF16, 'ub') for _ in range(2)]
                vT32 = self.sb(sc, [128, 16, 128], F32, 'vT32')
                g1 = self.sb(sc, [128, 64], F32, 'g1')
                g2 = self.sb(sc, [128, 64], F32, 'g2')
                g3 = self.sb(sc, [128, 16], F32, 'g3')
                mb = self.sb(sc, [128, 64], BF16, 'mb')
                for ti in range(ntile):
                    which = 0 if ti < 32 else 1
                    h, v, u = hb[ti % 2], vf32[ti % 2], ub[ti % 2]
                    rows = slice(ti * 128, (ti + 1) * 128)
                    self.dma(h.t[:], H['ap'][rows, :], reads=[H['r'][ti]], writes=[h.r])
                    self.tt('dve', v.t[:], h.t[:], msc.t[:, which, :], ALU.mult, reads=[h.r, msc.r], writes=[v.r])
                    self.tt('dve', v.t[:], v.t[:], msh.t[:, which, :], ALU.add, reads=[v.r, msh.r], writes=[v.r])
                    self.cp('act', u.t[:], v.t[:], reads=[v.r], writes=[u.r])
                    self.dma(VF['ap'][rows, :], u.t[:], reads=[u.r], pwrites=[VF['r']])
                    for q4 in range(4):
                        pbk = self.pb[q4 % 2]
                        for q in range(4):
                            kc = q4 * 4 + q
                            self.tr(pbk.t[:, q * 128:(q + 1) * 128], v.t[:, kc * 128:(kc + 1) * 128], self.ident32.t[:],
                                    reads=[v.r, self.ident32.r], writes=[pbk.r] if q == 0 else [], pwrites=[] if q == 0 else [pbk.r], inc=(q == 3))
                        self.cp('act' if q4 % 2 else 'dve', vT32.t[:, q4 * 4:(q4 + 1) * 4, :], pbk.t[:].rearrange("p (q k) -> p q k", q=4),
                                reads=[pbk.r], pwrites=[vT32.r])
                    pr = self.pb[2]
                    for kc in range(16):
                        self.mm(pr.t[:, 0:64], vT32.t[:, kc, :], rt32.t[:, kc, :], kc == 0, kc == 15,
                                reads=[vT32.r, rt32.r], writes=[pr.r] if kc == 0 else [], pwrites=[] if kc == 0 else [pr.r], inc=(kc == 15))
                    self.act(g1.t[:], pr.t[:, 0:64], AF.Sigmoid, reads=[pr.r], writes=[g1.r])
                    self.tt('dve', g2.t[:], g1.t[:], rb.t[:], ALU.add, reads=[g1.r, rb.r], writes=[g2.r])
                    self.P.add('dve', lambda e: e.max(out=g3.t[:, 0:8], in_=g2.t[:]), reads=[g2.r], writes=[g3.r])
                    self.ts('dve', Mall.t[:, ti, :], g2.t[:], g3.t[:, 5:6], None, ALU.is_ge, reads=[g2.r, g3.r], pwrites=[Mall.r])
                    self.tt('dve', g1.t[:], g1.t[:], Mall.t[:, ti, :], ALU.mult, reads=[g1.r, Mall.r], writes=[g1.r])
                    self.P.add('dve', lambda e: e.reduce_sum(out=g3.t[:, 8:9], in_=g1.t[:], axis=AX.X), reads=[g1.r], writes=[g3.r])
                    self.recip(g3.t[:, 9:10], g3.t[:, 8:9], reads=[g3.r], writes=[g3.r])
                    self.ts('dve', Gall.t[:, ti, :], g1.t[:], g3.t[:, 9:10], 2.5, ALU.mult, ALU.mult, reads=[g1.r, g3.r], pwrites=[Gall.r])
                    self.cp('dve', mb.t[:], Mall.t[:, ti, :], reads=[Mall.r], writes=[mb.r])
                    pk = self.pb[3]
                    self.mm(pk.t[:, 0:64], UT.t[:], mb.t[:], True, True, reads=[UT.r, mb.r], writes=[pk.r], inc=False)
                    self.mm(pk.t[:, 64:128], self.ones16.t[:], mb.t[:], True, True, reads=[self.ones16.r, mb.r], pwrites=[pk.r])
                    self.tt('dve', Rall.t[:, ti, :], pk.t[:, 0:64], base.t[:], ALU.add, reads=[pk.r, base.r], pwrites=[Rall.r])
                    self.tt('dve', base.t[:], base.t[:], pk.t[:, 64:128], ALU.add, reads=[pk.r, base.r], writes=[base.r])
            self.P.barrier()
            with contextlib.ExitStack() as sc:
                fi = self.sb(sc, [128, 128], I32, 'fi')
                ff = self.sb(sc, [128, 128], F32, 'ff')
                self.P.add('pool', lambda e: e.iota(fi.t[:], pattern=[[1, 128]], base=0, channel_multiplier=0), writes=[fi.r])
                self.cp('dve', ff.t[:], fi.t[:], reads=[fi.r], writes=[ff.r])
                thr = self.sb(sc, [128, 16], F32, 'thr')
                self.ts('dve', thr.t[:], ff.t[:, 0:16], float(BS), None, ALU.mult, reads=[ff.r], writes=[thr.r])
                cmp = self.sb(sc, [128, 64, MAXB], F32, 'cmp')
                self.tt('dve', cmp.t[:], base.t[:].unsqueeze(2).to_broadcast([128, 64, MAXB]),
                        thr.t[:, 0:MAXB].unsqueeze(1).to_broadcast([128, 64, MAXB]), ALU.is_gt, reads=[base.r, thr.r], writes=[cmp.r])
                nblk = self.sb(sc, [128, 64], F32, 'nblk')
                self.P.add('dve', lambda e: e.reduce_sum(out=nblk.t[:], in_=cmp.t[:], axis=AX.X), reads=[cmp.r], writes=[nblk.r])
                xa = self.sb(sc, [128, 64], F32, 'xa')
                xb = self.sb(sc, [128, 64], F32, 'xb')
                self.cp('dve', xa.t[:], nblk.t[:], reads=[nblk.r], writes=[xa.r])
                cur, oth = xa, xb
                for s in (1, 2, 4, 8, 16, 32):
                    self.cp('dve', oth.t[:, 0:s], cur.t[:, 0:s], reads=[cur.r], writes=[oth.r])
                    self.tt('dve', oth.t[:, s:64], cur.t[:, s:64], cur.t[:, 0:64 - s], ALU.add, reads=[cur.r], pwrites=[oth.r])
                    cur, oth = oth, cur
                pend = cur
                self.tt('dve', dbase.t[:], pend.t[:], nblk.t[:], ALU.subtract, reads=[pend.r, nblk.r], writes=[dbase.r])
                self.ts('dve', dbase.t[:], dbase.t[:], float(BS), None, ALU.mult, reads=[dbase.r], writes=[dbase.r])
                cmp2 = self.sb(sc, [128, 128, 64], F32, 'cmp2')
                self.tt('dve', cmp2.t[:], pend.t[:].unsqueeze(1).to_broadcast([128, 128, 64]),
                        ff.t[:].unsqueeze(2).to_broadcast([128, 128, 64]), ALU.is_le, reads=[pend.r, ff.r], writes=[cmp2.r])
                be = self.sb(sc, [128, 128], F32, 'be')
                self.P.add('dve', lambda e: e.reduce_sum(out=be.t[:], in_=cmp2.t[:], axis=AX.X), reads=[cmp2.r], writes=[be.r])
                self.ts('dve', be.t[:], be.t[:], 63.0, 128.0, ALU.min, ALU.mult, reads=[be.r], writes=[be.r])
                self.ts('dve', be.t[:], be.t[:], ipf.t[:, 0:1], None, ALU.add, reads=[be.r, ipf.r], writes=[be.r])
                self.cp('dve', idxW.t[:], be.t[:], reads=[be.r], writes=[idxW.r])
                zt = self.sb(sc, [128, NBLK, 16], I32, 'zt')
                self.memset('pool', zt.t[:], 0, writes=[zt.r])
                for i in range(4):
                    self.dma(TOKBUF['ap'][i * NBLK * 128:(i + 1) * NBLK * 128, :].rearrange("(a p) c -> p a c", p=128), zt.t[:], reads=[zt.r], pwrites=[TOKBUF['r']])
            self.P.barrier()
            with contextlib.ExitStack() as sc:
                dest = [self.sb(sc, [128, 64], F32, 'dest') for _ in range(2)]
                ca = [self.sb(sc, [128, 64], F32, 'ca') for _ in range(2)]
                cb_ = [self.sb(sc, [128, 64], F32, 'cb') for _ in range(2)]
                oh = [self.sb(sc, [128, 64], F32, 'oh') for _ in range(2)]
                o2 = [self.sb(sc, [128, 64], F32, 'o2') for _ in range(2)]
                dk = [self.sb(sc, [128, 8], F32, 'dk') for _ in range(2)]
                src = [self.sb(sc, [128, 16], I32, 'src') for _ in range(2)]
                for ti in range(ntile):
                    k2 = ti % 2
                    d_, s_ = dest[k2], src[k2]
                    self.tt('dve', d_.t[:], Rall.t[:, ti, :], dbase.t[:], ALU.add, reads=[Rall.r, dbase.r], writes=[d_.r])
                    cur, oth = ca[k2], cb_[k2]
                    self.cp('dve', cur.t[:], Mall.t[:, ti, :], reads=[Mall.r], writes=[cur.r])
                    for s in (1, 2, 4, 8, 16, 32):
                        self.cp('dve', oth.t[:, 0:s], cur.t[:, 0:s], reads=[cur.r], writes=[oth.r])
                        self.tt('dve', oth.t[:, s:64], cur.t[:, s:64], cur.t[:, 0:64 - s], ALU.add, reads=[cur.r], pwrites=[oth.r])
                        cur, oth = oth, cur
                    for k in range(6):
                        o_, p_ = oh[k % 2], o2[k % 2]
                        self.ts('dve', o_.t[:], cur.t[:], float(k + 1), None, ALU.is_equal, reads=[cur.r], writes=[o_.r])
                        self.tt('dve', o_.t[:], o_.t[:], Mall.t[:, ti, :], ALU.mult, reads=[o_.r, Mall.r], writes=[o_.r])
                        self.tt('dve', p_.t[:], o_.t[:], d_.t[:], ALU.mult, reads=[o_.r, d_.r], writes=[p_.r])
                        self.P.add('dve', lambda e, p_=p_, dkt=dk[k2], k=k: e.reduce_sum(out=dkt.t[:, k:k + 1], in_=p_.t[:], axis=AX.X), reads=[p_.r], pwrites=[dk[k2].r])
                        self.tt('dve', p_.t[:], o_.t[:], Gall.t[:, ti, :], ALU.mult, reads=[o_.r, Gall.r], writes=[p_.r])
                        self.P.add('dve', lambda e, p_=p_, ti=ti, k=k: e.reduce_sum(out=GK.t[:, ti, k:k + 1], in_=p_.t[:], axis=AX.X), reads=[p_.r], pwrites=[GK.r])
                    self.cp('dve', DKu.t[:, ti, :], dk[k2].t[:, 0:6], reads=[dk[k2].r], pwrites=[DKu.r])
                    self.P.add('pool', lambda e, s_=s_, ti=ti: e.iota(s_.t[:], pattern=[[0, 16]], base=ti * 128, channel_multiplier=1), writes=[s_.r])
                    for k in range(6):
                        self.P.add('pool', lambda e, s_=s_, ti=ti, k=k: e.indirect_dma_start(
                            out=TOKBUF['ap'], out_offset=bass.IndirectOffsetOnAxis(ap=DKu.t[:, ti, k:k + 1], axis=0), in_=s_.t[:], in_offset=None),
                            reads=[DKu.r, s_.r], pwrites=[TOKBUF['r']], dma=True)
            self.P.barrier()
            with contextlib.ExitStack() as sc:
                tb = [self.sb(sc, [128, 4, 16], I32, 'tb') for _ in range(2)]
                xg = [self.sb(sc, [128, 2048], BF16, 'xg') for _ in range(3)]
                xgT = [self.sb(sc, [128, 16, 512], BF16, 'xgT') for _ in range(2)]
                wt = [self.sb(sc, [128, 18432], BF16, 'wt') for _ in range(2)]
                aT = [self.sb(sc, [128, 3, 512], BF16, 'aT') for _ in range(2)]
                silt = [self.sb(sc, [128, 512], F32, 'silt') for _ in range(2)]
                yb = [self.sb(sc, [128, 2048], BF16, 'yb') for _ in range(2)]
                cx = 0
                cA = 0
                cY = 0
                cT = 0
                for b in range(NBLK + NSH):
                    routed = b < NBLK
                    w = wt[b % 2] if routed else wt[NBLK % 2]
                    xT = xgT[b % 2]
                    if routed:
                        t_ = tb[b % 2]
                        self.dma(t_.t[:], TOKBUF['ap'][b * BS:(b + 1) * BS, :].rearrange("(i p) c -> p i c", p=128), reads=[TOKBUF['r']], writes=[t_.r])
                        self.P.add('pool', lambda e, w=w, b=b: e.indirect_dma_start(
                            out=w.t[:, 0:12288], out_offset=None, in_=WB['ap'], in_offset=bass.IndirectOffsetOnAxis(ap=idxW.t[:, b:b + 1], axis=0)),
                            reads=[idxW.r, WB['r']], writes=[w.r], dma=True)
                        self.P.add('pool', lambda e, w=w, b=b: e.indirect_dma_start(
                            out=w.t[:, 12288:18432], out_offset=None, in_=WB['ap2'], in_offset=bass.IndirectOffsetOnAxis(ap=idxW.t[:, b:b + 1], axis=0)),
                            reads=[idxW.r, WB['r']], pwrites=[w.r], dma=True)
                        nsub = 4
                    else:
                        sbi = b - NBLK
                        if sbi == 0:
                            self.dma(w.t[:, 0:12288], WB['ap'][64 * 128:65 * 128, :], reads=[WB['r']], writes=[w.r])
                            self.dma(w.t[:, 12288:18432], WB['ap2'][64 * 128:65 * 128, :], reads=[WB['r']], pwrites=[w.r])
                        nsub = min(4, ntile - sbi * 4)
                    ns = nsub * 128
                    for i in range(nsub):
                        x = xg[cx % 3]
                        cx += 1
                        if routed:
                            self.P.add('pool', lambda e, x=x, t_=t_, i=i: e.indirect_dma_start(
                                out=x.t[:], out_offset=None, in_=VF['ap'], in_offset=bass.IndirectOffsetOnAxis(ap=t_.t[:, i, 0:1].bitcast(U32), axis=0)),
                                reads=[t_.r, VF['r']], writes=[x.r], dma=True)
                        else:
                            ti = (b - NBLK) * 4 + i
                            self.dma(x.t[:], VF['ap'][ti * 128:(ti + 1) * 128, :], reads=[VF['r']], writes=[x.r])
                        for half in range(2):
                            pbk = self.pb[4 + cT % 2]
                            cT += 1
                            pv = pbk.t[:].bitcast(BF16)
                            for q in range(8):
                                kc = half * 8 + q
                                self.tr(pv[:, q * 128:(q + 1) * 128], x.t[:, kc * 128:(kc + 1) * 128], self.ident16.t[:],
                                        reads=[x.r, self.ident16.r], writes=[pbk.r] if q == 0 else [], pwrites=[] if q == 0 else [pbk.r], inc=(q == 7))
                            self.cp('act' if half else 'dve', xT.t[:, half * 8:(half + 1) * 8, i * 128:(i + 1) * 128], pv.rearrange("p (q k) -> p q k", q=8),
                                    reads=[pbk.r], pwrites=[xT.r])
                    at = aT[b % 2]
                    for fc in range(3):
                        A = self.pb[(cA * 2) % 4]
                        B = self.pb[(cA * 2 + 1) % 4]
                        sl = silt[cA % 2]
                        cA += 1
                        for kc in range(16):
                            o1 = kc * 384 + fc * 128
                            self.mm(A.t[:, 0:ns], w.t[:, o1:o1 + 128], xT.t[:, kc, 0:ns], kc == 0, kc == 15,
                                    reads=[w.r, xT.r], writes=[A.r] if kc == 0 else [], pwrites=[] if kc == 0 else [A.r], inc=(kc == 15))
                        for kc in range(16):
                            o3 = 6144 + kc * 384 + fc * 128
                            self.mm(B.t[:, 0:ns], w.t[:, o3:o3 + 128], xT.t[:, kc, 0:ns], kc == 0, kc == 15,
                                    reads=[w.r, xT.r], writes=[B.r] if kc == 0 else [], pwrites=[] if kc == 0 else [B.r], inc=(kc == 15))
                        self.act(sl.t[:, 0:ns], A.t[:, 0:ns], AF.Silu, reads=[A.r], writes=[sl.r])
                        self.tt('dve', at.t[:, fc, 0:ns], sl.t[:, 0:ns], B.t[:, 0:ns], ALU.mult, reads=[sl.r, B.r], pwrites=[at.r])
                    for i in range(nsub):
                        y = yb[cY % 2]
                        for cb in range(4):
                            Yp = self.pb[4 + cY % 4] if False else self.pb[4 + (cY * 4 + cb) % 4]
                            for fc in range(3):
                                o2_ = 12288 + fc * 2048 + cb * 512
                                self.mm(Yp.t[:], at.t[:, fc, i * 128:(i + 1) * 128], w.t[:, o2_:o2_ + 512], fc == 0, fc == 2,
                                        reads=[at.r, w.r], writes=[Yp.r] if fc == 0 else [], pwrites=[] if fc == 0 else [Yp.r], inc=(fc == 2))
                            self.cp('act' if cb % 2 else 'dve', y.t[:, cb * 512:(cb + 1) * 512], Yp.t[:], reads=[Yp.r], pwrites=[y.r])
                        cY += 1
                        if routed:
                            r0 = b * BS + i * 128
                            self.dma(YB['ap'][r0:r0 + 128, :], y.t[:], reads=[y.r], pwrites=[YB['r']])
                        else:
                            r0 = ((b - NBLK) * 4 + i) * 128
                            self.dma(YB['ap2'][r0:r0 + 128, :], y.t[:], reads=[y.r], pwrites=[YB['r']])
            self.P.barrier()
            with contextlib.ExitStack() as sc:
                acc = [self.sb(sc, [128, 2048], F32, 'acc') for _ in range(2)]
                ysh = [self.sb(sc, [128, 2048], BF16, 'ysh') for _ in range(2)]
                yk = [self.sb(sc, [128, 2048], BF16, 'yk') for _ in range(4)]
                cg = 0
                for ti in range(ntile):
                    a, s = acc[ti % 2], ysh[ti % 2]
                    r0 = ti * 128
                    self.dma(s.t[:], YB['ap2'][r0:r0 + 128, :], reads=[YB['r']], writes=[s.r])
                    for k in range(6):
                        y = yk[cg % 4]
                        cg += 1
                        self.P.add('pool', lambda e, y=y, ti=ti, k=k: e.indirect_dma_start(
                            out=y.t[:], out_offset=None, in_=YB['ap'], in_offset=bass.IndirectOffsetOnAxis(ap=DKu.t[:, ti, k:k + 1], axis=0)),
                            reads=[DKu.r, YB['r']], writes=[y.r], dma=True)
                        self.stt('dve', a.t[:], y.t[:], GK.t[:, ti, k:k + 1], s.t[:] if k == 0 else a.t[:], ALU.mult, ALU.add,
                                 reads=[y.r, GK.r, s.r, a.r] if k == 0 else [y.r, GK.r, a.r], writes=[a.r])
                    self.dma(F['ap'][ti * 128:(ti + 1) * 128, :], a.t[:], reads=[a.r], writes=[F['r'][ti]])
        self.P.barrier()

    def stage_escan_chunked(self, SCN, V32, YS, nch=68):
        C = 64
        with contextlib.ExitStack() as sc:
            slots = [Tl(self.pp[i][:, s * 512:s * 512 + 128]) for i in range(4) for s in range(2)]
            ia_i = self.sb(sc, [128, 1], I32, 'ia_i')
            ia = self.sb(sc, [128, 1], F32, 'ia')
            ib_i = self.sb(sc, [128, 128], I32, 'ib_i')
            ib = self.sb(sc, [128, 128], F32, 'ib')
            tq = self.sb(sc, [128, 128], F32, 'tq')
            self.P.add('pool', lambda e: e.iota(ia_i.t[:], pattern=[[0, 1]], base=0, channel_multiplier=1), writes=[ia_i.r])
            self.cp('dve', ia.t[:], ia_i.t[:], reads=[ia_i.r], writes=[ia.r])
            self.ts('dve', tq.t[:, 0:1], ia.t[:], 64.0, -64.0, ALU.is_ge, ALU.mult, reads=[ia.r], writes=[tq.r])
            self.tt('dve', ia.t[:], ia.t[:], tq.t[:, 0:1], ALU.add, reads=[ia.r, tq.r], writes=[ia.r])
            self.P.add('pool', lambda e: e.iota(ib_i.t[:], pattern=[[1, 128]], base=0, channel_multiplier=0), writes=[ib_i.r])
            self.cp('dve', ib.t[:], ib_i.t[:], reads=[ib_i.r], writes=[ib.r])
            self.ts('dve', tq.t[:], ib.t[:], 64.0, -64.0, ALU.is_ge, ALU.mult, reads=[ib.r, tq.r], writes=[tq.r])
            self.tt('dve', ib.t[:], ib.t[:], tq.t[:], ALU.add, reads=[ib.r, tq.r], writes=[ib.r])
            masks = {}
            for nm, op, val in (('US', ALU.is_gt, 1.0), ('LS', ALU.is_lt, 1.0), ('UI', ALU.is_ge, 1.0), ('LI', ALU.is_le, 1.0),
                                ('nUI', ALU.is_ge, -1.0), ('nLI', ALU.is_le, -1.0)):
                m = self.sb(sc, [128, 128], F32, 'mask' + nm)
                self.ts('dve', m.t[:], ib.t[:], ia.t[:, 0:1], val, op, ALU.mult, reads=[ib.r, ia.r], writes=[m.r])
                masks[nm] = m
            zeros = self.sb(sc, [128, C], F32, 'zeros')
            self.memset('pool', zeros.t[:], 0.0, writes=[zeros.r])
            G32 = [[self.sb(sc, [128, C], F32, 'G32') for j in range(8)] for d in range(2)]
            G16 = [[self.sb(sc, [128, C], BF16, 'G16') for j in range(8)] for d in range(2)]
            for d in range(2):
                for j in range(8):
                    self.memset('pool', G32[d][j].t[:], 0.0, writes=[G32[d][j].r])
                    self.memset('pool', G16[d][j].t[:], 0.0, writes=[G16[d][j].r])
            qd = [[self.sb(sc, [128, 5, 8, C], F32, 'qd') for _ in range(2)] for d in range(2)]
            v32 = [[self.sb(sc, [128, 8, C], F32, 'v32') for _ in range(2)] for d in range(2)]
            v16 = [[self.sb(sc, [128, 8, C], BF16, 'v16') for _ in range(2)] for d in range(2)]
            ych = [[self.sb(sc, [128, 8, C], F32, 'ych') for _ in range(2)] for d in range(2)]
            NB = 2

            def mat(dt, name):
                return [[[self.sb(sc, [128, 128], dt, name) for _ in range(NB)] for j in range(8)] for d in range(2)]
            KKe, REe, BBe, KEe = mat(BF16, 'KKe'), mat(BF16, 'REe'), mat(BF16, 'BBe'), mat(BF16, 'KEe')
            LkT, nMrbT, MrkT = mat(BF16, 'LkT'), mat(BF16, 'nMrbT'), mat(BF16, 'MrkT')
            KEt, nBBt = mat(BF16, 'KEt'), mat(BF16, 'nBBt')
            InvT = mat(F32, 'InvT')
            tot = [[[self.sb(sc, [128, 1], F32, 'tot') for _ in range(NB)] for j in range(8)] for d in range(2)]
            for arr in (KKe, REe, BBe, KEe):
                for d in range(2):
                    for j in range(8):
                        for b in range(NB):
                            self.memset('pool', arr[d][j][b].t[:], 0.0, writes=[arr[d][j][b].r])
            Qt = [self.sb(sc, [128, C + 1], F32, 'Qt') for _ in range(2)]
            Qi = [self.sb(sc, [128, C + 1], F32, 'Qi') for _ in range(2)]
            Pb = [self.sb(sc, [128, C + 1], F32, 'Pb') for _ in range(2)]
            Pv = [self.sb(sc, [128, C], F32, 'Pv') for _ in range(2)]
            Nm = [self.sb(sc, [128, 128], F32, 'Nm') for _ in range(2)]
            NTm = [self.sb(sc, [128, 128], F32, 'NTm') for _ in range(2)]
            Np = [self.sb(sc, [128, 128], F32, 'Np') for _ in range(4)]
            NTp = [self.sb(sc, [128, 128], F32, 'NTp') for _ in range(4)]
            Xs = [self.sb(sc, [128, 128], F32, 'Xs') for _ in range(4)]
            rhs_sb = [self.sb(sc, [128, C], F32, 'rhs_sb') for _ in range(4)]
            u16 = [self.sb(sc, [128, C], BF16, 'u16') for _ in range(4)]
            gtmp = [self.sb(sc, [128, C], F32, 'gtmp') for _ in range(4)]
            for q in Qt + Qi:
                self.memset('pool', q.t[:, 0:1], 1.0, writes=[q.r])
            cnt = {'s': 0, 'e': 0, 't': 0}

            def slot():
                s = slots[cnt['s'] % 8]
                cnt['s'] += 1
                return s

            def ev():
                cnt['e'] += 1
                return 'act' if cnt['e'] % 2 else 'dve'

            def chunk_ranges(c):
                if c < 4:
                    return (4144 + 64 * c, 4144 + 192 - 64 * c, 4096 + 64 * c, 4096 + 192 - 64 * c)
                cc = c - 4
                return (16 + 64 * cc, 16 + 4096 - 64 * (cc + 1), 64 * cc, 4096 - 64 * (cc + 1))

            def load_chunk(c):
                cf0, cb0, tf0, tb0 = chunk_ranges(c)
                for d, c0, t0 in ((0, cf0, tf0), (1, cb0, tb0)):
                    qb = qd[d][c % 2]
                    self.dma(qb.t[:, 0:2, :, :], SCN['ap'][0:2, :, :, c0:c0 + C].rearrange("q j p c -> p q j c"), reads=[SCN['r']], pwrites=[qb.r])
                    self.dma(qb.t[:, 2:5, :, :], SCN['ap'][2 + 3 * d:5 + 3 * d, :, :, c0:c0 + C].rearrange("q j p c -> p q j c"), reads=[SCN['r']], pwrites=[qb.r])
                    vb = v32[d][c % 2]
                    for h2 in range(2):
                        src = V32['ap'][t0:t0 + C, :].rearrange("s (j h v) -> s j h v", j=8, h=2)[:, :, h2, :]
                        self.dma(vb.t[h2 * 64:(h2 + 1) * 64, :, :], src, reads=[V32['r']], pwrites=[vb.r])
                    self.cp('act', v16[d][c % 2].t[:], vb.t[:], reads=[vb.r], writes=[v16[d][c % 2].r])

            def precompute(c, d, j):
                b = c % NB
                qb = qd[d][c % 2]
                k2 = cnt['t'] % 2
                cnt['t'] += 1
                qt, qi, pb_, pv = Qt[k2], Qi[k2], Pb[k2], Pv[k2]
                r_, kk_, w_, b_, k_ = (qb.t[:, qi_, j, :] for qi_ in range(5))
                self.P.add('dve', lambda e: e.tensor_tensor_scan(out=qt.t[:, 1:C + 1], data0=w_, data1=zeros.t[:], initial=1.0, op0=ALU.mult, op1=ALU.add),
                           reads=[qb.r, zeros.r], pwrites=[qt.r])
                self.recip(qi.t[:, 1:C + 1], qt.t[:, 1:C + 1], reads=[qt.r], pwrites=[qi.r])
                tt_ = tot[d][j][b]
                self.cp('dve', tt_.t[:], qt.t[:, C:C + 1], reads=[qt.r], writes=[tt_.r])
                if d == 0:
                    P_, Pp_, Pi_ = qt.t[:, 1:C + 1], qt.t[:, 0:C], qi.t[:, 1:C + 1]
                    rd = [qt.r, qi.r]
                else:
                    self.ts('dve', pb_.t[:], qi.t[:], qt.t[:, C:C + 1], None, ALU.mult, reads=[qi.r, qt.r], writes=[pb_.r])
                    self.ts('dve', pv.t[:], qt.t[:, 0:C], qi.t[:, C:C + 1], None, ALU.mult, reads=[qi.r, qt.r], writes=[pv.r])
                    P_, Pp_, Pi_ = pb_.t[:, 0:C], pb_.t[:, 1:C + 1], pv.t[:]
                    rd = [pb_.r, pv.r]
                n_ = 0
                for (dst, src, fac) in ((REe, r_, P_), (KKe, kk_, Pp_), (BBe, b_, Pi_), (KEe, k_, Pi_)):
                    for h2 in range(2):
                        ps_ = slice(h2 * 64, (h2 + 1) * 64)
                        eng = 'pool' if n_ % 2 else 'dve'
                        n_ += 1
                        self.tt(eng, dst[d][j][b].t[ps_, h2 * 64:(h2 + 1) * 64], src[ps_], fac[ps_], ALU.mult, reads=[qb.r] + rd, pwrites=[dst[d][j][b].r])
                kke, ree, bbe, kee = KKe[d][j][b], REe[d][j][b], BBe[d][j][b], KEe[d][j][b]
                mS_T, mS, mI = (('US', 'LS', 'UI') if d == 0 else ('LS', 'US', 'LI'))
                nm, ntm = Nm[k2], NTm[k2]
                for (lh, rh, mk_, dst_) in ((bbe, kke, masks[mS_T], nm), (kke, bbe, masks[mS], ntm), (kee, kke, masks[mS_T], LkT[d][j][b]),
                                            (bbe, ree, masks['n' + mI], nMrbT[d][j][b]), (kee, ree, masks[mI], MrkT[d][j][b])):
                    s = slot()
                    self.mm(s.t[:], lh.t[:], rh.t[:], True, True, reads=[lh.r, rh.r], writes=[s.r])
                    self.tt('dve', dst_.t[:], s.t[:], mk_.t[:], ALU.mult, reads=[s.r, mk_.r], writes=[dst_.r])
                for (src_, dst_, neg) in ((kee, KEt[d][j][b], False), (bbe, nBBt[d][j][b], True)):
                    s = slot()
                    pv_ = s.t[:].bitcast(BF16)[:, 0:128]
                    self.tr(pv_, src_.t[:], self.ident16.t[:], reads=[src_.r, self.ident16.r], writes=[s.r])
                    if neg:
                        self.act(dst_.t[:], pv_, AF.Copy, reads=[s.r], writes=[dst_.r], scale=-1.0)
                    else:
                        self.cp('act', dst_.t[:], pv_, reads=[s.r], writes=[dst_.r])
                x = Xs[(cnt['t'] * 2) % 4]
                self.tt('dve', x.t[:], self.ident32.t[:], nm.t[:], ALU.subtract, reads=[self.ident32.r, nm.r], writes=[x.r])
                curN, curNT = nm, ntm
                for lvl in range(5):
                    nNT = NTp[lvl % 4] if lvl < 4 else NTp[0]
                    s = slot()
                    self.mm(s.t[:], curN.t[:], curNT.t[:], True, True, reads=[curN.r, curNT.r], writes=[s.r])
                    self.cp(ev(), nNT.t[:], s.t[:], reads=[s.r], writes=[nNT.r])
                    if lvl < 4:
                        nN = Np[lvl % 4]
                        s = slot()
                        self.mm(s.t[:], curNT.t[:], curN.t[:], True, True, reads=[curN.r, curNT.r], writes=[s.r])
                        self.cp(ev(), nN.t[:], s.t[:], reads=[s.r], writes=[nN.r])
                    s = slot()
                    self.mm(s.t[:], nNT.t[:], x.t[:], True, True, reads=[nNT.r, x.r], writes=[s.r])
                    xn = InvT[d][j][b] if lvl == 4 else Xs[(cnt['t'] * 2 + 1 + lvl) % 4]
                    if xn is x:
                        xn = Xs[(cnt['t'] * 2 + 2 + lvl) % 4]
                    self.tt('dve', xn.t[:], s.t[:], x.t[:], ALU.add, reads=[s.r, x.r], writes=[xn.r])
                    x = xn
                    curNT = nNT
                    if lvl < 4:
                        curN = nN

            def sequential(c, d, j):
                b = c % NB
                g32, g16 = G32[d][j], G16[d][j]
                vj = v16[d][c % 2]
                k4 = cnt['e'] % 4
                rs, u_, gt_ = rhs_sb[k4], u16[k4], gtmp[k4]
                s = slot()
                self.mm(s.t[:, 0:C], KKe[d][j][b].t[:], g16.t[:], True, False, reads=[KKe[d][j][b].r, g16.r], writes=[s.r], inc=False)
                self.mm(s.t[:, 0:C], LkT[d][j][b].t[:], vj.t[:, j, :], False, True, reads=[LkT[d][j][b].r, vj.r], pwrites=[s.r])
                self.cp(ev(), rs.t[:], s.t[:, 0:C], reads=[s.r], writes=[rs.r])
                s = slot()
                self.mm(s.t[:, 0:C], InvT[d][j][b].t[:], rs.t[:], True, True, reads=[InvT[d][j][b].r, rs.r], writes=[s.r])
                self.cp(ev(), u_.t[:], s.t[:, 0:C], reads=[s.r], writes=[u_.r])
                s = slot()
                self.mm(s.t[:, 0:C], REe[d][j][b].t[:], g16.t[:], True, False, reads=[REe[d][j][b].r, g16.r], writes=[s.r], inc=False)
                self.mm(s.t[:, 0:C], nMrbT[d][j][b].t[:], u_.t[:], False, False, reads=[nMrbT[d][j][b].r, u_.r], pwrites=[s.r], inc=False)
                self.mm(s.t[:, 0:C], MrkT[d][j][b].t[:], vj.t[:, j, :], False, True, reads=[MrkT[d][j][b].r, vj.r], pwrites=[s.r])
                yc = ych[d][c % 2]
                self.cp(ev(), yc.t[:, j, :], s.t[:, 0:C], reads=[s.r], pwrites=[yc.r])
                s = slot()
                self.mm(s.t[:, 0:C], KEt[d][j][b].t[:], vj.t[:, j, :], True, False, reads=[KEt[d][j][b].r, vj.r], writes=[s.r], inc=False)
                self.mm(s.t[:, 0:C], nBBt[d][j][b].t[:], u_.t[:], False, True, reads=[nBBt[d][j][b].r, u_.r], pwrites=[s.r])
                self.tt('dve', gt_.t[:], s.t[:, 0:C], g32.t[:], ALU.add, reads=[s.r, g32.r], writes=[gt_.r])
                self.ts('dve', g32.t[:], gt_.t[:], tot[d][j][b].t[:, 0:1], None, ALU.mult, reads=[gt_.r, tot[d][j][b].r], writes=[g32.r])
                self.cp('act', g16.t[:], g32.t[:], reads=[g32.r], writes=[g16.r])

            load_chunk(0)
            for c in range(nch):
                if c + 1 < nch:
                    load_chunk(c + 1)
                for d in range(2):
                    for j in range(8):
                        precompute(c, d, j)
                for d in range(2):
                    for j in range(8):
                        sequential(c, d, j)
                cf0, cb0, tf0, tb0 = chunk_ranges(c)
                for d, t0 in ((0, tf0), (1, tb0)):
                    yc = ych[d][c % 2]
                    for h2 in range(2):
                        dst = YS['ap'][d, t0:t0 + C, :].rearrange("t (j h v) -> t j h v", j=8, h=2)[:, :, h2, :]
                        self.dma(dst, yc.t[h2 * 64:(h2 + 1) * 64, :, :], reads=[yc.r], pwrites=[YS['r']])
        self.P.barrier()

    def stage_escan_chunked2(self, SCN, V32, YS, nch=68, conv=None):
        C = 64
        with contextlib.ExitStack() as sc:
            hook = self.make_conv_hook(sc, *conv, total_calls=nch - 2) if conv else None
            slots = [Tl(self.pp[i][:, s * 512:s * 512 + 128]) for i in range(4) for s in range(2)]
            ia_i = self.sb(sc, [128, 1], I32, 'ia_i')
            ia = self.sb(sc, [128, 1], F32, 'ia')
            ib_i = self.sb(sc, [128, 128], I32, 'ib_i')
            ib = self.sb(sc, [128, 128], F32, 'ib')
            tq = self.sb(sc, [128, 128], F32, 'tq')
            self.P.add('pool', lambda e: e.iota(ia_i.t[:], pattern=[[0, 1]], base=0, channel_multiplier=1), writes=[ia_i.r])
            self.cp('dve', ia.t[:], ia_i.t[:], reads=[ia_i.r], writes=[ia.r])
            self.ts('dve', tq.t[:, 0:1], ia.t[:], 64.0, -64.0, ALU.is_ge, ALU.mult, reads=[ia.r], writes=[tq.r])
            self.tt('dve', ia.t[:], ia.t[:], tq.t[:, 0:1], ALU.add, reads=[ia.r, tq.r], writes=[ia.r])
            self.P.add('pool', lambda e: e.iota(ib_i.t[:], pattern=[[1, 128]], base=0, channel_multiplier=0), writes=[ib_i.r])
            self.cp('dve', ib.t[:], ib_i.t[:], reads=[ib_i.r], writes=[ib.r])
            self.ts('dve', tq.t[:], ib.t[:], 64.0, -64.0, ALU.is_ge, ALU.mult, reads=[ib.r, tq.r], writes=[tq.r])
            self.tt('dve', ib.t[:], ib.t[:], tq.t[:], ALU.add, reads=[ib.r, tq.r], writes=[ib.r])
            masks = {}
            for nm, op, val in (('US', ALU.is_gt, 1.0), ('LS', ALU.is_lt, 1.0), ('UI', ALU.is_ge, 1.0), ('LI', ALU.is_le, 1.0),
                                ('nUI', ALU.is_ge, -1.0), ('nLI', ALU.is_le, -1.0)):
                m = self.sb(sc, [128, 128], F32, 'mask' + nm)
                self.ts('dve', m.t[:], ib.t[:], ia.t[:, 0:1], val, op, ALU.mult, reads=[ib.r, ia.r], writes=[m.r])
                masks[nm] = m
            zeros = self.sb(sc, [128, C], F32, 'zeros')
            self.memset('pool', zeros.t[:], 0.0, writes=[zeros.r])
            units = [(d, j) for d in range(2) for j in range(8)]

            def per_unit(shape, dt, name):
                return {u: self.sb(sc, shape, dt, name) for u in units}
            G32 = per_unit([128, C], F32, 'G32')
            G16 = per_unit([128, C], BF16, 'G16')
            KKe, REe, BBe, KEe = (per_unit([128, 128], BF16, n) for n in ('KKe', 'REe', 'BBe', 'KEe'))
            LkT, nMrbT, MrkT, KEt, nBBt, InvT = (per_unit([128, 128], BF16, n) for n in ('LkT', 'nMrbT', 'MrkT', 'KEt', 'nBBt', 'InvT'))
            Na, NTa, Nb, NTb, Xa, Xb = (per_unit([128, 128], BF16, n) for n in ('Na', 'NTa', 'Nb', 'NTb', 'Xa', 'Xb'))
            tot = per_unit([128, 1], F32, 'tot')
            Qt = per_unit([128, C + 1], F32, 'Qt')
            Qi = per_unit([128, C + 1], F32, 'Qi')
            Pb = {u: self.sb(sc, [128, C + 1], F32, 'Pb') for u in units if u[0] == 1}
            Pv = {u: self.sb(sc, [128, C], F32, 'Pv') for u in units if u[0] == 1}
            rs16 = per_unit([128, C], BF16, 'rs16')
            u16 = per_unit([128, C], BF16, 'u16')
            gtmp = per_unit([128, C], F32, 'gtmp')
            for u in units:
                self.memset('pool', G32[u].t[:], 0.0, writes=[G32[u].r])
                self.memset('pool', G16[u].t[:], 0.0, writes=[G16[u].r])
                for arr in (KKe, REe, BBe, KEe):
                    self.memset('pool', arr[u].t[:], 0.0, writes=[arr[u].r])
                self.memset('pool', Qt[u].t[:, 0:1], 1.0, writes=[Qt[u].r])
                self.memset('pool', Qi[u].t[:, 0:1], 1.0, writes=[Qi[u].r])
            qd = [[self.sb(sc, [128, 5, 8, C], F32, 'qd') for _ in range(2)] for d in range(2)]
            v32 = [[self.sb(sc, [128, 8, C], F32, 'v32') for _ in range(2)] for d in range(2)]
            v16 = [[self.sb(sc, [128, 8, C], BF16, 'v16') for _ in range(2)] for d in range(2)]
            ych = [[self.sb(sc, [128, 8, C], F32, 'ych') for _ in range(2)] for d in range(2)]
            cnt = {'s': 0, 'e': 0}

            def slot():
                s = slots[cnt['s'] % 8]
                cnt['s'] += 1
                return s

            def ev():
                cnt['e'] += 1
                return 'act' if cnt['e'] % 2 else 'dve'

            def chunk_ranges(c):
                if c < 4:
                    return (4144 + 64 * c, 4144 + 192 - 64 * c, 4096 + 64 * c, 4096 + 192 - 64 * c)
                cc = c - 4
                return (16 + 64 * cc, 16 + 4096 - 64 * (cc + 1), 64 * cc, 4096 - 64 * (cc + 1))

            def load_chunk(c):
                cf0, cb0, tf0, tb0 = chunk_ranges(c)
                for d, c0, t0 in ((0, cf0, tf0), (1, cb0, tb0)):
                    qb = qd[d][c % 2]
                    self.dma(qb.t[:, 0:2, :, :], SCN['ap'][0:2, :, :, c0:c0 + C].rearrange("q j p c -> p q j c"), reads=[SCN['r']], pwrites=[qb.r])
                    self.dma(qb.t[:, 2:5, :, :], SCN['ap'][2 + 3 * d:5 + 3 * d, :, :, c0:c0 + C].rearrange("q j p c -> p q j c"), reads=[SCN['r']], pwrites=[qb.r])
                    vb = v32[d][c % 2]
                    for h2 in range(2):
                        src = V32['ap'][t0:t0 + C, :].rearrange("s (j h v) -> s j h v", j=8, h=2)[:, :, h2, :]
                        self.dma(vb.t[h2 * 64:(h2 + 1) * 64, :, :], src, reads=[V32['r']], pwrites=[vb.r])
                    self.cp('act', v16[d][c % 2].t[:], vb.t[:], reads=[vb.r], writes=[v16[d][c % 2].r])

            def mm1(dst_slot, lh, rh, n=128):
                self.mm(dst_slot.t[:, 0:n], lh.t[:], rh if not isinstance(rh, Tl) else rh.t[:], True, True,
                        reads=[lh.r] + ([rh.r] if isinstance(rh, Tl) else []), writes=[dst_slot.r])

            load_chunk(0)
            for c in range(nch):
                if c + 1 < nch:
                    load_chunk(c + 1)
                if hook:
                    hook()
                for u in units:
                    d, j = u
                    qb = qd[d][c % 2]
                    qt, qi = Qt[u], Qi[u]
                    r_, kk_, w_, b_, k_ = (qb.t[:, qi_, j, :] for qi_ in range(5))
                    self.P.add('dve', lambda e, qt=qt, w_=w_: e.tensor_tensor_scan(out=qt.t[:, 1:C + 1], data0=w_, data1=zeros.t[:], initial=1.0, op0=ALU.mult, op1=ALU.add),
                               reads=[qb.r, zeros.r], pwrites=[qt.r])
                    self.recip(qi.t[:, 1:C + 1], qt.t[:, 1:C + 1], reads=[qt.r], pwrites=[qi.r])
                    self.cp('act', tot[u].t[:], qt.t[:, C:C + 1], reads=[qt.r], writes=[tot[u].r])
                    if d == 0:
                        P_, Pp_, Pi_ = qt.t[:, 1:C + 1], qt.t[:, 0:C], qi.t[:, 1:C + 1]
                        rd = [qt.r, qi.r]
                    else:
                        self.ts('dve', Pb[u].t[:], qi.t[:], qt.t[:, C:C + 1], None, ALU.mult, reads=[qi.r, qt.r], writes=[Pb[u].r])
                        self.ts('dve', Pv[u].t[:], qt.t[:, 0:C], qi.t[:, C:C + 1], None, ALU.mult, reads=[qi.r, qt.r], writes=[Pv[u].r])
                        P_, Pp_, Pi_ = Pb[u].t[:, 0:C], Pb[u].t[:, 1:C + 1], Pv[u].t[:]
                        rd = [Pb[u].r, Pv[u].r]
                    n_ = 0
                    for (dst, src, fac) in ((REe, r_, P_), (KKe, kk_, Pp_), (BBe, b_, Pi_), (KEe, k_, Pi_)):
                        for h2 in range(2):
                            ps_ = slice(h2 * 64, (h2 + 1) * 64)
                            eng = 'pool' if n_ % 2 else 'dve'
                            n_ += 1
                            self.tt(eng, dst[u].t[ps_, h2 * 64:(h2 + 1) * 64], src[ps_], fac[ps_], ALU.mult, reads=[qb.r] + rd, pwrites=[dst[u].r])
                for u in units:
                    d, j = u
                    mS_T, mS, mI = (('US', 'LS', 'UI') if d == 0 else ('LS', 'US', 'LI'))
                    for (lh, rh, mk_, dst_) in ((BBe[u], KKe[u], masks[mS_T], Na[u]), (KKe[u], BBe[u], masks[mS], NTa[u]), (KEe[u], KKe[u], masks[mS_T], LkT[u]),
                                                (BBe[u], REe[u], masks['n' + mI], nMrbT[u]), (KEe[u], REe[u], masks[mI], MrkT[u])):
                        s = slot()
                        mm1(s, lh, rh)
                        self.tt('dve', dst_.t[:], s.t[:, 0:128], mk_.t[:], ALU.mult, reads=[s.r, mk_.r], writes=[dst_.r])
                for u in units:
                    for (src_, dst_, neg) in ((KEe[u], KEt[u], False), (BBe[u], nBBt[u], True)):
                        s = slot()
                        pv_ = s.t[:, 0:128].bitcast(BF16)[:, 0:128]
                        self.tr(pv_, src_.t[:], self.ident16.t[:], reads=[src_.r, self.ident16.r], writes=[s.r])
                        if neg:
                            self.act(dst_.t[:], pv_, AF.Copy, reads=[s.r], writes=[dst_.r], scale=-1.0)
                        else:
                            self.cp('act', dst_.t[:], pv_, reads=[s.r], writes=[dst_.r])
                curN = {u: Na[u] for u in units}
                curNT = {u: NTa[u] for u in units}
                othN = {u: Nb[u] for u in units}
                othNT = {u: NTb[u] for u in units}
                X = {u: Xa[u] for u in units}
                Xo = {u: Xb[u] for u in units}
                for u in units:
                    self.tt('pool', X[u].t[:], self.ident16.t[:], curN[u].t[:], ALU.subtract, reads=[self.ident16.r, curN[u].r], writes=[X[u].r])
                for lvl in range(5):
                    for u in units:
                        s = slot()
                        mm1(s, curN[u], curNT[u])
                        self.cp(ev(), othNT[u].t[:], s.t[:, 0:128], reads=[s.r], writes=[othNT[u].r])
                        if lvl < 4:
                            s = slot()
                            mm1(s, curNT[u], curN[u])
                            self.cp(ev(), othN[u].t[:], s.t[:, 0:128], reads=[s.r], writes=[othN[u].r])
                    for u in units:
                        s = slot()
                        self.mm(s.t[:, 0:128], othNT[u].t[:], X[u].t[:], True, False, reads=[othNT[u].r, X[u].r], writes=[s.r], inc=False)
                        self.mm(s.t[:, 0:128], self.ident16.t[:], X[u].t[:], False, True, reads=[self.ident16.r, X[u].r], pwrites=[s.r])
                        xn = InvT[u] if lvl == 4 else Xo[u]
                        self.cp(ev(), xn.t[:], s.t[:, 0:128], reads=[s.r], writes=[xn.r])
                        if lvl < 4:
                            X[u], Xo[u] = Xo[u], X[u]
                    for u in units:
                        curN[u], othN[u] = othN[u], curN[u]
                        curNT[u], othNT[u] = othNT[u], curNT[u]
                for u in units:
                    d, j = u
                    vj = v16[d][c % 2]
                    s = slot()
                    self.mm(s.t[:, 0:C], KKe[u].t[:], G16[u].t[:], True, False, reads=[KKe[u].r, G16[u].r], writes=[s.r], inc=False)
                    self.mm(s.t[:, 0:C], LkT[u].t[:], vj.t[:, j, :], False, True, reads=[LkT[u].r, vj.r], pwrites=[s.r])
                    self.cp(ev(), rs16[u].t[:], s.t[:, 0:C], reads=[s.r], writes=[rs16[u].r])
                for u in units:
                    s = slot()
                    self.mm(s.t[:, 0:C], InvT[u].t[:], rs16[u].t[:], True, True, reads=[InvT[u].r, rs16[u].r], writes=[s.r])
                    self.cp(ev(), u16[u].t[:], s.t[:, 0:C], reads=[s.r], writes=[u16[u].r])
                for u in units:
                    d, j = u
                    vj = v16[d][c % 2]
                    s = slot()
                    self.mm(s.t[:, 0:C], REe[u].t[:], G16[u].t[:], True, False, reads=[REe[u].r, G16[u].r], writes=[s.r], inc=False)
                    self.mm(s.t[:, 0:C], nMrbT[u].t[:], u16[u].t[:], False, False, reads=[nMrbT[u].r, u16[u].r], pwrites=[s.r], inc=False)
                    self.mm(s.t[:, 0:C], MrkT[u].t[:], vj.t[:, j, :], False, True, reads=[MrkT[u].r, vj.r], pwrites=[s.r])
                    yc = ych[d][c % 2]
                    self.cp('act', yc.t[:, j, :], s.t[:, 0:C], reads=[s.r], pwrites=[yc.r])
                    s = slot()
                    self.mm(s.t[:, 0:C], KEt[u].t[:], vj.t[:, j, :], True, False, reads=[KEt[u].r, vj.r], writes=[s.r], inc=False)
                    self.mm(s.t[:, 0:C], nBBt[u].t[:], u16[u].t[:], False, True, reads=[nBBt[u].r, u16[u].r], pwrites=[s.r])
                    self.tt('dve', gtmp[u].t[:], s.t[:, 0:C], G32[u].t[:], ALU.add, reads=[s.r, G32[u].r], writes=[gtmp[u].r])
                    self.ts('dve', G32[u].t[:], gtmp[u].t[:], tot[u].t[:, 0:1], None, ALU.mult, reads=[gtmp[u].r, tot[u].r], writes=[G32[u].r])
                    self.cp('act', G16[u].t[:], G32[u].t[:], reads=[G32[u].r], writes=[G16[u].r])
                cf0, cb0, tf0, tb0 = chunk_ranges(c)
                for d, t0 in ((0, tf0), (1, tb0)):
                    yc = ych[d][c % 2]
                    for h2 in range(2):
                        dst = YS['ap'][d, t0:t0 + C, :].rearrange("t (j h v) -> t j h v", j=8, h=2)[:, :, h2, :]
                        self.dma(dst, yc.t[h2 * 64:(h2 + 1) * 64, :, :], reads=[yc.r], pwrites=[YS['r']])
            if hook:
                hook(final=True)
        self.P.barrier()


WNAMES = ['ada_w', 'ada_b', 'ln1_g', 'ln1_b', 'ln2_g', 'ln2_b', 'even_w_in', 'even_w_out',
          'rw_mu', 'rw_w0', 'rw_w_up', 'rw_a0', 'rw_a_up', 'rw_g_up', 'rw_kk', 'rw_ka', 'rw_rk', 'rw_gn_g', 'rw_gn_b',
          'cv_w', 'cv_b', 'cv_ln_g', 'cv_ln_b', 'odd_w_in', 'odd_w_out', 'q_norm', 'k_norm',
          'moe_router', 'moe_bias', 'moe_w1', 'moe_w3', 'moe_w2', 'sh_w1', 'sh_w3', 'sh_w2']
WSHAPES = {'ada_w': (4, 2048, 12288), 'ada_b': (4, 12288), 'ln1_g': (4, 2048), 'ln1_b': (4, 2048), 'ln2_g': (4, 2048), 'ln2_b': (4, 2048),
           'even_w_in': (2, 2048, 5568), 'even_w_out': (2, 2048, 2048), 'rw_mu': (2, 2, 3520), 'rw_w0': (2, 2, 1024),
           'rw_w_up': (2, 2, 96, 1024), 'rw_a0': (2, 2, 1024), 'rw_a_up': (2, 2, 96, 1024), 'rw_g_up': (2, 64, 1024),
           'rw_kk': (2, 1024), 'rw_ka': (2, 1024), 'rw_rk': (2, 16, 64), 'rw_gn_g': (2, 1024), 'rw_gn_b': (2, 1024),
           'cv_w': (2, 31, 1024), 'cv_b': (2, 1024), 'cv_ln_g': (2, 1024), 'cv_ln_b': (2, 1024),
           'odd_w_in': (2, 2048, 3072), 'odd_w_out': (2, 2048, 2048), 'q_norm': (2, 128), 'k_norm': (2, 128),
           'moe_router': (4, 2048, 64), 'moe_bias': (4, 64), 'moe_w1': (4, 64, 2048, 384), 'moe_w3': (4, 64, 2048, 384),
           'moe_w2': (4, 64, 384, 2048), 'sh_w1': (4, 2048, 384), 'sh_w3': (4, 2048, 384), 'sh_w2': (4, 384, 2048)}


def host_consts():
    tok = np.arange(4096)
    row = tok // 64
    col = tok % 64
    inv = 10000.0 ** (-np.arange(32, dtype=np.float32) / 32)
    ang = np.zeros((128, 4096), np.float32)
    for a, pos in enumerate((row, col)):
        for half in range(2):
            ang[a * 64 + half * 32:a * 64 + half * 32 + 32, :] = inv[:, None] * pos[None, :].astype(np.float32)
    cosT = np.cos(ang).astype(np.float32)
    sinT = np.sin(ang).astype(np.float32)
    rotT = np.zeros((128, 128), np.float32)
    for m in range(128):
        half = (m % 64) // 32
        if half == 0:
            rotT[m + 32, m] = -1.0
        else:
            rotT[m - 32, m] = 1.0
    return {'cosT': cosT, 'sinT': sinT, 'rotT': rotT}


def build_program(plan=None, dbg_out=(), dbg_in=(), declare=None):
    nc = bass.Bass("TRN2", target_bir_lowering=False)
    mk = MK(nc, dbg_out)
    ins = {}
    for k in WNAMES:
        if declare is not None and k not in declare:
            continue
        ins[k] = nc.dram_tensor(k, list(WSHAPES[k]), F32, kind="ExternalInput").ap()
    xin = nc.dram_tensor("xin", [T, D], F32, kind="ExternalInput").ap()
    cvec = nc.dram_tensor("cvec", [2, D], F32, kind="ExternalInput").ap()
    cosT = nc.dram_tensor("cosT", [128, 4096], F32, kind="ExternalInput").ap()
    sinT = nc.dram_tensor("sinT", [128, 4096], F32, kind="ExternalInput").ap()
    rotT = nc.dram_tensor("rotT", [128, 128], F32, kind="ExternalInput").ap()
    out = nc.dram_tensor("out", [4096, D], F32, kind="ExternalOutput").ap()

    def scratch(name, shape, dt, nres=None):
        kind = "ExternalInput" if name in dbg_in else None
        ap = mk.dram(name, shape, dt, kind=kind)
        if nres is None:
            return {'ap': ap, 'r': Res(name)}
        return {'ap': ap, 'r': [Res(name) for _ in range(nres)]}
    with mk.top:
        mk.setup()
        H = {'ap': xin, 'r': [Res() for _ in range(NT)]}
        Hs = scratch('H', [T, D], F32, NT)
        Y = scratch('Y', [T, D], F32, NT)
        Fm = scratch('F', [T, D], F32, NT)
        MODB = scratch('MODB', [4, 2, 128, 12288], F32)
        QT = scratch('QT', [20, 128, T], BF16)
        Vd = scratch('V', [T, 512], BF16)
        OT = scratch('OT', [16, 128, T], BF16)
        OUT = {'ap': out, 'r': [Res() for _ in range(NT)]}
        PT = scratch('PT', [45, 128, TP], F32)
        WB = scratch('WB', [65 * 128, 12288], BF16)
        WB['ap2'] = mk.dram('WB2', [65 * 128, 6144], BF16)
        VF = scratch('VF', [T, D], BF16)
        TOKBUF = scratch('TOKBUF', [115 * 512, 16], I32)
        YB = scratch('YB', [115 * 512, D], BF16)
        YB['ap2'] = mk.dram('YSHB', [T, D], BF16)
        SCN = scratch('SCN', [8, 8, 128, TP], F32)
        VH = scratch('VH', [2, T, 512], BF16)
        V32 = scratch('V32', [T, 1024], F32)
        G = scratch('G', [T, 1024], F32)
        RK = scratch('RK', [T, 32], F32)
        YS = scratch('YS', [2, T, 1024], F32)
        CAT = scratch('CAT', [T, 2048], BF16)

        def EW(j):
            return {'mu': ins['rw_mu'][j], 'w0': ins['rw_w0'][j], 'w_up': ins['rw_w_up'][j], 'a0': ins['rw_a0'][j], 'a_up': ins['rw_a_up'][j],
                    'g_up': ins['rw_g_up'][j], 'kk': ins['rw_kk'][j], 'ka': ins['rw_ka'][j], 'rk': ins['rw_rk'][j],
                    'gn_g': ins['rw_gn_g'][j], 'gn_b': ins['rw_gn_b'][j], 'cv_w': ins['cv_w'][j], 'cv_b': ins['cv_b'][j],
                    'cv_ln_g': ins['cv_ln_g'][j], 'cv_ln_b': ins['cv_ln_b'][j]}
        if plan is None:
            plan = full_plan()
        for (stg, l) in plan:
            j = l // 2 if isinstance(l, int) else 0
            last = (l == 3)
            if stg == 'copyin':
                with contextlib.ExitStack() as sc:
                    hb = [mk.sb(sc, [128, D], F32, 'cp') for _ in range(2)]
                    for ti in range(NT):
                        mk.dma(hb[ti % 2].t[:], xin[ti * 128:(ti + 1) * 128, :], writes=[hb[ti % 2].r])
                        mk.dma(Hs['ap'][ti * 128:(ti + 1) * 128, :], hb[ti % 2].t[:], reads=[hb[ti % 2].r], writes=[Hs['r'][ti]])
                mk.P.barrier()
            elif stg == 'mod':
                mk.stage_mod(cvec, ins['ada_w'], ins['ada_b'], MODB, layers=l if isinstance(l, (list, range)) else [l])
            elif stg == 'moe':
                mk.stage_moe(Hs, Fm, MODB, l, ins['moe_router'][l], ins['moe_bias'][l], ins['moe_w1'], ins['moe_w3'], ins['moe_w2'],
                             ins['sh_w1'], ins['sh_w3'], ins['sh_w2'], with_ctx=not last)
            elif stg == 'moeconv':
                mk.stage_moe_conv(l, ins['moe_w1'], ins['moe_w3'], ins['moe_w2'], ins['sh_w1'], ins['sh_w3'], ins['sh_w2'], WB)
            elif stg == 'moes':
                mk.stage_moe_sparse(Hs, Fm, MODB, l, ins['moe_router'][l], ins['moe_bias'][l], WB, VF, TOKBUF, YB, with_ctx=not last)
            elif stg == 'ln1':
                tiles = range(32) if last else range(NT)
                mk.stage_ln(Hs, Y, MODB, l, 4096, ins['ln1_g'][l], ins['ln1_b'][l], tiles)
            elif stg == 'ln2':
                tiles = range(32) if last else range(NT)
                mk.stage_ln(Hs, Fm, MODB, l, 10240, ins['ln2_g'][l], ins['ln2_b'][l], tiles, OUTF=OUT if last else None)
            elif stg == 'qkv':
                mk.stage_qkv(Hs, MODB, l, ins['odd_w_in'][j], ins['q_norm'][j], ins['k_norm'][j], cosT, sinT, rotT, QT, Vd)
            elif stg == 'attn':
                mk.stage_attn(QT, Vd, OT, with_ctx=not last)
            elif stg == 'oproj':
                mk.stage_proj_T(OT, ins['odd_w_out'][j], Y, with_ctx=not last)
            elif stg == 'zpad':
                mk.stage_zpad(PT)
            elif stg == 'ein':
                mk.stage_ein(Hs, MODB, l, ins['even_w_in'][j], PT)
            elif stg == 'efeat':
                mk.stage_efeat(j, EW(j), PT, SCN, VH, V32, G, RK)
            elif stg == 'escan':
                mk.stage_escan(SCN, VH, YS)
            elif stg == 'escanc':
                mk.stage_escan_chunked(SCN, V32, YS)
            elif stg == 'escanc2':
                mk.stage_escan_chunked2(SCN, V32, YS)
            elif stg == 'escanc2w':
                mk.stage_escan_chunked2(SCN, V32, YS, conv=(l, ins['moe_w1'], ins['moe_w3'], ins['moe_w2'], ins['sh_w1'], ins['sh_w3'], ins['sh_w2'], WB))
            elif stg == 'attnw':
                mk.stage_attn(QT, Vd, OT, with_ctx=not last, conv=(l, ins['moe_w1'], ins['moe_w3'], ins['moe_w2'], ins['sh_w1'], ins['sh_w3'], ins['sh_w2'], WB))
            elif stg == 'eepi':
                mk.stage_eepi(EW(j), YS, V32, G, RK, CAT)
            elif stg == 'econv':
                mk.stage_econv(EW(j), PT, CAT)
            elif stg == 'eproj':
                mk.stage_eproj(CAT, ins['even_w_out'][j], Y)
            else:
                raise ValueError(stg)
        fin = [r for r in OUT['r']]
        for d in (Hs, Y, Fm):
            fin += d['r']
        fin += [WB['r'], VF['r'], TOKBUF['r'], YB['r'], MODB['r'], QT['r'], Vd['r'], OT['r'], PT['r'], SCN['r'], VH['r'], V32['r'], G['r'], RK['r'], YS['r'], CAT['r']]
        mk.P.finish(fin)
        mk.P.emit()
    return nc


def full_plan():
    plan = [('copyin', 0), ('zpad', 0), ('mod', range(4))]
    for l in range(4):
        if l % 2 == 0:
            plan += [('ein', l), ('efeat', l), ('escanc2', l), ('eepi', l), ('econv', l), ('eproj', l)]
            plan += [('ln1', l), ('moeconv', l), ('moes', l), ('ln2', l)]
        else:
            plan += [('qkv', l), ('attnw', l), ('oproj', l)]
            plan += [('ln1', l), ('moes', l), ('ln2', l)]
    return plan


_CACHE = {}


def kernel(**inputs):
    n = 8
    if 'nc' not in _CACHE:
        _CACHE['nc'] = build_program()
    nc = _CACHE['nc']
    consts = host_consts()
    shared = {k: np.ascontiguousarray(np.asarray(inputs[k], dtype=np.float32)) for k in WNAMES}
    x = np.asarray(inputs['x'], dtype=np.float32)
    ctx = np.asarray(inputs['ctx'], dtype=np.float32)
    c = np.asarray(inputs['c'], dtype=np.float32)
    c_ctx = np.asarray(inputs['c_ctx'], dtype=np.float32)
    in_maps = []
    for b in range(n):
        m = dict(shared)
        m.update(consts)
        m['xin'] = np.ascontiguousarray(np.concatenate([x[b], ctx[b]], axis=0))
        m['cvec'] = np.ascontiguousarray(np.stack([c[b], c_ctx], axis=0))
        in_maps.append(m)
    res = run_bass_kernel_spmd(nc, in_maps, core_ids=list(range(n)))
    return np.stack([res.results[b]['out'] for b in range(n)], axis=0).astype(np.float32)
```
